# Optimizing a Trainium2 kernel written in Bass

```python
import math
import jax, jax.numpy as jnp
from jax import lax
import numpy as np

D_MODEL = 2048
BATCH = 1
SEQ = 16384
DEPTH = 2

MIX_WIDTH = D_MODEL
POOL_WIDTH = MIX_WIDTH // 2
POOL_WINDOWS = (2, 4, 8, 16)
N_POOL_GROUPS = len(POOL_WINDOWS)
POOL_GROUP = POOL_WIDTH // N_POOL_GROUPS
GLA_WIDTH = MIX_WIDTH - POOL_WIDTH
GLA_HEADS = 4
GLA_DV = GLA_WIDTH // GLA_HEADS
GLA_KEY_WIDTH = GLA_WIDTH // 2
GLA_DK = GLA_KEY_WIDTH // GLA_HEADS
GATE_RANK = 16
GATE_TAU = 16.0
CHUNK = 64
IN_PROJ_WIDTH = POOL_WIDTH + 2 * GLA_KEY_WIDTH + GLA_WIDTH + GATE_RANK + GLA_WIDTH
D_FF_DENSE = 5632
N_EXPERTS = 8
TOP_K = 2
D_FF_EXPERT = 7168
MOE_BLOCK = 256
EPS = 1e-6
N_DENSE = (DEPTH + 1) // 2
N_MOE = DEPTH // 2

kernel_name = "hybrid_pool_gla_moe_trunk"


def rmsnorm(x, g):
    xf = x.astype(jnp.float32)
    y = xf * lax.rsqrt(jnp.mean(xf * xf, axis=-1, keepdims=True) + EPS)
    return (y * g.astype(jnp.float32)).astype(x.dtype)


def pool_mixer(u, w_pool, pool_scale):
    B, S, _ = u.shape
    ug = u.reshape(B, S, N_POOL_GROUPS, POOL_GROUP)
    c = jnp.cumsum(ug.astype(jnp.float32), axis=1)
    c = jnp.pad(c, ((0, 0), (1, 0), (0, 0), (0, 0)))
    t1 = jnp.arange(1, S + 1)
    diffs = []
    for gi, w in enumerate(POOL_WINDOWS):
        lo = jnp.maximum(t1 - w, 0)
        cg = c[:, :, gi]
        win_sum = cg[:, 1:] - cg[:, lo]
        count = (t1 - lo).astype(jnp.float32)[None, :, None]
        diffs.append(win_sum / count - ug[:, :, gi].astype(jnp.float32))
    d = jnp.stack(diffs, axis=2)
    y = jnp.einsum('bsgc,gcd->bsgd', d, w_pool.astype(jnp.float32)) * pool_scale.astype(jnp.float32)
    return y.reshape(B, S, POOL_WIDTH).astype(u.dtype)


def gla_mixer(q, k, v, gate_lr, r, w_gate_up, b_gate, gla_norm):
    B, S, _ = q.shape
    H, C, N = GLA_HEADS, CHUNK, S // CHUNK
    f32 = jnp.float32
    q = q.astype(f32).reshape(B, N, C, H, GLA_DK) * (GLA_DK ** -0.5)
    k = k.astype(f32).reshape(B, N, C, H, GLA_DK)
    v = v.astype(f32).reshape(B, N, C, H, GLA_DV)
    g = jax.nn.log_sigmoid(gate_lr.astype(f32) @ w_gate_up.astype(f32) + b_gate.astype(f32)) / GATE_TAU
    g = g.reshape(B, N, C, H, GLA_DK)
    bc = jnp.cumsum(g, axis=2)
    b_last = bc[:, :, -1]
    q_dec = q * jnp.exp(bc)
    k_dec = k * jnp.exp(-bc)
    k_to_end = k * jnp.exp(b_last[:, :, None] - bc)
    chunk_state = jnp.einsum('bnchk,bnchv->bnhkv', k_to_end, v)
    decay = jnp.exp(b_last)

    def step(state, inp):
        dec, cs = inp
        return dec[..., None] * state + cs, state

    init = jnp.zeros((B, H, GLA_DK, GLA_DV), f32)
    _, s_prev = lax.scan(step, init, (jnp.moveaxis(decay, 1, 0), jnp.moveaxis(chunk_state, 1, 0)))
    s_prev = jnp.moveaxis(s_prev, 0, 1)
    o_inter = jnp.einsum('bnchk,bnhkv->bnchv', q_dec, s_prev)
    att = jnp.einsum('bnihk,bnjhk->bnhij', q_dec, k_dec)
    causal = jnp.tril(jnp.ones((C, C), dtype=bool))
    att = jnp.where(causal, att, 0.0)
    o_intra = jnp.einsum('bnhij,bnjhv->bnihv', att, v)
    o = (o_inter + o_intra).reshape(B, S, H, GLA_DV)
    o = o * lax.rsqrt(jnp.mean(o * o, axis=-1, keepdims=True) + EPS) * gla_norm.astype(f32).reshape(H, GLA_DV)
    o = o.reshape(B, S, GLA_WIDTH) * jax.nn.silu(r.astype(f32))
    return o.astype(r.dtype)


def swiglu(h, w_gate, w_up, w_down):
    return (jax.nn.silu(h @ w_gate) * (h @ w_up)) @ w_down


def moe_swiglu(h, w_router, e_gate, e_up, e_down):
    B, S, D = h.shape
    T = B * S
    A = T * TOP_K
    hf = h.reshape(T, D)
    logits = (hf @ w_router).astype(jnp.float32)
    top_vals, top_idx = lax.top_k(logits, TOP_K)
    top_w = jax.nn.softmax(top_vals, axis=-1)
    flat_e = top_idx.reshape(-1)
    flat_w = top_w.reshape(-1)
    flat_tok = jnp.arange(A, dtype=jnp.int32) // TOP_K
    order = jnp.argsort(flat_e)
    e_sorted = flat_e[order]
    counts = jnp.bincount(flat_e, length=N_EXPERTS)
    padded = ((counts + MOE_BLOCK - 1) // MOE_BLOCK) * MOE_BLOCK
    grp_start = jnp.cumsum(counts) - counts
    cum_padded = jnp.cumsum(padded)
    pad_start = cum_padded - padded
    dest = pad_start[e_sorted] + (jnp.arange(A) - grp_start[e_sorted])
    n_blocks = (A + MOE_BLOCK - 1) // MOE_BLOCK + N_EXPERTS
    L = n_blocks * MOE_BLOCK
    slot_tok = jnp.full((L,), T, jnp.int32).at[dest].set(flat_tok[order])
    slot_w = jnp.zeros((L,), jnp.float32).at[dest].set(flat_w[order])
    block_expert = jnp.minimum(
        jnp.searchsorted(cum_padded, jnp.arange(n_blocks) * MOE_BLOCK, side='right'), N_EXPERTS - 1)
    x_pad = jnp.concatenate([hf, jnp.zeros((1, D), hf.dtype)], axis=0)
    xb = x_pad[slot_tok].reshape(n_blocks, MOE_BLOCK, D)

    def run_block(args):
        xblk, e = args
        return (jax.nn.silu(xblk @ e_gate[e]) * (xblk @ e_up[e])) @ e_down[e]

    yb = lax.map(run_block, (xb, block_expert))
    y = yb.reshape(L, D) * slot_w.astype(yb.dtype)[:, None]
    out = jnp.zeros((T + 1, D), y.dtype).at[slot_tok].add(y)[:T]
    return out.reshape(B, S, D)


def setup_inputs(seed: int = 0) -> dict:
    key = jax.random.key(seed)
    ks = jax.random.split(key, 20)
    nrm = lambda k, shape, fan_in: jax.random.normal(k, shape, jnp.float32) * (fan_in ** -0.5)
    gain = lambda k, shape: 1.0 + 0.02 * jax.random.normal(k, shape, jnp.float32)
    return {
        "x": jax.random.normal(ks[0], (BATCH, SEQ, D_MODEL), jnp.float32),
        "mix_norm": gain(ks[1], (DEPTH, D_MODEL)),
        "w_in": nrm(ks[2], (DEPTH, D_MODEL, IN_PROJ_WIDTH), D_MODEL),
        "w_pool": nrm(ks[3], (DEPTH, N_POOL_GROUPS, POOL_GROUP, POOL_GROUP), POOL_GROUP),
        "pool_scale": gain(ks[4], (DEPTH, N_POOL_GROUPS, POOL_GROUP)),
        "w_gate_up": nrm(ks[5], (DEPTH, GATE_RANK, GLA_KEY_WIDTH), GATE_RANK),
        "b_gate": 0.1 * jax.random.normal(ks[6], (DEPTH, GLA_KEY_WIDTH), jnp.float32),
        "gla_norm": gain(ks[7], (DEPTH, GLA_WIDTH)),
        "w_out": nrm(ks[8], (DEPTH, MIX_WIDTH, D_MODEL), MIX_WIDTH),
        "ffn_norm": gain(ks[9], (DEPTH, D_MODEL)),
        "dense_w_gate": nrm(ks[10], (N_DENSE, D_MODEL, D_FF_DENSE), D_MODEL),
        "dense_w_up": nrm(ks[11], (N_DENSE, D_MODEL, D_FF_DENSE), D_MODEL),
        "dense_w_down": nrm(ks[12], (N_DENSE, D_FF_DENSE, D_MODEL), D_FF_DENSE),
        "w_router": nrm(ks[13], (N_MOE, D_MODEL, N_EXPERTS), D_MODEL),
        "exp_w_gate": nrm(ks[14], (N_MOE, N_EXPERTS, D_MODEL, D_FF_EXPERT), D_MODEL),
        "exp_w_up": nrm(ks[15], (N_MOE, N_EXPERTS, D_MODEL, D_FF_EXPERT), D_MODEL),
        "exp_w_down": nrm(ks[16], (N_MOE, N_EXPERTS, D_FF_EXPERT, D_MODEL), D_FF_EXPERT),
        "final_norm": gain(ks[17], (D_MODEL,)),
    }


def reference(x, mix_norm, w_in, w_pool, pool_scale, w_gate_up, b_gate, gla_norm, w_out,
              ffn_norm, dense_w_gate, dense_w_up, dense_w_down, w_router,
              exp_w_gate, exp_w_up, exp_w_down, final_norm):
    splits = np.cumsum([POOL_WIDTH, GLA_KEY_WIDTH, GLA_KEY_WIDTH, GLA_WIDTH, GATE_RANK]).tolist()
    for l in range(DEPTH):
        h = rmsnorm(x, mix_norm[l])
        z = h @ w_in[l]
        u_pool, q, k, v, gate_lr, r = jnp.split(z, splits, axis=-1)
        pool_out = pool_mixer(u_pool, w_pool[l], pool_scale[l])
        gla_out = gla_mixer(q, k, v, gate_lr, r, w_gate_up[l], b_gate[l], gla_norm[l])
        x = x + jnp.concatenate([pool_out, gla_out], axis=-1) @ w_out[l]
        h = rmsnorm(x, ffn_norm[l])
        if l % 2 == 0:
            i = l // 2
            x = x + swiglu(h, dense_w_gate[i], dense_w_up[i], dense_w_down[i])
        else:
            i = l // 2
            x = x + moe_swiglu(h, w_router[i], exp_w_gate[i], exp_w_up[i], exp_w_down[i])
    return rmsnorm(x, final_norm)
```

```python
import numpy as np
import ml_dtypes
import concourse.bass as bass
import concourse.mybir as mybir
from concourse.bass_utils import run_bass_kernel_spmd

F32 = mybir.dt.float32
BF16 = mybir.dt.bfloat16
AF = mybir.ActivationFunctionType
ALU = mybir.AluOpType
AX = mybir.AxisListType

NCORES = 8
D = 2048
SEQ = 16384
TPC = SEQ // NCORES
NT = TPC // 128
ST = 4
NST = NT // ST
KC = D // 128
INW = 4112
FF_DENSE = 5632
FF_EXP = 7168
NEXP = 8
EPS = 1e-6
POOL_WINDOWS = (2, 4, 8, 16)
WSLOT = 8192
NWSLOT = 3
STOP = 99
EVAC_ACT_SCALE = False


class TT:
    def __init__(self, t, esz, name):
        self.t = t
        self.esz = esz
        self.name = name
        self.hist = []
        self.whole = False

    def c(self, lo, hi, p0=0, p1=None):
        ap = self.t[p0:p1, lo:hi] if p1 is not None else (self.t[p0:, lo:hi] if p0 else self.t[:, lo:hi])
        if self.whole:
            return V(self, ap, 0, 1 << 20)
        return V(self, ap, lo * self.esz, hi * self.esz)


class V:
    def __init__(self, tt, ap, lo, hi):
        self.tt = tt
        self.ap = ap
        self.lo = lo
        self.hi = hi

    def r(self, pat, **kw):
        return V(self.tt, self.ap.rearrange(pat, **kw), self.lo, self.hi)

    def s(self, *key):
        return V(self.tt, self.ap[key], self.lo, self.hi)


class Eng:
    def __init__(self, key, sem):
        self.key = key
        self.sem = sem
        self.count = 0
        self.known = {}
        self.ops = []
        self.lanes = []
        self.lane_val = []
        self.lane_next = 0


class Prog:
    def __init__(self, nc):
        self.nc = nc
        self.E = {}
        self.sems = {}

    def add_engine(self, key, sem, lanes=()):
        e = Eng(key, sem)
        self.sems[id(sem)] = sem
        e.lanes = list(lanes)
        e.lane_val = [0] * len(lanes)
        for s in lanes:
            self.sems[id(s)] = s
        self.E[key] = e

    def _need(self, e, reads, writes):
        need = {}

        def add(h):
            if h[2] == 'pe' and e.key == 'pe' and h[3] == id(e.sem):
                return
            if need.get(h[3], 0) < h[4]:
                need[h[3]] = h[4]
        for v in reads:
            whole = v.tt.whole
            for h in v.tt.hist:
                if (h[5] or (whole and h[2] != e.key)) and h[0] < v.hi and v.lo < h[1]:
                    add(h)
        for v in writes:
            for h in v.tt.hist:
                if h[0] < v.hi and v.lo < h[1]:
                    add(h)
        waits = []
        for sid, val in need.items():
            if e.known.get(sid, 0) < val:
                e.known[sid] = val
                waits.append((self.sems[sid], val))
        return waits

    def _record(self, e, reads, writes, sid, val):
        for v in writes:
            hist = v.tt.hist
            v.tt.hist = [h for h in hist if not (h[0] >= v.lo and h[1] <= v.hi)]
            v.tt.hist.append([v.lo, v.hi, e.key, sid, val, True])
        for v in reads:
            hist = v.tt.hist
            v.tt.hist = [h for h in hist if not ((not h[5]) and h[3] == sid and h[0] >= v.lo and h[1] <= v.hi)]
            v.tt.hist.append([v.lo, v.hi, e.key, sid, val, False])

    def op(self, ek, fn, reads=(), writes=(), sig=True):
        e = self.E[ek]
        waits = self._need(e, reads, writes)
        val = e.count + 1
        if sig:
            e.count = val
        e.ops.append((waits, fn, (e.sem, 1) if sig else None))
        self._record(e, reads, writes, id(e.sem), val)

    def dma(self, ek, fn, reads=(), writes=()):
        e = self.E[ek]
        li = e.lane_next
        e.lane_next = (li + 1) % len(e.lanes)
        sem = e.lanes[li]
        waits = self._need(e, reads, writes)
        prev = e.lane_val[li]
        if prev > 0 and e.known.get(id(sem), 0) < prev:
            e.known[id(sem)] = prev
            waits.append((sem, prev))
        val = prev + 16
        e.lane_val[li] = val
        e.ops.append((waits, fn, (sem, 16)))
        self._record(e, reads, writes, id(sem), val)
        return sem, val

    def wait_all(self, ek, items):
        e = self.E[ek]
        waits = [(s, v) for (s, v) in items]
        e.ops.append((waits, None, None))

    def emit_engine(self, ek, h):
        for waits, fn, inc in self.E[ek].ops:
            for s, v in waits:
                h.wait_ge(s, v)
            if fn is None:
                continue
            ins = fn(h)
            if inc is not None:
                ins.then_inc(inc[0], inc[1])


def _pool_mats(first_core):
    cur = np.zeros((4, 128, 128), np.float32)
    cur0 = np.zeros((4, 128, 128), np.float32)
    prev = np.zeros((4, 128, 128), np.float32)
    for g, w in enumerate(POOL_WINDOWS):
        for t in range(128):
            for s in range(t - w + 1, t + 1):
                if s >= 0:
                    cur[g, s, t] += 1.0 / w
                else:
                    prev[g, 128 + s, t] += 1.0 / w
            cur[g, t, t] -= 1.0
            cnt = min(t + 1, w)
            for s in range(max(0, t - w + 1), t + 1):
                cur0[g, s, t] += 1.0 / cnt
            cur0[g, t, t] -= 1.0
    return cur, (cur0 if first_core else cur), prev


def _consts(core):
    bf = ml_dtypes.bfloat16
    c = {}
    c["ident_f"] = np.eye(128, dtype=np.float32)
    c["ident_b"] = np.eye(128, dtype=np.float32).astype(bf)
    j = np.arange(128)[:, None]
    i = np.arange(128)[None, :]
    tri = (j <= i).astype(np.float32)
    c["trin"] = (tri * (-1.0 / 16.0)).astype(bf)
    c["mask4"] = np.tile(tri, (1, 4)).astype(bf)
    cur, cur0, prev = _pool_mats(core == 0)
    c["mcur"] = np.ascontiguousarray(cur.transpose(1, 0, 2).reshape(128, 512)).astype(bf)
    c["mcur0"] = np.ascontiguousarray(cur0.transpose(1, 0, 2).reshape(128, 512)).astype(bf)
    c["mprev"] = np.ascontiguousarray(prev.transpose(1, 0, 2).reshape(128, 512)).astype(bf)
    m = np.zeros((128, 8), np.float32)
    m[:, :core] = 1.0
    c["cmask"] = m
    return c


def _fm(vec):
    return np.ascontiguousarray(vec.reshape(-1, 128).T)


def build(layer_kind, phase, final):
    nc = bass.Bass("TRN2", target_bir_lowering=False)
    moe = layer_kind == 'moe'
    FF = FF_EXP if moe else FF_DENSE
    NE = NEXP if moe else 1
    NFC = FF // 128
    HALF = NFC // 2

    def din(name, shape, dt=F32):
        return nc.dram_tensor(name, list(shape), dt, kind="ExternalInput").ap()

    def dout(name, shape, dt=F32):
        return nc.dram_tensor(name, list(shape), dt, kind="ExternalOutput").ap()

    x_in = din("x_in", [TPC, D])
    mixg = din("mixg", [128, KC])
    w_in = din("w_in", [D, INW])
    wgu = din("wgu", [16, 512])
    bgate = din("bgate", [1, 512])
    ident_f_d = din("ident_f", [128, 128])
    ident_b_d = din("ident_b", [128, 128], BF16)
    trin_d = din("trin", [128, 128], BF16)
    if phase == 1:
        lst_o = dout("lst", [128, 1024])
        bsum_o = dout("bsum", [128, 4])
        ulast_o = dout("ulast", [128, 1024], BF16)
    else:
        mask4_d = din("mask4", [128, 512], BF16)
        mcur_d = din("mcur", [128, 512], BF16)
        mcur0_d = din("mcur0", [128, 512], BF16)
        mprev_d = din("mprev", [128, 512], BF16)
        cmask_d = din("cmask", [128, 8])
        lall = din("lall", [NCORES * 128, 1024])
        ball = din("ball", [NCORES * 128, 4])
        uhalo = din("uhalo", [128, 1024], BF16)
        w_pool = din("w_pool", [4 * 256, 256])
        pscale = din("pscale", [128, 8])
        gnorm = din("gnorm", [1, 1024])
        w_out = din("w_out", [D, D])
        ffng = din("ffng", [128, KC])
        if moe:
            w_router = din("w_router", [D, NEXP])
            wg_d = din("wg", [NEXP * D, FF])
            wu_d = din("wu", [NEXP * D, FF])
            wd_d = din("wd", [NEXP * FF, D])
        else:
            wg_d = din("wg", [D, FF])
            wu_d = din("wu", [D, FF])
            wd_d = din("wd", [FF, D])
        if final:
            fing = din("fing", [1, D])
        x_out = dout("x_out", [TPC, D])

    import contextlib
    es = contextlib.ExitStack()
    with es:
        def sb(name, n, dt=F32, p=128):
            t = es.enter_context(nc.sbuf_tensor("sb_" + name, [p, n], dt))
            return TT(t, 4 if dt == F32 else 2, name)

        def ps(name, n, dt=F32):
            t = es.enter_context(nc.psum_tensor("ps_" + name, [128, n], dt))
            tt = TT(t, 4 if dt == F32 else 2, name)
            tt.whole = True
            return tt

        def sem(name):
            return es.enter_context(nc.semaphore(name))

        P = Prog(nc)
        P.add_engine('pe', sem("s_pe"))
        P.add_engine('act', sem("s_act"))
        P.add_engine('dve', sem("s_dve"))
        P.add_engine('pool', sem("s_pool"), [sem("lp%d" % i) for i in range(16)])
        P.add_engine('sp', sem("s_sp"), [sem("ls%d" % i) for i in range(16)])

        xt = [sb("xt%d" % i, D) for i in range(ST)]
        hs = sb("hs", D)
        hT = sb("hT", KC * 512, BF16)
        wsl = [sb("wsl%d" % i, WSLOT, BF16) for i in range(NWSLOT)]
        arena = sb("arena", 27 * 1024, BF16)
        mixT = hT
        stat = sb("stat", 64)
        cst_identf = sb("identf", 128)
        cst_identb = sb("identb", 128, BF16)
        cst_trin = sb("trin", 128, BF16)
        g_mix = sb("g_mix", KC)
        wgu_a = sb("wgu_a", 512, BF16, p=32)
        Sf = sb("Sf", 1024)
        Sb = sb("Sb", 1024, BF16)
        glT = sb("glT", 512, BF16, p=32)
        if phase == 2:
            cst_mask4 = sb("mask4", 512, BF16)
            cst_mcur = sb("mcur", 512, BF16)
            cst_mcur0 = sb("mcur0", 512, BF16)
            cst_mprev = sb("mprev", 512, BF16)
            cmask = sb("cmask", 8)
            g_ffn = sb("g_ffn", KC)
            psc = sb("psc", 8)
            gnb = sb("gnb", 1024)
            wpool = sb("wpool", 8 * 256, BF16)
            uprev = sb("uprev", 1024, BF16)
            if moe:
                wr = sb("wr", KC * NEXP)
                hTf = sb("hTf", KC * 128)
                wgt = sb("wgt", ST * NEXP)
                rt = sb("rt", 64)
            if final:
                fgb = hTf if moe else sb("fgb", D)
        bsum = sb("bsum", 4)

        AO = {}
        off = 0

        def carve(name, n_bf16):
            nonlocal off
            AO[name] = (off, off + n_bf16)
            off += n_bf16
        carve("z", ST * 3072)
        carve("qT", ST * 512)
        carve("kT", ST * 512)
        carve("ysp", 1024)
        carve("sp", 512)
        carve("epos", 1024)
        carve("eneg", 1024)
        carve("qd", 512)
        carve("kd", 512)
        carve("kteT", 512)
        carve("kte", 512)
        carve("att", 512)
        carve("otmp", 2048)
        carve("sr", 1024)
        carve("og", 1024)
        carve("dT", 1024)
        assert off <= 27 * 1024, off

        def A(name, lo=0, hi=None, f32=False):
            a, b = AO[name]
            if f32:
                n = (b - a) // 2
                hi_ = n if hi is None else hi
                ap = arena.t[:, a:b].bitcast(F32)[:, lo:hi_]
                return V(arena, ap, a * 2 + lo * 4, a * 2 + hi_ * 4)
            hi_ = (b - a) if hi is None else hi
            return arena.c(a + lo, a + hi_)

        def ACT_(fc, lo=0, hi=512):
            base = fc * 512
            return arena.c(base + lo, base + hi)
        assert HALF * 512 <= 27 * 1024

        pb = [ps("pb%d" % i, 512) for i in range(7)]
        pbb = ps("pbb", 1024, BF16)

        def load(eng, dst, src_ap, **kw):
            P.dma(eng, lambda h: h.dma_start(out=dst.ap, in_=src_ap, **kw), writes=[dst])

        rr = [0]

        def evac_copy(dst, src, scale_ap=None):
            rr[0] ^= 1
            if rr[0] and (scale_ap is None or EVAC_ACT_SCALE):
                if scale_ap is None:
                    P.op('act', lambda h: h.activation(out=dst.ap, in_=src.ap, func=AF.Copy), reads=[src], writes=[dst])
                else:
                    P.op('act', lambda h: h.activation(out=dst.ap, in_=src.ap, func=AF.Identity, scale=scale_ap.ap),
                         reads=[src, scale_ap], writes=[dst])
            else:
                if scale_ap is None:
                    P.op('dve', lambda h: h.tensor_copy(out=dst.ap, in_=src.ap), reads=[src], writes=[dst])
                else:
                    P.op('dve', lambda h: h.tensor_scalar(out=dst.ap, in0=src.ap, scalar1=scale_ap.ap, scalar2=None,
                                                          op0=ALU.mult), reads=[src, scale_ap], writes=[dst])

        def mm(out, lhsT, rhs, start, stop):
            P.op('pe', lambda h: h.matmul(out.ap, lhsT=lhsT.ap, rhs=rhs.ap, start=start, stop=stop),
                 reads=[lhsT, rhs], writes=[out], sig=stop)

        def tr(out, in_, ident):
            P.op('pe', lambda h: h.transpose(out.ap, in_.ap, ident.ap), reads=[in_, ident], writes=[out])

        wrr = [0]

        def wslot():
            s = wsl[wrr[0] % NWSLOT]
            wrr[0] += 1
            return s

        def load_w(dst_view, src_ap):
            P.dma('pool', lambda h: h.dma_start(out=dst_view.ap, in_=src_ap, max_dma_last_dim=8192), writes=[dst_view])

        def rstd_from_ssq(ssq, n, out):
            P.op('dve', lambda h: h.tensor_scalar(out=out.ap, in0=ssq.ap, scalar1=1.0 / n, scalar2=EPS,
                                                  op0=ALU.mult, op1=ALU.add), reads=[ssq], writes=[out])
            P.op('act', lambda h: h.activation(out=out.ap, in_=out.ap, func=AF.Sqrt), reads=[out], writes=[out])
            P.op('dve', lambda h: h.reciprocal(out=out.ap, in_=out.ap), reads=[out], writes=[out])

        def norm_tile(x_t, gain, ti, want_f32=False):
            ssq = stat.c(0, 1)
            rs = stat.c(1, 2)
            hsv = hs.c(0, D)
            xv = x_t.c(0, D)
            if STOP <= 0:
                return
            P.op('act', lambda h: h.activation(out=hsv.ap, in_=xv.ap, func=AF.Square, accum_out=ssq.ap),
                 reads=[xv], writes=[hsv, ssq])
            rstd_from_ssq(ssq, D, rs)
            P.op('dve', lambda h: h.tensor_scalar(out=hsv.ap, in0=xv.ap, scalar1=rs.ap, scalar2=None, op0=ALU.mult),
                 reads=[xv, rs], writes=[hsv])
            if STOP <= 0.3:
                return
            for b in range(4):
                for j in range(4):
                    kc = b * 4 + j
                    tr(pb[b].c(j * 128, (j + 1) * 128), hs.c(kc * 128, (kc + 1) * 128), cst_identf.c(0, 128))
            if STOP <= 0.6:
                return
            for b in range(4):
                for j in range(4):
                    kc = b * 4 + j
                    src = pb[b].c(j * 128, (j + 1) * 128)
                    evac_copy(hT.c(kc * 512 + ti * 128, kc * 512 + (ti + 1) * 128), src, gain.c(kc, kc + 1))
                    if want_f32:
                        evac_copy(hTf.c(kc * 128, (kc + 1) * 128), src, gain.c(kc, kc + 1))

        load('sp', cst_identf.c(0, 128), ident_f_d[:, :])
        load('sp', cst_identb.c(0, 128), ident_b_d[:, :])
        load('sp', cst_trin.c(0, 128), trin_d[:, :])
        load('sp', g_mix.c(0, KC), mixg[:, :])
        P.op('pool', lambda h: h.memset(glT.t[:, :], 1.0), writes=[glT.c(0, 512)])
        load_w(wgu_a.c(0, 512, 0, 16), wgu[:, :])
        load_w(wgu_a.c(0, 512, 16, 17), bgate[:, :])
        P.op('pool', lambda h: h.memset(bsum.t[:, :], 0.0), writes=[bsum.c(0, 4)])
        if phase == 1:
            P.op('pool', lambda h: h.memset(Sf.t[:, :], 0.0), writes=[Sf.c(0, 1024)])
            P.op('pool', lambda h: h.memset(Sb.t[:, :], 0.0), writes=[Sb.c(0, 1024)])
        else:
            load('sp', cst_mask4.c(0, 512), mask4_d[:, :])
            load('sp', cst_mcur.c(0, 512), mcur_d[:, :])
            load('sp', cst_mcur0.c(0, 512), mcur0_d[:, :])
            load('sp', cst_mprev.c(0, 512), mprev_d[:, :])
            load('sp', cmask.c(0, 8), cmask_d[:, :])
            load('sp', g_ffn.c(0, KC), ffng[:, :])
            load('sp', psc.c(0, 8), pscale[:, :])
            load('sp', gnb.c(0, 1024), gnorm.partition_broadcast(128))
            load('sp', uprev.c(0, 1024), uhalo[:, :])
            load_w(wpool.c(0, 2048).r("p (a b) -> p a b", b=256), w_pool.rearrange("(a p) d -> p a d", p=128))
            if moe:
                load('sp', wr.c(0, KC * NEXP).r("p (a b) -> p a b", b=NEXP), w_router.rearrange("(a p) e -> p a e", p=128))
            P.op('pool', lambda h: h.memset(Sf.t[:, :], 0.0), writes=[Sf.c(0, 1024)])
            Lb = A("otmp", f32=True)
            bj = stat.c(8, 12)
            aj = stat.c(12, 16)
            for j in range(NCORES):
                load('sp', Lb, lall[j * 128:(j + 1) * 128, :])
                load('sp', bj, ball[j * 128:(j + 1) * 128, :])
                mj = cmask.c(j, j + 1)
                P.op('dve', lambda h, mj=mj: h.tensor_scalar(out=aj.ap, in0=bj.ap, scalar1=mj.ap, scalar2=None, op0=ALU.mult),
                     reads=[bj, mj], writes=[aj])
                P.op('act', lambda h: h.activation(out=aj.ap, in_=aj.ap, func=AF.Exp), reads=[aj], writes=[aj])
                P.op('dve', lambda h, mj=mj: h.tensor_scalar(out=Lb.ap, in0=Lb.ap, scalar1=mj.ap, scalar2=None, op0=ALU.mult),
                     reads=[Lb, mj], writes=[Lb])
                for hh in range(4):
                    sv = Sf.c(hh * 256, (hh + 1) * 256)
                    lv = V(arena, Lb.ap[:, hh * 256:(hh + 1) * 256], Lb.lo, Lb.hi)
                    av = stat.c(12 + hh, 13 + hh)
                    P.op('dve', lambda h, sv=sv, lv=lv, av=av: h.scalar_tensor_tensor(
                        out=sv.ap, in0=sv.ap, scalar=av.ap, in1=lv.ap, op0=ALU.mult, op1=ALU.add),
                        reads=[sv, lv, av], writes=[sv])
            P.op('dve', lambda h: h.tensor_copy(out=Sb.t[:, :], in_=Sf.t[:, :]), reads=[Sf.c(0, 1024)], writes=[Sb.c(0, 1024)])

        out_sems = []
        zoff = AO["z"][0]

        def Z(ti, lo, hi):
            return arena.c(zoff + ti * 3072 + lo, zoff + ti * 3072 + hi)

        for st in range(NST):
            for ti in range(ST):
                g = st * ST + ti
                load('sp', xt[ti].c(0, D), x_in[g * 128:(g + 1) * 128, :])
                norm_tile(xt[ti], g_mix, ti)

            if STOP <= 1:
                continue
            blocks = [('u', 0, 512, 0), ('u', 512, 512, 512), ('q', 1024, 512, 0), ('k', 1536, 512, 0),
                      ('v', 2048, 512, 1024), ('v', 2560, 512, 1536), ('g', 3072, 16, 0),
                      ('r', 3088, 512, 2048), ('r', 3600, 512, 2560)]
            for kind, c0, ncol, zc in blocks:
                if phase == 1 and (kind in ('q', 'r') or (kind == 'u' and st != NST - 1)):
                    continue
                ws = wslot()
                wv = ws.c(0, KC * ncol)
                load_w(wv.r("p (a b) -> p a b", b=ncol), w_in[:, c0:c0 + ncol].rearrange("(a p) c -> p a c", p=128))
                if kind in ('u', 'v', 'r'):
                    for ti in range(ST):
                        bank = pb[ti % 4]
                        for kc in range(KC):
                            mm(bank.c(0, 512), hT.c(kc * 512 + ti * 128, kc * 512 + (ti + 1) * 128),
                               ws.c(kc * 512, (kc + 1) * 512), kc == 0, kc == KC - 1)
                        evac_copy(Z(ti, zc, zc + 512), bank.c(0, 512))
                elif kind in ('q', 'k'):
                    for hh in range(4):
                        bank = pb[hh]
                        for kc in range(KC):
                            mm(bank.c(0, 512), ws.c(kc * 512 + hh * 128, kc * 512 + (hh + 1) * 128),
                               hT.c(kc * 512, (kc + 1) * 512), kc == 0, kc == KC - 1)
                        dst = A("qT" if kind == 'q' else "kT").r("p (a b c) -> p a b c", a=ST, b=4).s(slice(None), slice(None), hh, slice(None))
                        evac_copy(dst, bank.c(0, 512).r("p (a c) -> p a c", a=ST))
                else:
                    bank = pb[4]
                    for kc in range(KC):
                        mm(bank.c(0, 512, 0, 16), ws.c(kc * 16, (kc + 1) * 16), hT.c(kc * 512, (kc + 1) * 512),
                           kc == 0, kc == KC - 1)
                    evac_copy(glT.c(0, 512, 0, 16), bank.c(0, 512, 0, 16))

            if STOP <= 2:
                continue
            for ti in range(ST):
                g = st * ST + ti
                kT_t = A("kT", ti * 512, (ti + 1) * 512)
                mm(pb[5].c(0, 512), glT.c(ti * 128, (ti + 1) * 128, 0, 17), wgu_a.c(0, 512, 0, 17), True, True)
                ysp = A("ysp", f32=True)
                spv = A("sp")
                P.op('act', lambda h, ysp=ysp: h.activation(out=ysp.ap, in_=pb[5].t[:, 0:512], func=AF.Exp, scale=-1.0),
                     reads=[pb[5].c(0, 512)], writes=[ysp])
                P.op('act', lambda h, ysp=ysp, spv=spv: h.activation(out=spv.ap, in_=ysp.ap, func=AF.Ln, bias=1.0),
                     reads=[ysp], writes=[spv])
                if STOP <= 2.2:
                    continue
                for hh in range(4):
                    mm(pb[6].c(hh * 128, (hh + 1) * 128), V(arena, spv.ap[:, hh * 128:(hh + 1) * 128], spv.lo, spv.hi),
                       cst_trin.c(0, 128), True, True)
                epos = A("epos", f32=True)
                eneg = A("eneg", f32=True)
                bcv = pb[6].c(0, 512)
                P.op('act', lambda h, epos=epos: h.activation(out=epos.ap, in_=pb[6].t[:, 0:512], func=AF.Exp),
                     reads=[bcv], writes=[epos])
                P.op('act', lambda h, eneg=eneg: h.activation(out=eneg.ap, in_=pb[6].t[:, 0:512], func=AF.Exp, scale=-1.0),
                     reads=[bcv], writes=[eneg])
                if STOP <= 2.3:
                    continue
                bl = V(pb[6], pb[6].t[:, 0:512].rearrange("p (a b) -> p a b", b=128)[:, :, 127], 0, 2048)
                P.op('dve', lambda h, bl=bl: h.tensor_tensor(out=bsum.t[:, :], in0=bl.ap, in1=bsum.t[:, :], op=ALU.add),
                     reads=[bsum.c(0, 4), bl], writes=[bsum.c(0, 4)])
                if STOP <= 2.4:
                    continue
                kd = A("kd")
                P.op('dve', lambda h, kd=kd, kT_t=kT_t, eneg=eneg: h.tensor_tensor(out=kd.ap, in0=kT_t.ap, in1=eneg.ap, op=ALU.mult),
                     reads=[kT_t, eneg], writes=[kd])
                if phase == 2:
                    qT_t = A("qT", ti * 512, (ti + 1) * 512)
                    qd = A("qd")
                    P.op('dve', lambda h, qd=qd, qT_t=qT_t, epos=epos: h.scalar_tensor_tensor(
                        out=qd.ap, in0=qT_t.ap, scalar=float(128 ** -0.5), in1=epos.ap, op0=ALU.mult, op1=ALU.mult),
                        reads=[qT_t, epos], writes=[qd])
                    for hh in range(4):
                        mm(pb[4].c(hh * 128, (hh + 1) * 128), V(arena, kd.ap[:, hh * 128:(hh + 1) * 128], kd.lo, kd.hi),
                           V(arena, qd.ap[:, hh * 128:(hh + 1) * 128], qd.lo, qd.hi), True, True)
                    att = A("att")
                    P.op('dve', lambda h, att=att: h.tensor_tensor(out=att.ap, in0=pb[4].t[:, 0:512], in1=cst_mask4.t[:, :], op=ALU.mult),
                         reads=[pb[4].c(0, 512), cst_mask4.c(0, 512)], writes=[att])
                    for hh in range(4):
                        ob = pb[hh // 2].c((hh % 2) * 256, (hh % 2) * 256 + 256)
                        mm(ob, V(arena, qd.ap[:, hh * 128:(hh + 1) * 128], qd.lo, qd.hi), Sb.c(hh * 256, (hh + 1) * 256), True, False)
                        mm(ob, V(arena, att.ap[:, hh * 128:(hh + 1) * 128], att.lo, att.hi), Z(ti, 1024 + hh * 256, 1024 + (hh + 1) * 256), False, True)
                kteT = A("kteT")
                for hh in range(4):
                    el = V(arena, epos.ap[:, hh * 128 + 127:hh * 128 + 128], epos.lo, epos.hi)
                    o_ = V(arena, kteT.ap[:, hh * 128:(hh + 1) * 128], kteT.lo, kteT.hi)
                    i_ = V(arena, kd.ap[:, hh * 128:(hh + 1) * 128], kd.lo, kd.hi)
                    P.op('dve', lambda h, o_=o_, i_=i_, el=el: h.tensor_scalar(out=o_.ap, in0=i_.ap, scalar1=el.ap, scalar2=None, op0=ALU.mult),
                         reads=[i_, el], writes=[o_])
                if STOP <= 2.6:
                    continue
                for hh in range(4):
                    tr(pbb.c(hh * 128, (hh + 1) * 128), V(arena, kteT.ap[:, hh * 128:(hh + 1) * 128], kteT.lo, kteT.hi), cst_identb.c(0, 128))
                kte = A("kte")
                P.op('act', lambda h, kte=kte: h.activation(out=kte.ap, in_=pbb.t[:, 0:512], func=AF.Copy),
                     reads=[pbb.c(0, 512)], writes=[kte])
                if STOP <= 2.8:
                    continue
                for hh in range(4):
                    cb = pb[2 + hh // 2].c((hh % 2) * 256, (hh % 2) * 256 + 256)
                    mm(cb, V(arena, kte.ap[:, hh * 128:(hh + 1) * 128], kte.lo, kte.hi), Z(ti, 1024 + hh * 256, 1024 + (hh + 1) * 256), True, True)
                for hh in range(4):
                    cb = pb[2 + hh // 2].c((hh % 2) * 256, (hh % 2) * 256 + 256)
                    el = V(arena, epos.ap[:, hh * 128 + 127:hh * 128 + 128], epos.lo, epos.hi)
                    sv = Sf.c(hh * 256, (hh + 1) * 256)
                    P.op('dve', lambda h, sv=sv, el=el: h.tensor_scalar(out=sv.ap, in0=sv.ap, scalar1=el.ap, scalar2=None, op0=ALU.mult),
                         reads=[sv, el], writes=[sv])
                    P.op('dve', lambda h, sv=sv, cb=cb: h.tensor_tensor(out=sv.ap, in0=cb.ap, in1=sv.ap, op=ALU.add),
                         reads=[sv, cb], writes=[sv])
                P.op('dve', lambda h: h.tensor_copy(out=Sb.t[:, :], in_=Sf.t[:, :]), reads=[Sf.c(0, 1024)], writes=[Sb.c(0, 1024)])

                if phase == 1:
                    continue

                otmp = A("otmp", f32=True)
                ssq4 = stat.c(16, 20)
                rs4 = stat.c(20, 24)
                for hh in range(4):
                    ob = pb[hh // 2].c((hh % 2) * 256, (hh % 2) * 256 + 256)
                    sq = stat.c(16 + hh, 17 + hh)
                    ot = V(arena, otmp.ap[:, hh * 256:(hh + 1) * 256], otmp.lo, otmp.hi)
                    P.op('act', lambda h, ob=ob, sq=sq, ot=ot: h.activation(out=ot.ap, in_=ob.ap, func=AF.Square, accum_out=sq.ap),
                         reads=[ob], writes=[ot, sq])
                rstd_from_ssq(ssq4, 256, rs4)
                for hh in range(4):
                    ob = pb[hh // 2].c((hh % 2) * 256, (hh % 2) * 256 + 256)
                    rv = stat.c(20 + hh, 21 + hh)
                    ot = V(arena, otmp.ap[:, hh * 256:(hh + 1) * 256], otmp.lo, otmp.hi)
                    gv = gnb.c(hh * 256, (hh + 1) * 256)
                    P.op('dve', lambda h, ob=ob, rv=rv, ot=ot, gv=gv: h.scalar_tensor_tensor(
                        out=ot.ap, in0=ob.ap, scalar=rv.ap, in1=gv.ap, op0=ALU.mult, op1=ALU.mult),
                        reads=[ob, rv, gv], writes=[ot])
                sr = A("sr")
                rz = Z(ti, 2048, 3072)
                P.op('act', lambda h, sr=sr, rz=rz: h.activation(out=sr.ap, in_=rz.ap, func=AF.Silu), reads=[rz], writes=[sr])
                og = A("og")
                P.op('dve', lambda h, og=og, otmp=otmp, sr=sr: h.tensor_tensor(out=og.ap, in0=otmp.ap, in1=sr.ap, op=ALU.mult),
                     reads=[otmp, sr], writes=[og])
                for c in range(8):
                    tr(pbb.c(c * 128, (c + 1) * 128), V(arena, og.ap[:, c * 128:(c + 1) * 128], og.lo, og.hi), cst_identb.c(0, 128))
                for c in range(8):
                    evac_copy(mixT.c((8 + c) * 512 + ti * 128, (8 + c) * 512 + (ti + 1) * 128), pbb.c(c * 128, (c + 1) * 128))

                ucur = Z(ti, 0, 1024)
                mc = cst_mcur0 if g == 0 else cst_mcur
                for c in range(8):
                    gi = c // 2
                    ob = pb[4 + c // 4].c((c % 4) * 128, (c % 4 + 1) * 128)
                    mm(ob, uprev.c(c * 128, (c + 1) * 128), cst_mprev.c(gi * 128, (gi + 1) * 128), True, False)
                    mm(ob, V(arena, ucur.ap[:, c * 128:(c + 1) * 128], ucur.lo, ucur.hi), mc.c(gi * 128, (gi + 1) * 128), False, True)
                dT = A("dT")
                evac_copy(V(arena, dT.ap[:, 0:512], dT.lo, dT.hi), pb[4].c(0, 512))
                evac_copy(V(arena, dT.ap[:, 512:1024], dT.lo, dT.hi), pb[5].c(0, 512))
                for oc in range(8):
                    gi, jj = oc // 2, oc % 2
                    ob = pb[4 + oc // 4].c((oc % 4) * 128, (oc % 4 + 1) * 128)
                    for ci in range(2):
                        wv = wpool.c((gi * 2 + ci) * 256 + jj * 128, (gi * 2 + ci) * 256 + (jj + 1) * 128)
                        mm(ob, wv, V(arena, dT.ap[:, (gi * 2 + ci) * 128:(gi * 2 + ci + 1) * 128], dT.lo, dT.hi), ci == 0, ci == 1)
                for oc in range(8):
                    ob = pb[4 + oc // 4].c((oc % 4) * 128, (oc % 4 + 1) * 128)
                    evac_copy(mixT.c(oc * 512 + ti * 128, oc * 512 + (ti + 1) * 128), ob, psc.c(oc, oc + 1))
                P.op('pool', lambda h, ucur=ucur: h.tensor_copy(out=uprev.t[:, :], in_=ucur.ap), reads=[ucur], writes=[uprev.c(0, 1024)])

            if phase == 1:
                continue

            for cb in range(4):
                ws = wslot()
                load_w(ws.c(0, KC * 512).r("p (a b) -> p a b", b=512), w_out[:, cb * 512:(cb + 1) * 512].rearrange("(a p) c -> p a c", p=128))
                for ti in range(ST):
                    bank = pb[ti]
                    for kc in range(KC):
                        mm(bank.c(0, 512), mixT.c(kc * 512 + ti * 128, kc * 512 + (ti + 1) * 128), ws.c(kc * 512, (kc + 1) * 512),
                           kc == 0, kc == KC - 1)
                    xv = xt[ti].c(cb * 512, (cb + 1) * 512)
                    P.op('dve', lambda h, xv=xv, bank=bank: h.tensor_tensor(out=xv.ap, in0=bank.t[:, 0:512], in1=xv.ap, op=ALU.add),
                         reads=[bank.c(0, 512), xv], writes=[xv])

            for ti in range(ST):
                norm_tile(xt[ti], g_ffn, ti, want_f32=moe)
                if moe:
                    lgp = pb[6].c(0, NEXP)
                    for kc in range(KC):
                        mm(lgp, hTf.c(kc * 128, (kc + 1) * 128), wr.c(kc * NEXP, (kc + 1) * NEXP), kc == 0, kc == KC - 1)
                    lg = rt.c(0, 8)
                    lg2 = rt.c(8, 16)
                    eq1 = rt.c(16, 24)
                    eq2 = rt.c(24, 32)
                    m1 = rt.c(32, 33)
                    m2 = rt.c(33, 34)
                    ex = rt.c(34, 35)
                    w1 = rt.c(35, 36)
                    w2 = rt.c(36, 37)
                    wv = wgt.c(ti * NEXP, (ti + 1) * NEXP)
                    P.op('dve', lambda h: h.tensor_copy(out=lg.ap, in_=lgp.ap), reads=[lgp], writes=[lg])
                    P.op('dve', lambda h: h.tensor_reduce(out=m1.ap, in_=lg.ap, axis=AX.X, op=ALU.max), reads=[lg], writes=[m1])
                    P.op('dve', lambda h: h.tensor_scalar(out=eq1.ap, in0=lg.ap, scalar1=m1.ap, scalar2=None, op0=ALU.is_equal),
                         reads=[lg, m1], writes=[eq1])
                    P.op('dve', lambda h: h.scalar_tensor_tensor(out=lg2.ap, in0=eq1.ap, scalar=-1e30, in1=lg.ap, op0=ALU.mult, op1=ALU.add),
                         reads=[eq1, lg], writes=[lg2])
                    P.op('dve', lambda h: h.tensor_reduce(out=m2.ap, in_=lg2.ap, axis=AX.X, op=ALU.max), reads=[lg2], writes=[m2])
                    P.op('dve', lambda h: h.tensor_scalar(out=eq2.ap, in0=lg2.ap, scalar1=m2.ap, scalar2=None, op0=ALU.is_equal),
                         reads=[lg2, m2], writes=[eq2])
                    P.op('dve', lambda h: h.tensor_tensor(out=ex.ap, in0=m2.ap, in1=m1.ap, op=ALU.subtract), reads=[m1, m2], writes=[ex])
                    P.op('act', lambda h: h.activation(out=ex.ap, in_=ex.ap, func=AF.Exp), reads=[ex], writes=[ex])
                    P.op('dve', lambda h: h.tensor_scalar(out=w1.ap, in0=ex.ap, scalar1=1.0, scalar2=None, op0=ALU.add), reads=[ex], writes=[w1])
                    P.op('dve', lambda h: h.reciprocal(out=w1.ap, in_=w1.ap), reads=[w1], writes=[w1])
                    P.op('dve', lambda h: h.tensor_tensor(out=w2.ap, in0=ex.ap, in1=w1.ap, op=ALU.mult), reads=[ex, w1], writes=[w2])
                    P.op('dve', lambda h, wv=wv: h.tensor_scalar(out=wv.ap, in0=eq1.ap, scalar1=w1.ap, scalar2=None, op0=ALU.mult),
                         reads=[eq1, w1], writes=[wv])
                    P.op('dve', lambda h, wv=wv: h.scalar_tensor_tensor(out=wv.ap, in0=eq2.ap, scalar=w2.ap, in1=wv.ap, op0=ALU.mult, op1=ALU.add),
                         reads=[eq2, w2, wv], writes=[wv])

            for e in range(NE):
                for half in range(2):
                    f0 = half * HALF
                    for fb in range(HALF // 2):
                        ws = wslot()
                        c0 = (f0 + fb * 2) * 128
                        gv = ws.c(0, KC * 256)
                        uv = ws.c(KC * 256, 2 * KC * 256)
                        load_w(gv.r("p (a b) -> p a b", b=256), wg_d[e * D:(e + 1) * D, c0:c0 + 256].rearrange("(a p) c -> p a c", p=128))
                        load_w(uv.r("p (a b) -> p a b", b=256), wu_d[e * D:(e + 1) * D, c0:c0 + 256].rearrange("(a p) c -> p a c", p=128))
                        for j in range(2):
                            fc = fb * 2 + j
                            gb_ = pb[(fc % 2) * 2]
                            ub_ = pb[(fc % 2) * 2 + 1]
                            for kc in range(KC):
                                mm(gb_.c(0, 512), ws.c(kc * 256 + j * 128, kc * 256 + (j + 1) * 128), hT.c(kc * 512, (kc + 1) * 512),
                                   kc == 0, kc == KC - 1)
                            for kc in range(KC):
                                mm(ub_.c(0, 512), ws.c(KC * 256 + kc * 256 + j * 128, KC * 256 + kc * 256 + (j + 1) * 128),
                                   hT.c(kc * 512, (kc + 1) * 512), kc == 0, kc == KC - 1)
                            sg = hs.c((fc % 2) * 512, (fc % 2) * 512 + 512)
                            P.op('act', lambda h, sg=sg, gb_=gb_: h.activation(out=sg.ap, in_=gb_.t[:, 0:512], func=AF.Silu),
                                 reads=[gb_.c(0, 512)], writes=[sg])
                            av = ACT_(fc)
                            P.op('dve', lambda h, av=av, sg=sg, ub_=ub_: h.tensor_tensor(out=av.ap, in0=ub_.t[:, 0:512], in1=sg.ap, op=ALU.mult),
                                 reads=[ub_.c(0, 512), sg], writes=[av])
                    PIECE = 11 if not moe else 14
                    for cb in range(4):
                        for pi in range(HALF // PIECE):
                            ws = wslot()
                            r0 = e * FF + (f0 + pi * PIECE) * 128
                            load_w(ws.c(0, PIECE * 512).r("p (a b) -> p a b", b=512),
                                   wd_d[r0:r0 + PIECE * 128, cb * 512:(cb + 1) * 512].rearrange("(a p) c -> p a c", p=128))
                            for ti in range(ST):
                                bank = pb[3 + ti]
                                for q in range(PIECE):
                                    fc = pi * PIECE + q
                                    mm(bank.c(0, 512), ACT_(fc, ti * 128, (ti + 1) * 128), ws.c(q * 512, (q + 1) * 512),
                                       fc == 0, fc == HALF - 1)
                        for ti in range(ST):
                            bank = pb[3 + ti]
                            xv = xt[ti].c(cb * 512, (cb + 1) * 512)
                            if moe:
                                wv = wgt.c(ti * NEXP + e, ti * NEXP + e + 1)
                                P.op('dve', lambda h, xv=xv, bank=bank, wv=wv: h.scalar_tensor_tensor(
                                    out=xv.ap, in0=bank.t[:, 0:512], scalar=wv.ap, in1=xv.ap, op0=ALU.mult, op1=ALU.add),
                                    reads=[bank.c(0, 512), wv, xv], writes=[xv])
                            else:
                                P.op('dve', lambda h, xv=xv, bank=bank: h.tensor_tensor(out=xv.ap, in0=bank.t[:, 0:512], in1=xv.ap, op=ALU.add),
                                     reads=[bank.c(0, 512), xv], writes=[xv])

            if final:
                load('sp', fgb.c(0, D), fing.partition_broadcast(128))
            for ti in range(ST):
                g = st * ST + ti
                xv = xt[ti].c(0, D)
                if final:
                    ssq = stat.c(0, 1)
                    rs = stat.c(1, 2)
                    hsv = hs.c(0, D)
                    P.op('act', lambda h, xv=xv: h.activation(out=hsv.ap, in_=xv.ap, func=AF.Square, accum_out=ssq.ap),
                         reads=[xv], writes=[hsv, ssq])
                    rstd_from_ssq(ssq, D, rs)
                    P.op('dve', lambda h, xv=xv: h.scalar_tensor_tensor(out=xv.ap, in0=xv.ap, scalar=rs.ap, in1=fgb.t[:, :], op0=ALU.mult, op1=ALU.mult),
                         reads=[xv, rs, fgb.c(0, D)], writes=[xv])
                out_sems.append(P.dma('sp', lambda h, xv=xv, g=g: h.dma_start(out=x_out[g * 128:(g + 1) * 128, :], in_=xv.ap), reads=[xv]))

        if phase == 1:
            out_sems.append(P.dma('sp', lambda h: h.dma_start(out=lst_o[:, :], in_=Sf.t[:, :]), reads=[Sf.c(0, 1024)]))
            out_sems.append(P.dma('sp', lambda h: h.dma_start(out=bsum_o[:, :], in_=bsum.t[:, :]), reads=[bsum.c(0, 4)]))
            lastu = Z(ST - 1, 0, 1024)
            out_sems.append(P.dma('sp', lambda h: h.dma_start(out=ulast_o[:, :], in_=lastu.ap), reads=[lastu]))
        fin = {}
        for s, v in out_sems:
            fin[id(s)] = (s, max(v, fin.get(id(s), (s, 0))[1]))
        P.wait_all('sp', list(fin.values()))

        with nc.Block() as block:
            @block.tensor
            def _(h):
                P.emit_engine('pe', h)

            @block.scalar
            def _(h):
                P.emit_engine('act', h)

            @block.vector
            def _(h):
                P.emit_engine('dve', h)

            @block.gpsimd
            def _(h):
                P.emit_engine('pool', h)

            @block.sync
            def _(h):
                P.emit_engine('sp', h)
    return nc


_PROG_CACHE = {}


def _get(kind, phase, final):
    key = (kind, phase, final)
    if key not in _PROG_CACHE:
        _PROG_CACHE[key] = build(kind, phase, final)
    return _PROG_CACHE[key]


def kernel(x, mix_norm, w_in, w_pool, pool_scale, w_gate_up, b_gate, gla_norm, w_out,
           ffn_norm, dense_w_gate, dense_w_up, dense_w_down, w_router,
           exp_w_gate, exp_w_up, exp_w_down, final_norm, _nlayers=2):
    f = lambda a: np.ascontiguousarray(np.asarray(a, dtype=np.float32))
    x = f(x).reshape(SEQ, D)
    consts = [_consts(c) for c in range(NCORES)]
    xs = [np.ascontiguousarray(x[c * TPC:(c + 1) * TPC]) for c in range(NCORES)]
    bf = ml_dtypes.bfloat16
    for l in range(_nlayers):
        kind = 'dense' if l % 2 == 0 else 'moe'
        common = {
            "mixg": _fm(f(mix_norm[l])), "w_in": f(w_in[l]), "wgu": f(w_gate_up[l]),
            "bgate": f(b_gate[l]).reshape(1, 512),
        }
        nc1 = _get(kind, 1, False)
        maps = []
        for c in range(NCORES):
            m = dict(common)
            m["x_in"] = xs[c]
            for k in ("ident_f", "ident_b", "trin"):
                m[k] = consts[c][k]
            maps.append(m)
        r1 = run_bass_kernel_spmd(nc1, maps, core_ids=list(range(NCORES))).results
        lall = np.concatenate([np.asarray(r["lst"]) for r in r1], axis=0)
        ball = np.concatenate([np.asarray(r["bsum"]) for r in r1], axis=0)
        final = l == 1
        nc2 = _get(kind, 2, final)
        maps = []
        for c in range(NCORES):
            m = dict(common)
            m["x_in"] = xs[c]
            m.update(consts[c])
            m["lall"] = lall
            m["ball"] = ball
            m["uhalo"] = np.asarray(r1[c - 1]["ulast"]) if c > 0 else np.zeros((128, 1024), bf)
            m["w_pool"] = f(w_pool[l]).reshape(4 * 256, 256)
            m["pscale"] = _fm(f(pool_scale[l]).reshape(-1))
            m["gnorm"] = f(gla_norm[l]).reshape(1, 1024)
            m["w_out"] = f(w_out[l])
            m["ffng"] = _fm(f(ffn_norm[l]))
            if kind == 'moe':
                i = l // 2
                m["w_router"] = f(w_router[i])
                m["wg"] = f(exp_w_gate[i]).reshape(NEXP * D, FF_EXP)
                m["wu"] = f(exp_w_up[i]).reshape(NEXP * D, FF_EXP)
                m["wd"] = f(exp_w_down[i]).reshape(NEXP * FF_EXP, D)
            else:
                i = l // 2
                m["wg"] = f(dense_w_gate[i])
                m["wu"] = f(dense_w_up[i])
                m["wd"] = f(dense_w_down[i])
            if final:
                m["fing"] = f(final_norm).reshape(1, D)
            maps.append(m)
        r2 = run_bass_kernel_spmd(nc2, maps, core_ids=list(range(NCORES))).results
        xs = [np.asarray(r["x_out"]) for r in r2]
    out = np.concatenate(xs, axis=0).reshape(1, SEQ, D).astype(np.float32)
    return out
```

```python
import numpy as np
import ml_dtypes
import concourse.bass as bass
import concourse.mybir as mybir
from concourse.bass_utils import run_bass_kernel_spmd

F32 = mybir.dt.float32
BF16 = mybir.dt.bfloat16
AF = mybir.ActivationFunctionType
ALU = mybir.AluOpType
AX = mybir.AxisListType

NCORES = 8
D = 2048
SEQ = 16384
TPC = SEQ // NCORES
NT = TPC // 128
ST = 4
NST = NT // ST
KC = D // 128
INW = 4112
FF_DENSE = 5632
FF_EXP = 7168
NEXP = 8
EPS = 1e-6
POOL_WINDOWS = (2, 4, 8, 16)
WSLOT = 8192
NWSLOT = 3
STOP = 99
EVAC_ACT_SCALE = False


class TT:
    def __init__(self, t, esz, name, holder=None, base=0):
        self.t = t
        self.esz = esz
        self.name = name
        self.H = holder if holder is not None else [[]]
        self.base = base
        self.whole = False

    @property
    def hist(self):
        return self.H[0]

    @hist.setter
    def hist(self, v):
        self.H[0] = v

    def c(self, lo, hi, p0=0, p1=None):
        ap = self.t[p0:p1, lo:hi] if p1 is not None else (self.t[p0:, lo:hi] if p0 else self.t[:, lo:hi])
        if self.whole:
            return V(self, ap, 0, 1 << 20)
        return V(self, ap, self.base + lo * self.esz, self.base + hi * self.esz)

    def view(self, lo_bytes, n, dt, name=None):
        esz = 4 if dt == F32 else 2
        a = lo_bytes // self.esz
        b = a + (n * esz) // self.esz
        ap = self.t[:, a:b]
        if esz != self.esz or dt != getattr(self, "dt", None):
            ap = ap.bitcast(dt)
        tt = TT(ap, esz, name or self.name, holder=self.H, base=self.base + lo_bytes)
        tt.dt = dt
        return tt


class V:
    def __init__(self, tt, ap, lo, hi):
        self.tt = tt
        self.ap = ap
        self.lo = lo
        self.hi = hi

    def r(self, pat, **kw):
        return V(self.tt, self.ap.rearrange(pat, **kw), self.lo, self.hi)

    def s(self, *key):
        return V(self.tt, self.ap[key], self.lo, self.hi)


class Eng:
    def __init__(self, key, sem):
        self.key = key
        self.sem = sem
        self.count = 0
        self.known = {}
        self.ops = []
        self.lanes = []
        self.lane_val = []
        self.lane_next = 0


class Prog:
    def __init__(self, nc):
        self.nc = nc
        self.E = {}
        self.sems = {}

    def add_engine(self, key, sem, lanes=()):
        e = Eng(key, sem)
        self.sems[id(sem)] = sem
        e.lanes = list(lanes)
        e.lane_val = [0] * len(lanes)
        for s in lanes:
            self.sems[id(s)] = s
        self.E[key] = e

    def _need(self, e, reads, writes):
        need = {}

        def add(h):
            if h[2] == 'pe' and e.key == 'pe' and h[3] == id(e.sem):
                return
            if need.get(h[3], 0) < h[4]:
                need[h[3]] = h[4]
        for v in reads:
            whole = v.tt.whole
            for h in v.tt.hist:
                if (h[5] or (whole and h[2] != e.key)) and h[0] < v.hi and v.lo < h[1]:
                    add(h)
        for v in writes:
            for h in v.tt.hist:
                if h[0] < v.hi and v.lo < h[1]:
                    add(h)
        waits = []
        for sid, val in need.items():
            if e.known.get(sid, 0) < val:
                e.known[sid] = val
                waits.append((self.sems[sid], val))
        return waits

    def _record(self, e, reads, writes, sid, val):
        for v in writes:
            hist = v.tt.hist
            v.tt.hist = [h for h in hist if not (h[0] >= v.lo and h[1] <= v.hi)]
            v.tt.hist.append([v.lo, v.hi, e.key, sid, val, True])
        for v in reads:
            hist = v.tt.hist
            v.tt.hist = [h for h in hist if not ((not h[5]) and h[3] == sid and h[0] >= v.lo and h[1] <= v.hi)]
            v.tt.hist.append([v.lo, v.hi, e.key, sid, val, False])

    def op(self, ek, fn, reads=(), writes=(), sig=True):
        e = self.E[ek]
        waits = self._need(e, reads, writes)
        val = e.count + 1
        if sig:
            e.count = val
        e.ops.append((waits, fn, (e.sem, 1) if sig else None))
        self._record(e, reads, writes, id(e.sem), val)

    def dma(self, ek, fn, reads=(), writes=()):
        e = self.E[ek]
        li = e.lane_next
        e.lane_next = (li + 1) % len(e.lanes)
        sem = e.lanes[li]
        waits = self._need(e, reads, writes)
        prev = e.lane_val[li]
        if prev > 0 and e.known.get(id(sem), 0) < prev:
            e.known[id(sem)] = prev
            waits.append((sem, prev))
        val = prev + 16
        e.lane_val[li] = val
        e.ops.append((waits, fn, (sem, 16)))
        self._record(e, reads, writes, id(sem), val)
        return sem, val

    def wait_all(self, ek, items):
        e = self.E[ek]
        waits = [(s, v) for (s, v) in items]
        e.ops.append((waits, None, None))

    def emit_engine(self, ek, h):
        for waits, fn, inc in self.E[ek].ops:
            for s, v in waits:
                h.wait_ge(s, v)
            if fn is None:
                continue
            ins = fn(h)
            if inc is not None:
                ins.then_inc(inc[0], inc[1])


def _pool_mats(first_core):
    cur = np.zeros((4, 128, 128), np.float32)
    cur0 = np.zeros((4, 128, 128), np.float32)
    prev = np.zeros((4, 128, 128), np.float32)
    for g, w in enumerate(POOL_WINDOWS):
        for t in range(128):
            for s in range(t - w + 1, t + 1):
                if s >= 0:
                    cur[g, s, t] += 1.0 / w
                else:
                    prev[g, 128 + s, t] += 1.0 / w
            cur[g, t, t] -= 1.0
            cnt = min(t + 1, w)
            for s in range(max(0, t - w + 1), t + 1):
                cur0[g, s, t] += 1.0 / cnt
            cur0[g, t, t] -= 1.0
    return cur, (cur0 if first_core else cur), prev


def _consts(core):
    bf = ml_dtypes.bfloat16
    c = {}
    c["ident_f"] = np.eye(128, dtype=np.float32)
    c["ident_b"] = np.eye(128, dtype=np.float32).astype(bf)
    j = np.arange(128)[:, None]
    i = np.arange(128)[None, :]
    tri = (j <= i).astype(np.float32)
    c["trin"] = (tri * (-1.0 / 16.0)).astype(bf)
    c["mask4"] = np.tile(tri, (1, 4)).astype(bf)
    cur, cur0, prev = _pool_mats(core == 0)
    c["mcur"] = np.ascontiguousarray(cur.transpose(1, 0, 2).reshape(128, 512)).astype(bf)
    c["mcur0"] = np.ascontiguousarray(cur0.transpose(1, 0, 2).reshape(128, 512)).astype(bf)
    c["mprev"] = np.ascontiguousarray(prev.transpose(1, 0, 2).reshape(128, 512)).astype(bf)
    m = np.zeros((128, 8), np.float32)
    m[:, :core] = 1.0
    c["cmask"] = m
    c["iota"] = np.tile(np.arange(640, dtype=np.float32)[None, :], (128, 1))
    c["ustrict"] = (j < i).astype(np.float32).astype(bf)
    c["ones"] = np.ones((128, 128), np.float32).astype(bf)
    return c


def _fm(vec):
    return np.ascontiguousarray(vec.reshape(-1, 128).T)


def build(layer_kind, phase, final):
    nc = bass.Bass("TRN2", target_bir_lowering=False)
    moe = layer_kind == 'moe'
    FF = FF_EXP if moe else FF_DENSE
    NE = NEXP if moe else 1
    NFC = FF // 128
    HALF = NFC // 2

    def din(name, shape, dt=F32):
        return nc.dram_tensor(name, list(shape), dt, kind="ExternalInput").ap()

    def dout(name, shape, dt=F32):
        return nc.dram_tensor(name, list(shape), dt, kind="ExternalOutput").ap()

    x_in = din("x_in", [TPC, D])
    mixg = din("mixg", [128, KC])
    w_in = din("w_in", [D, INW])
    wgu = din("wgu", [16, 512])
    bgate = din("bgate", [1, 512])
    ident_f_d = din("ident_f", [128, 128])
    ident_b_d = din("ident_b", [128, 128], BF16)
    trin_d = din("trin", [128, 128], BF16)
    if phase == 1:
        lst_o = dout("lst", [128, 1024])
        bsum_o = dout("bsum", [128, 4])
        ulast_o = dout("ulast", [128, 1024], BF16)
    else:
        mask4_d = din("mask4", [128, 512], BF16)
        mcur_d = din("mcur", [128, 512], BF16)
        mcur0_d = din("mcur0", [128, 512], BF16)
        mprev_d = din("mprev", [128, 512], BF16)
        cmask_d = din("cmask", [128, 8])
        lall = din("lall", [NCORES * 128, 1024])
        ball = din("ball", [NCORES * 128, 4])
        uhalo = din("uhalo", [128, 1024], BF16)
        w_pool = din("w_pool", [4 * 256, 256])
        pscale = din("pscale", [128, 8])
        gnorm = din("gnorm", [1, 1024])
        w_out = din("w_out", [D, D])
        ffng = din("ffng", [128, KC])
        if moe:
            w_router = din("w_router", [D, NEXP])
            wg_d = din("wg", [NEXP * D, FF])
            wu_d = din("wu", [NEXP * D, FF])
            wd_d = din("wd", [NEXP * FF, D])
        else:
            wg_d = din("wg", [D, FF])
            wu_d = din("wu", [D, FF])
            wd_d = din("wd", [FF, D])
        if final:
            fing = din("fing", [1, D])
        x_out = dout("x_out", [TPC, D])
        if moe:
            iota_d = din("iota", [128, 640])
            ustrict_d = din("ustrict", [128, 128], BF16)
            ones_d = din("ones", [128, 128], BF16)
            h2s = nc.dram_tensor("h2s", [TPC, D], BF16, kind="Internal").ap()

    import contextlib
    es = contextlib.ExitStack()
    with es:
        def sb(name, n, dt=F32, p=128):
            t = es.enter_context(nc.sbuf_tensor("sb_" + name, [p, n], dt))
            tt = TT(t, 4 if dt == F32 else 2, name)
            tt.dt = dt
            return tt

        def ps(name, n, dt=F32):
            t = es.enter_context(nc.psum_tensor("ps_" + name, [128, n], dt))
            tt = TT(t, 4 if dt == F32 else 2, name)
            tt.whole = True
            return tt

        def sem(name):
            return es.enter_context(nc.semaphore(name))

        P = Prog(nc)
        P.add_engine('pe', sem("s_pe"))
        P.add_engine('act', sem("s_act"))
        P.add_engine('dve', sem("s_dve"))
        P.add_engine('pool', sem("s_pool"), [sem("lp%d" % i) for i in range(16)])
        P.add_engine('sp', sem("s_sp"), [sem("ls%d" % i) for i in range(16)])

        xtall = sb("xtall", ST * D)
        xt = [xtall.view(i * D * 4, D, F32, "xt%d" % i) for i in range(ST)]
        hsf = sb("hsf", 2 * D)
        hs = hsf.view(0, D, F32, "hs")
        mixc = sb("mixc", 5 * 1024)
        hT = sb("hT", KC * 512, BF16)
        wsl = [sb("wsl%d" % i, WSLOT, BF16) for i in range(NWSLOT)]
        arena = sb("arena", 27 * 1024, BF16)
        mixT = hT
        stat = sb("stat", 64)
        cst_identf = sb("identf", 128)
        cst_identb = sb("identb", 128, BF16)
        cst_trin = sb("trin", 128, BF16)
        g_mix = sb("g_mix", KC)
        wgu_a = sb("wgu_a", 512, BF16, p=32)
        Sf = mixc.view(0, 1024, F32, "Sf")
        Sb = mixc.view(4096, 1024, BF16, "Sb")
        glT = sb("glT", 512, BF16, p=32)
        if phase == 2:
            cst_mask4 = sb("mask4", 512, BF16)
            cst_mcur = sb("mcur", 512, BF16)
            cst_mcur0 = sb("mcur0", 512, BF16)
            cst_mprev = sb("mprev", 512, BF16)
            cmask = sb("cmask", 8)
            g_ffn = sb("g_ffn", KC)
            psc = sb("psc", 8)
            gnb = mixc.view(6144, 1024, F32, "gnb")
            wpool = mixc.view(10240, 8 * 256, BF16, "wpool")
            uprev = mixc.view(14336, 1024, BF16, "uprev")
            if moe:
                wr = sb("wr", KC * NEXP)
                hTf = hsf.view(D * 4, KC * 128, F32, "hTf")
                wgt_all = sb("wgt_all", NT * NEXP)
                mask_all = sb("mask_all", NT * NEXP)
                pos_all = sb("pos_all", NT * NEXP)
                carry = sb("carry", NEXP)
                maskb = sb("maskb", NEXP, BF16)
                cst_ustrict = sb("ustrict", 128, BF16)
                cst_ones = sb("ones", 128, BF16)
                rt = sb("rt", 64)
            if final:
                fgb = hTf if moe else sb("fgb", D)
        bsum = sb("bsum", 4)

        AO = {}
        off = 0

        def carve(name, n_bf16):
            nonlocal off
            AO[name] = (off, off + n_bf16)
            off += n_bf16
        carve("z", ST * 3072)
        carve("qT", ST * 512)
        carve("kT", ST * 512)
        carve("ysp", 1024)
        carve("sp", 512)
        carve("epos", 1024)
        carve("eneg", 1024)
        carve("qd", 512)
        carve("kd", 512)
        carve("kteT", 512)
        carve("kte", 512)
        carve("att", 512)
        carve("otmp", 2048)
        carve("sr", 1024)
        carve("og", 1024)
        carve("dT", 1024)
        assert off <= 27 * 1024, off

        def A(name, lo=0, hi=None, f32=False):
            a, b = AO[name]
            if f32:
                n = (b - a) // 2
                hi_ = n if hi is None else hi
                ap = arena.t[:, a:b].bitcast(F32)[:, lo:hi_]
                return V(arena, ap, a * 2 + lo * 4, a * 2 + hi_ * 4)
            hi_ = (b - a) if hi is None else hi
            return arena.c(a + lo, a + hi_)

        def ACT_(fc, lo=0, hi=512):
            base = fc * 512
            return arena.c(base + lo, base + hi)
        assert HALF * 512 <= 27 * 1024

        pb = [ps("pb%d" % i, 512) for i in range(7)]
        pbb = ps("pbb", 1024, BF16)

        def load(eng, dst, src_ap, **kw):
            P.dma(eng, lambda h: h.dma_start(out=dst.ap, in_=src_ap, **kw), writes=[dst])

        rr = [0]

        def evac_copy(dst, src, scale_ap=None):
            rr[0] ^= 1
            if rr[0] and (scale_ap is None or EVAC_ACT_SCALE):
                if scale_ap is None:
                    P.op('act', lambda h: h.activation(out=dst.ap, in_=src.ap, func=AF.Copy), reads=[src], writes=[dst])
                else:
                    P.op('act', lambda h: h.activation(out=dst.ap, in_=src.ap, func=AF.Identity, scale=scale_ap.ap),
                         reads=[src, scale_ap], writes=[dst])
            else:
                if scale_ap is None:
                    P.op('dve', lambda h: h.tensor_copy(out=dst.ap, in_=src.ap), reads=[src], writes=[dst])
                else:
                    P.op('dve', lambda h: h.tensor_scalar(out=dst.ap, in0=src.ap, scalar1=scale_ap.ap, scalar2=None,
                                                          op0=ALU.mult), reads=[src, scale_ap], writes=[dst])

        def mm(out, lhsT, rhs, start, stop):
            P.op('pe', lambda h: h.matmul(out.ap, lhsT=lhsT.ap, rhs=rhs.ap, start=start, stop=stop),
                 reads=[lhsT, rhs], writes=[out], sig=stop)

        def tr(out, in_, ident):
            P.op('pe', lambda h: h.transpose(out.ap, in_.ap, ident.ap), reads=[in_, ident], writes=[out])

        wrr = [0]

        def wslot():
            s = wsl[wrr[0] % NWSLOT]
            wrr[0] += 1
            return s

        def load_w(dst_view, src_ap):
            P.dma('pool', lambda h: h.dma_start(out=dst_view.ap, in_=src_ap, max_dma_last_dim=8192), writes=[dst_view])

        def rstd_from_ssq(ssq, n, out):
            P.op('dve', lambda h: h.tensor_scalar(out=out.ap, in0=ssq.ap, scalar1=1.0 / n, scalar2=EPS,
                                                  op0=ALU.mult, op1=ALU.add), reads=[ssq], writes=[out])
            P.op('act', lambda h: h.activation(out=out.ap, in_=out.ap, func=AF.Sqrt), reads=[out], writes=[out])
            P.op('dve', lambda h: h.reciprocal(out=out.ap, in_=out.ap), reads=[out], writes=[out])

        def norm_tile(x_t, gain, ti, want_f32=False):
            ssq = stat.c(0, 1)
            rs = stat.c(1, 2)
            hsv = hs.c(0, D)
            xv = x_t.c(0, D)
            if STOP <= 0:
                return
            P.op('act', lambda h: h.activation(out=hsv.ap, in_=xv.ap, func=AF.Square, accum_out=ssq.ap),
                 reads=[xv], writes=[hsv, ssq])
            rstd_from_ssq(ssq, D, rs)
            P.op('dve', lambda h: h.tensor_scalar(out=hsv.ap, in0=xv.ap, scalar1=rs.ap, scalar2=None, op0=ALU.mult),
                 reads=[xv, rs], writes=[hsv])
            if STOP <= 0.3:
                return
            for b in range(4):
                for j in range(4):
                    kc = b * 4 + j
                    tr(pb[b].c(j * 128, (j + 1) * 128), hs.c(kc * 128, (kc + 1) * 128), cst_identf.c(0, 128))
            if STOP <= 0.6:
                return
            for b in range(4):
                for j in range(4):
                    kc = b * 4 + j
                    src = pb[b].c(j * 128, (j + 1) * 128)
                    evac_copy(hT.c(kc * 512 + ti * 128, kc * 512 + (ti + 1) * 128), src, gain.c(kc, kc + 1))
                    if want_f32:
                        evac_copy(hTf.c(kc * 128, (kc + 1) * 128), src, gain.c(kc, kc + 1))

        CAP = 640
        if moe and phase == 2:
            XO = TT(x_out, 1, "x_out_d")
            H2S = TT(h2s, 1, "h2s_d")

        def xo_cells(tt, dg=None):
            if dg is None:
                return tt * 4, tt * 4 + 4
            return tt * 4 + dg, tt * 4 + dg + 1

        def moe_route_tile(ti, g):
            x_t = xt[ti]
            ssq = stat.c(0, 1)
            rs = stat.c(1, 2)
            hsv = hs.c(0, D)
            xv = x_t.c(0, D)
            P.op('act', lambda h: h.activation(out=hsv.ap, in_=xv.ap, func=AF.Square, accum_out=ssq.ap),
                 reads=[xv], writes=[hsv, ssq])
            rstd_from_ssq(ssq, D, rs)
            P.op('dve', lambda h: h.tensor_scalar(out=hsv.ap, in0=xv.ap, scalar1=rs.ap, scalar2=None, op0=ALU.mult),
                 reads=[xv, rs], writes=[hsv])
            P.dma('pool', lambda h: h.dma_start(out=h2s[g * 128:(g + 1) * 128, :], in_=hsv.ap, max_dma_last_dim=8192),
                  reads=[hsv], writes=[V(H2S, None, 0, 1)])
            lo, hi = xo_cells(g)
            P.dma('sp', lambda h: h.dma_start(out=x_out[g * 128:(g + 1) * 128, :], in_=xv.ap), reads=[xv], writes=[V(XO, None, lo, hi)])
            for b in range(4):
                for j in range(4):
                    kc = b * 4 + j
                    tr(pb[b].c(j * 128, (j + 1) * 128), hs.c(kc * 128, (kc + 1) * 128), cst_identf.c(0, 128))
            for b in range(4):
                for j in range(4):
                    kc = b * 4 + j
                    evac_copy(hTf.c(kc * 128, (kc + 1) * 128), pb[b].c(j * 128, (j + 1) * 128), g_ffn.c(kc, kc + 1))
            lgp = pb[6].c(0, NEXP)
            for kc in range(KC):
                mm(lgp, hTf.c(kc * 128, (kc + 1) * 128), wr.c(kc * NEXP, (kc + 1) * NEXP), kc == 0, kc == KC - 1)
            lg = rt.c(0, 8)
            lg2 = rt.c(8, 16)
            eq1 = rt.c(16, 24)
            eq2 = rt.c(24, 32)
            m1 = rt.c(32, 33)
            m2 = rt.c(33, 34)
            ex = rt.c(34, 35)
            w1 = rt.c(35, 36)
            w2 = rt.c(36, 37)
            wv = wgt_all.c(g * NEXP, (g + 1) * NEXP)
            mk = mask_all.c(g * NEXP, (g + 1) * NEXP)
            pv = pos_all.c(g * NEXP, (g + 1) * NEXP)
            P.op('dve', lambda h: h.tensor_copy(out=lg.ap, in_=lgp.ap), reads=[lgp], writes=[lg])
            P.op('dve', lambda h: h.tensor_reduce(out=m1.ap, in_=lg.ap, axis=AX.X, op=ALU.max), reads=[lg], writes=[m1])
            P.op('dve', lambda h: h.tensor_scalar(out=eq1.ap, in0=lg.ap, scalar1=m1.ap, scalar2=None, op0=ALU.is_equal),
                 reads=[lg, m1], writes=[eq1])
            P.op('dve', lambda h: h.scalar_tensor_tensor(out=lg2.ap, in0=eq1.ap, scalar=-1e30, in1=lg.ap, op0=ALU.mult, op1=ALU.add),
                 reads=[eq1, lg], writes=[lg2])
            P.op('dve', lambda h: h.tensor_reduce(out=m2.ap, in_=lg2.ap, axis=AX.X, op=ALU.max), reads=[lg2], writes=[m2])
            P.op('dve', lambda h: h.tensor_scalar(out=eq2.ap, in0=lg2.ap, scalar1=m2.ap, scalar2=None, op0=ALU.is_equal),
                 reads=[lg2, m2], writes=[eq2])
            P.op('dve', lambda h: h.tensor_tensor(out=ex.ap, in0=m2.ap, in1=m1.ap, op=ALU.subtract), reads=[m1, m2], writes=[ex])
            P.op('act', lambda h: h.activation(out=ex.ap, in_=ex.ap, func=AF.Exp), reads=[ex], writes=[ex])
            P.op('dve', lambda h: h.tensor_scalar(out=w1.ap, in0=ex.ap, scalar1=1.0, scalar2=None, op0=ALU.add), reads=[ex], writes=[w1])
            P.op('dve', lambda h: h.reciprocal(out=w1.ap, in_=w1.ap), reads=[w1], writes=[w1])
            P.op('dve', lambda h: h.tensor_tensor(out=w2.ap, in0=ex.ap, in1=w1.ap, op=ALU.mult), reads=[ex, w1], writes=[w2])
            P.op('dve', lambda h: h.tensor_scalar(out=wv.ap, in0=eq1.ap, scalar1=w1.ap, scalar2=None, op0=ALU.mult),
                 reads=[eq1, w1], writes=[wv])
            P.op('dve', lambda h: h.scalar_tensor_tensor(out=wv.ap, in0=eq2.ap, scalar=w2.ap, in1=wv.ap, op0=ALU.mult, op1=ALU.add),
                 reads=[eq2, w2, wv], writes=[wv])
            P.op('dve', lambda h: h.tensor_tensor(out=mk.ap, in0=eq1.ap, in1=eq2.ap, op=ALU.add), reads=[eq1, eq2], writes=[mk])
            mb = maskb.c(0, NEXP)
            P.op('dve', lambda h: h.tensor_copy(out=mb.ap, in_=mk.ap), reads=[mk], writes=[mb])
            mm(pb[5].c(0, NEXP), cst_ustrict.c(0, 128), mb, True, True)
            mm(pb[5].c(NEXP, 2 * NEXP), cst_ones.c(0, 128), mb, True, True)
            cv = carry.c(0, NEXP)
            P.op('dve', lambda h: h.tensor_tensor(out=pv.ap, in0=pb[5].t[:, 0:NEXP], in1=cv.ap, op=ALU.add),
                 reads=[pb[5].c(0, NEXP), cv], writes=[pv])
            P.op('dve', lambda h: h.tensor_tensor(out=cv.ap, in0=pb[5].t[:, NEXP:2 * NEXP], in1=cv.ap, op=ALU.add),
                 reads=[pb[5].c(0, NEXP), cv], writes=[cv])

        def moe_experts():
            NPART = 8
            PCH = NFC // NPART
            Pe = xtall.view(0, NT * CAP, BF16, "Pe")
            sg = xtall.view(NT * CAP * 2, CAP, F32, "sg")
            actq = xtall.view(NT * CAP * 2 + CAP * 4, PCH * CAP, BF16, "actq")
            assert NT * CAP * 2 + CAP * 4 + PCH * CAP * 2 <= ST * D * 4
            yacc = arena.view(0, 5 * D, F32, "yacc")
            PT = arena.view(5 * D * 4, CAP, BF16, "PT")
            contrib = [arena.view(5 * D * 4 + CAP * 2 + i * 2048, 512, F32, "contrib%d" % i) for i in range(4)]
            iota = arena.view(5 * D * 4 + CAP * 2 + 4 * 2048, CAP, F32, "iota")
            assert 5 * D * 4 + CAP * 2 + 4 * 2048 + CAP * 4 <= 27 * 1024 * 2
            hTe = mixc.view(0, KC * CAP, BF16, "hTe")
            yb = mixc.view(0, 5 * D, BF16, "yb")
            h2c = [hT.view(0, NT * 512, BF16, "h2c0"), hsf.view(0, NT * 512, BF16, "h2c1")]
            load('sp', iota.c(0, CAP), iota_d[:, :])
            for e in range(NEXP):
                for tt in range(NT):
                    pv = pos_all.c(tt * NEXP + e, tt * NEXP + e + 1)
                    mk = mask_all.c(tt * NEXP + e, tt * NEXP + e + 1)
                    pe_ = Pe.c(tt * CAP, (tt + 1) * CAP)
                    P.op('dve', lambda h, pe_=pe_, pv=pv, mk=mk: h.tensor_scalar(
                        out=pe_.ap, in0=iota.t[:, :], scalar1=pv.ap, scalar2=mk.ap, op0=ALU.is_equal, op1=ALU.mult),
                        reads=[iota.c(0, CAP), pv, mk], writes=[pe_])
                for kg in range(4):
                    hc = h2c[kg % 2]
                    hcv = hc.c(0, NT * 512)
                    P.dma('sp', lambda h, hcv=hcv, kg=kg: h.dma_start(
                        out=hcv.ap.rearrange("p (a b) -> p a b", b=512),
                        in_=h2s[:, kg * 512:(kg + 1) * 512].rearrange("(a p) c -> p a c", p=128)),
                        reads=[V(H2S, None, 0, 1)], writes=[hcv])
                    for k4 in range(4):
                        kc = kg * 4 + k4
                        for cg, (c0, c1) in enumerate(((0, 512), (512, CAP))):
                            bank = pb[(kc * 2 + cg) % 4]
                            for tt in range(NT):
                                mm(bank.c(0, c1 - c0), hc.c(tt * 512 + k4 * 128, tt * 512 + (k4 + 1) * 128),
                                   Pe.c(tt * CAP + c0, tt * CAP + c1), tt == 0, tt == NT - 1)
                            evac_copy(hTe.c(kc * CAP + c0, kc * CAP + c1), bank.c(0, c1 - c0), g_ffn.c(kc, kc + 1))
                for qi in range(NPART):
                    f0 = qi * PCH
                    fl = 0
                    for nb in (2, 2, 2, 1):
                        ws = wslot()
                        c0 = (f0 + fl) * 128
                        gv = ws.c(0, KC * nb * 128)
                        uv = ws.c(KC * 256, KC * 256 + KC * nb * 128)
                        load_w(gv.r("p (a b) -> p a b", b=nb * 128), wg_d[e * D:(e + 1) * D, c0:c0 + nb * 128].rearrange("(a p) c -> p a c", p=128))
                        load_w(uv.r("p (a b) -> p a b", b=nb * 128), wu_d[e * D:(e + 1) * D, c0:c0 + nb * 128].rearrange("(a p) c -> p a c", p=128))
                        for j in range(nb):
                            fc = fl + j
                            par = fc % 2
                            g0, u0, xb = pb[3 * par], pb[3 * par + 1], pb[3 * par + 2]
                            wgs = lambda kc: ws.c(kc * nb * 128 + j * 128, kc * nb * 128 + (j + 1) * 128)
                            wus = lambda kc: ws.c(KC * 256 + kc * nb * 128 + j * 128, KC * 256 + kc * nb * 128 + (j + 1) * 128)
                            for kc in range(KC):
                                mm(g0.c(0, 512), wgs(kc), hTe.c(kc * CAP, kc * CAP + 512), kc == 0, kc == KC - 1)
                            for kc in range(KC):
                                mm(xb.c(0, 128), wgs(kc), hTe.c(kc * CAP + 512, (kc + 1) * CAP), kc == 0, kc == KC - 1)
                            for kc in range(KC):
                                mm(u0.c(0, 512), wus(kc), hTe.c(kc * CAP, kc * CAP + 512), kc == 0, kc == KC - 1)
                            for kc in range(KC):
                                mm(xb.c(128, 256), wus(kc), hTe.c(kc * CAP + 512, (kc + 1) * CAP), kc == 0, kc == KC - 1)
                            s0, s1 = sg.c(0, 512), sg.c(512, CAP)
                            P.op('act', lambda h, s0=s0, g0=g0: h.activation(out=s0.ap, in_=g0.t[:, 0:512], func=AF.Silu),
                                 reads=[g0.c(0, 512)], writes=[s0])
                            P.op('act', lambda h, s1=s1, xb=xb: h.activation(out=s1.ap, in_=xb.t[:, 0:128], func=AF.Silu),
                                 reads=[xb.c(0, 128)], writes=[s1])
                            a0 = actq.c(fc * CAP, fc * CAP + 512)
                            a1 = actq.c(fc * CAP + 512, (fc + 1) * CAP)
                            P.op('dve', lambda h, a0=a0, s0=s0, u0=u0: h.tensor_tensor(out=a0.ap, in0=u0.t[:, 0:512], in1=s0.ap, op=ALU.mult),
                                 reads=[u0.c(0, 512), s0], writes=[a0])
                            P.op('dve', lambda h, a1=a1, s1=s1, xb=xb: h.tensor_tensor(out=a1.ap, in0=xb.t[:, 128:256], in1=s1.ap, op=ALU.mult),
                                 reads=[xb.c(0, 256), s1], writes=[a1])
                        fl += nb
                    for dg in range(4):
                        ws = wslot()
                        r0 = e * FF + f0 * 128
                        load_w(ws.c(0, PCH * 512).r("p (a b) -> p a b", b=512),
                               wd_d[r0:r0 + PCH * 128, dg * 512:(dg + 1) * 512].rearrange("(a p) c -> p a c", p=128))
                        for s5 in range(5):
                            bank = pb[4 + (dg * 5 + s5) % 3]
                            for q in range(PCH):
                                mm(bank.c(0, 512), actq.c(q * CAP + s5 * 128, q * CAP + (s5 + 1) * 128), ws.c(q * 512, (q + 1) * 512),
                                   q == 0, q == PCH - 1)
                            yv = yacc.c(s5 * D + dg * 512, s5 * D + (dg + 1) * 512)
                            if qi == 0:
                                evac_copy(yv, bank.c(0, 512))
                            elif qi < NPART - 1:
                                P.op('dve', lambda h, yv=yv, bank=bank: h.tensor_tensor(out=yv.ap, in0=bank.t[:, 0:512], in1=yv.ap, op=ALU.add),
                                     reads=[bank.c(0, 512), yv], writes=[yv])
                            else:
                                ybv = yb.c(s5 * D + dg * 512, s5 * D + (dg + 1) * 512)
                                P.op('dve', lambda h, yv=yv, ybv=ybv, bank=bank: h.tensor_tensor(out=ybv.ap, in0=bank.t[:, 0:512], in1=yv.ap, op=ALU.add),
                                     reads=[bank.c(0, 512), yv], writes=[ybv])
                for tt in range(NT):
                    for s5 in range(5):
                        tr(pbb.c(s5 * 128, (s5 + 1) * 128), Pe.c(tt * CAP + s5 * 128, tt * CAP + (s5 + 1) * 128), cst_identb.c(0, 128))
                    ptv = PT.c(0, CAP)
                    P.op('act', lambda h, ptv=ptv: h.activation(out=ptv.ap, in_=pbb.t[:, 0:CAP], func=AF.Copy),
                         reads=[pbb.c(0, CAP)], writes=[ptv])
                    wv = wgt_all.c(tt * NEXP + e, tt * NEXP + e + 1)
                    for dg in range(4):
                        bank = pb[dg]
                        for s5 in range(5):
                            mm(bank.c(0, 512), PT.c(s5 * 128, (s5 + 1) * 128), yb.c(s5 * D + dg * 512, s5 * D + (dg + 1) * 512),
                               s5 == 0, s5 == 4)
                        cvw = contrib[dg].c(0, 512)
                        P.op('dve', lambda h, cvw=cvw, bank=bank, wv=wv: h.tensor_scalar(out=cvw.ap, in0=bank.t[:, 0:512], scalar1=wv.ap, scalar2=None, op0=ALU.mult),
                             reads=[bank.c(0, 512), wv], writes=[cvw])
                        lo, hi = xo_cells(tt, dg)
                        P.dma('pool', lambda h, cvw=cvw, tt=tt, dg=dg: h.dma_start(
                            out=x_out[tt * 128:(tt + 1) * 128, dg * 512:(dg + 1) * 512], in_=cvw.ap, accum_op=ALU.add),
                            reads=[cvw], writes=[V(XO, None, lo, hi)])

        def moe_final():
            load('sp', fgb.c(0, D), fing.partition_broadcast(128))
            for tt in range(NT):
                xv = xt[tt % ST].c(0, D)
                lo, hi = xo_cells(tt)
                P.dma('sp', lambda h, xv=xv, tt=tt: h.dma_start(out=xv.ap, in_=x_out[tt * 128:(tt + 1) * 128, :]),
                      reads=[V(XO, None, lo, hi)], writes=[xv])
                ssq = stat.c(0, 1)
                rs = stat.c(1, 2)
                hsv = A("otmp", f32=True)
                for hf in range(2):
                    sq = stat.c(2 + hf, 3 + hf)
                    xh = xt[tt % ST].c(hf * 1024, (hf + 1) * 1024)
                    P.op('act', lambda h, xh=xh, sq=sq: h.activation(out=hsv.ap, in_=xh.ap, func=AF.Square, accum_out=sq.ap),
                         reads=[xh], writes=[hsv, sq])
                P.op('dve', lambda h: h.tensor_tensor(out=ssq.ap, in0=stat.t[:, 2:3], in1=stat.t[:, 3:4], op=ALU.add),
                     reads=[stat.c(2, 4)], writes=[ssq])
                rstd_from_ssq(ssq, D, rs)
                P.op('dve', lambda h, xv=xv: h.scalar_tensor_tensor(out=xv.ap, in0=xv.ap, scalar=rs.ap, in1=fgb.t[:, :], op0=ALU.mult, op1=ALU.mult),
                     reads=[xv, rs, fgb.c(0, D)], writes=[xv])
                out_sems.append(P.dma('sp', lambda h, xv=xv, tt=tt: h.dma_start(out=x_out[tt * 128:(tt + 1) * 128, :], in_=xv.ap),
                                      reads=[xv], writes=[V(XO, None, lo, hi)]))

        load('sp', cst_identf.c(0, 128), ident_f_d[:, :])
        load('sp', cst_identb.c(0, 128), ident_b_d[:, :])
        load('sp', cst_trin.c(0, 128), trin_d[:, :])
        load('sp', g_mix.c(0, KC), mixg[:, :])
        P.op('pool', lambda h: h.memset(glT.t[:, :], 1.0), writes=[glT.c(0, 512)])
        load_w(wgu_a.c(0, 512, 0, 16), wgu[:, :])
        load_w(wgu_a.c(0, 512, 16, 17), bgate[:, :])
        P.op('pool', lambda h: h.memset(bsum.t[:, :], 0.0), writes=[bsum.c(0, 4)])
        if phase == 1:
            P.op('pool', lambda h: h.memset(Sf.t[:, :], 0.0), writes=[Sf.c(0, 1024)])
            P.op('pool', lambda h: h.memset(Sb.t[:, :], 0.0), writes=[Sb.c(0, 1024)])
        else:
            load('sp', cst_mask4.c(0, 512), mask4_d[:, :])
            load('sp', cst_mcur.c(0, 512), mcur_d[:, :])
            load('sp', cst_mcur0.c(0, 512), mcur0_d[:, :])
            load('sp', cst_mprev.c(0, 512), mprev_d[:, :])
            load('sp', cmask.c(0, 8), cmask_d[:, :])
            load('sp', g_ffn.c(0, KC), ffng[:, :])
            load('sp', psc.c(0, 8), pscale[:, :])
            load('sp', gnb.c(0, 1024), gnorm.partition_broadcast(128))
            load('sp', uprev.c(0, 1024), uhalo[:, :])
            load_w(wpool.c(0, 2048).r("p (a b) -> p a b", b=256), w_pool.rearrange("(a p) d -> p a d", p=128))
            if moe:
                load('sp', wr.c(0, KC * NEXP).r("p (a b) -> p a b", b=NEXP), w_router.rearrange("(a p) e -> p a e", p=128))
                load('sp', cst_ustrict.c(0, 128), ustrict_d[:, :])
                load('sp', cst_ones.c(0, 128), ones_d[:, :])
                P.op('pool', lambda h: h.memset(carry.t[:, :], 0.0), writes=[carry.c(0, NEXP)])
            P.op('pool', lambda h: h.memset(Sf.t[:, :], 0.0), writes=[Sf.c(0, 1024)])
            Lb = A("otmp", f32=True)
            bj = stat.c(8, 12)
            aj = stat.c(12, 16)
            for j in range(NCORES):
                load('sp', Lb, lall[j * 128:(j + 1) * 128, :])
                load('sp', bj, ball[j * 128:(j + 1) * 128, :])
                mj = cmask.c(j, j + 1)
                P.op('dve', lambda h, mj=mj: h.tensor_scalar(out=aj.ap, in0=bj.ap, scalar1=mj.ap, scalar2=None, op0=ALU.mult),
                     reads=[bj, mj], writes=[aj])
                P.op('act', lambda h: h.activation(out=aj.ap, in_=aj.ap, func=AF.Exp), reads=[aj], writes=[aj])
                P.op('dve', lambda h, mj=mj: h.tensor_scalar(out=Lb.ap, in0=Lb.ap, scalar1=mj.ap, scalar2=None, op0=ALU.mult),
                     reads=[Lb, mj], writes=[Lb])
                for hh in range(4):
                    sv = Sf.c(hh * 256, (hh + 1) * 256)
                    lv = V(arena, Lb.ap[:, hh * 256:(hh + 1) * 256], Lb.lo, Lb.hi)
                    av = stat.c(12 + hh, 13 + hh)
                    P.op('dve', lambda h, sv=sv, lv=lv, av=av: h.scalar_tensor_tensor(
                        out=sv.ap, in0=sv.ap, scalar=av.ap, in1=lv.ap, op0=ALU.mult, op1=ALU.add),
                        reads=[sv, lv, av], writes=[sv])
            P.op('dve', lambda h: h.tensor_copy(out=Sb.t[:, :], in_=Sf.t[:, :]), reads=[Sf.c(0, 1024)], writes=[Sb.c(0, 1024)])

        out_sems = []
        zoff = AO["z"][0]

        def Z(ti, lo, hi):
            return arena.c(zoff + ti * 3072 + lo, zoff + ti * 3072 + hi)

        for st in range(NST):
            for ti in range(ST):
                g = st * ST + ti
                load('sp', xt[ti].c(0, D), x_in[g * 128:(g + 1) * 128, :])
                norm_tile(xt[ti], g_mix, ti)

            if STOP <= 1:
                continue
            blocks = [('u', 0, 512, 0), ('u', 512, 512, 512), ('q', 1024, 512, 0), ('k', 1536, 512, 0),
                      ('v', 2048, 512, 1024), ('v', 2560, 512, 1536), ('g', 3072, 16, 0),
                      ('r', 3088, 512, 2048), ('r', 3600, 512, 2560)]
            for kind, c0, ncol, zc in blocks:
                if phase == 1 and (kind in ('q', 'r') or (kind == 'u' and st != NST - 1)):
                    continue
                ws = wslot()
                wv = ws.c(0, KC * ncol)
                load_w(wv.r("p (a b) -> p a b", b=ncol), w_in[:, c0:c0 + ncol].rearrange("(a p) c -> p a c", p=128))
                if kind in ('u', 'v', 'r'):
                    for ti in range(ST):
                        bank = pb[ti % 4]
                        for kc in range(KC):
                            mm(bank.c(0, 512), hT.c(kc * 512 + ti * 128, kc * 512 + (ti + 1) * 128),
                               ws.c(kc * 512, (kc + 1) * 512), kc == 0, kc == KC - 1)
                        evac_copy(Z(ti, zc, zc + 512), bank.c(0, 512))
                elif kind in ('q', 'k'):
                    for hh in range(4):
                        bank = pb[hh]
                        for kc in range(KC):
                            mm(bank.c(0, 512), ws.c(kc * 512 + hh * 128, kc * 512 + (hh + 1) * 128),
                               hT.c(kc * 512, (kc + 1) * 512), kc == 0, kc == KC - 1)
                        dst = A("qT" if kind == 'q' else "kT").r("p (a b c) -> p a b c", a=ST, b=4).s(slice(None), slice(None), hh, slice(None))
                        evac_copy(dst, bank.c(0, 512).r("p (a c) -> p a c", a=ST))
                else:
                    bank = pb[4]
                    for kc in range(KC):
                        mm(bank.c(0, 512, 0, 16), ws.c(kc * 16, (kc + 1) * 16), hT.c(kc * 512, (kc + 1) * 512),
                           kc == 0, kc == KC - 1)
                    evac_copy(glT.c(0, 512, 0, 16), bank.c(0, 512, 0, 16))

            if STOP <= 2:
                continue
            for ti in range(ST):
                g = st * ST + ti
                kT_t = A("kT", ti * 512, (ti + 1) * 512)
                mm(pb[5].c(0, 512), glT.c(ti * 128, (ti + 1) * 128, 0, 17), wgu_a.c(0, 512, 0, 17), True, True)
                ysp = A("ysp", f32=True)
                spv = A("sp")
                P.op('act', lambda h, ysp=ysp: h.activation(out=ysp.ap, in_=pb[5].t[:, 0:512], func=AF.Exp, scale=-1.0),
                     reads=[pb[5].c(0, 512)], writes=[ysp])
                P.op('act', lambda h, ysp=ysp, spv=spv: h.activation(out=spv.ap, in_=ysp.ap, func=AF.Ln, bias=1.0),
                     reads=[ysp], writes=[spv])
                if STOP <= 2.2:
                    continue
                for hh in range(4):
                    mm(pb[6].c(hh * 128, (hh + 1) * 128), V(arena, spv.ap[:, hh * 128:(hh + 1) * 128], spv.lo, spv.hi),
                       cst_trin.c(0, 128), True, True)
                epos = A("epos", f32=True)
                eneg = A("eneg", f32=True)
                bcv = pb[6].c(0, 512)
                P.op('act', lambda h, epos=epos: h.activation(out=epos.ap, in_=pb[6].t[:, 0:512], func=AF.Exp),
                     reads=[bcv], writes=[epos])
                P.op('act', lambda h, eneg=eneg: h.activation(out=eneg.ap, in_=pb[6].t[:, 0:512], func=AF.Exp, scale=-1.0),
                     reads=[bcv], writes=[eneg])
                if STOP <= 2.3:
                    continue
                bl = V(pb[6], pb[6].t[:, 0:512].rearrange("p (a b) -> p a b", b=128)[:, :, 127], 0, 2048)
                P.op('dve', lambda h, bl=bl: h.tensor_tensor(out=bsum.t[:, :], in0=bl.ap, in1=bsum.t[:, :], op=ALU.add),
                     reads=[bsum.c(0, 4), bl], writes=[bsum.c(0, 4)])
                if STOP <= 2.4:
                    continue
                kd = A("kd")
                P.op('dve', lambda h, kd=kd, kT_t=kT_t, eneg=eneg: h.tensor_tensor(out=kd.ap, in0=kT_t.ap, in1=eneg.ap, op=ALU.mult),
                     reads=[kT_t, eneg], writes=[kd])
                if phase == 2:
                    qT_t = A("qT", ti * 512, (ti + 1) * 512)
                    qd = A("qd")
                    P.op('dve', lambda h, qd=qd, qT_t=qT_t, epos=epos: h.scalar_tensor_tensor(
                        out=qd.ap, in0=qT_t.ap, scalar=float(128 ** -0.5), in1=epos.ap, op0=ALU.mult, op1=ALU.mult),
                        reads=[qT_t, epos], writes=[qd])
                    for hh in range(4):
                        mm(pb[4].c(hh * 128, (hh + 1) * 128), V(arena, kd.ap[:, hh * 128:(hh + 1) * 128], kd.lo, kd.hi),
                           V(arena, qd.ap[:, hh * 128:(hh + 1) * 128], qd.lo, qd.hi), True, True)
                    att = A("att")
                    P.op('dve', lambda h, att=att: h.tensor_tensor(out=att.ap, in0=pb[4].t[:, 0:512], in1=cst_mask4.t[:, :], op=ALU.mult),
                         reads=[pb[4].c(0, 512), cst_mask4.c(0, 512)], writes=[att])
                    for hh in range(4):
                        ob = pb[hh // 2].c((hh % 2) * 256, (hh % 2) * 256 + 256)
                        mm(ob, V(arena, qd.ap[:, hh * 128:(hh + 1) * 128], qd.lo, qd.hi), Sb.c(hh * 256, (hh + 1) * 256), True, False)
                        mm(ob, V(arena, att.ap[:, hh * 128:(hh + 1) * 128], att.lo, att.hi), Z(ti, 1024 + hh * 256, 1024 + (hh + 1) * 256), False, True)
                kteT = A("kteT")
                for hh in range(4):
                    el = V(arena, epos.ap[:, hh * 128 + 127:hh * 128 + 128], epos.lo, epos.hi)
                    o_ = V(arena, kteT.ap[:, hh * 128:(hh + 1) * 128], kteT.lo, kteT.hi)
                    i_ = V(arena, kd.ap[:, hh * 128:(hh + 1) * 128], kd.lo, kd.hi)
                    P.op('dve', lambda h, o_=o_, i_=i_, el=el: h.tensor_scalar(out=o_.ap, in0=i_.ap, scalar1=el.ap, scalar2=None, op0=ALU.mult),
                         reads=[i_, el], writes=[o_])
                if STOP <= 2.6:
                    continue
                for hh in range(4):
                    tr(pbb.c(hh * 128, (hh + 1) * 128), V(arena, kteT.ap[:, hh * 128:(hh + 1) * 128], kteT.lo, kteT.hi), cst_identb.c(0, 128))
                kte = A("kte")
                P.op('act', lambda h, kte=kte: h.activation(out=kte.ap, in_=pbb.t[:, 0:512], func=AF.Copy),
                     reads=[pbb.c(0, 512)], writes=[kte])
                if STOP <= 2.8:
                    continue
                for hh in range(4):
                    cb = pb[2 + hh // 2].c((hh % 2) * 256, (hh % 2) * 256 + 256)
                    mm(cb, V(arena, kte.ap[:, hh * 128:(hh + 1) * 128], kte.lo, kte.hi), Z(ti, 1024 + hh * 256, 1024 + (hh + 1) * 256), True, True)
                for hh in range(4):
                    cb = pb[2 + hh // 2].c((hh % 2) * 256, (hh % 2) * 256 + 256)
                    el = V(arena, epos.ap[:, hh * 128 + 127:hh * 128 + 128], epos.lo, epos.hi)
                    sv = Sf.c(hh * 256, (hh + 1) * 256)
                    P.op('dve', lambda h, sv=sv, el=el: h.tensor_scalar(out=sv.ap, in0=sv.ap, scalar1=el.ap, scalar2=None, op0=ALU.mult),
                         reads=[sv, el], writes=[sv])
                    P.op('dve', lambda h, sv=sv, cb=cb: h.tensor_tensor(out=sv.ap, in0=cb.ap, in1=sv.ap, op=ALU.add),
                         reads=[sv, cb], writes=[sv])
                P.op('dve', lambda h: h.tensor_copy(out=Sb.t[:, :], in_=Sf.t[:, :]), reads=[Sf.c(0, 1024)], writes=[Sb.c(0, 1024)])

                if phase == 1:
                    continue

                otmp = A("otmp", f32=True)
                ssq4 = stat.c(16, 20)
                rs4 = stat.c(20, 24)
                for hh in range(4):
                    ob = pb[hh // 2].c((hh % 2) * 256, (hh % 2) * 256 + 256)
                    sq = stat.c(16 + hh, 17 + hh)
                    ot = V(arena, otmp.ap[:, hh * 256:(hh + 1) * 256], otmp.lo, otmp.hi)
                    P.op('act', lambda h, ob=ob, sq=sq, ot=ot: h.activation(out=ot.ap, in_=ob.ap, func=AF.Square, accum_out=sq.ap),
                         reads=[ob], writes=[ot, sq])
                rstd_from_ssq(ssq4, 256, rs4)
                for hh in range(4):
                    ob = pb[hh // 2].c((hh % 2) * 256, (hh % 2) * 256 + 256)
                    rv = stat.c(20 + hh, 21 + hh)
                    ot = V(arena, otmp.ap[:, hh * 256:(hh + 1) * 256], otmp.lo, otmp.hi)
                    gv = gnb.c(hh * 256, (hh + 1) * 256)
                    P.op('dve', lambda h, ob=ob, rv=rv, ot=ot, gv=gv: h.scalar_tensor_tensor(
                        out=ot.ap, in0=ob.ap, scalar=rv.ap, in1=gv.ap, op0=ALU.mult, op1=ALU.mult),
                        reads=[ob, rv, gv], writes=[ot])
                sr = A("sr")
                rz = Z(ti, 2048, 3072)
                P.op('act', lambda h, sr=sr, rz=rz: h.activation(out=sr.ap, in_=rz.ap, func=AF.Silu), reads=[rz], writes=[sr])
                og = A("og")
                P.op('dve', lambda h, og=og, otmp=otmp, sr=sr: h.tensor_tensor(out=og.ap, in0=otmp.ap, in1=sr.ap, op=ALU.mult),
                     reads=[otmp, sr], writes=[og])
                for c in range(8):
                    tr(pbb.c(c * 128, (c + 1) * 128), V(arena, og.ap[:, c * 128:(c + 1) * 128], og.lo, og.hi), cst_identb.c(0, 128))
                for c in range(8):
                    evac_copy(mixT.c((8 + c) * 512 + ti * 128, (8 + c) * 512 + (ti + 1) * 128), pbb.c(c * 128, (c + 1) * 128))

                ucur = Z(ti, 0, 1024)
                mc = cst_mcur0 if g == 0 else cst_mcur
                for c in range(8):
                    gi = c // 2
                    ob = pb[4 + c // 4].c((c % 4) * 128, (c % 4 + 1) * 128)
                    mm(ob, uprev.c(c * 128, (c + 1) * 128), cst_mprev.c(gi * 128, (gi + 1) * 128), True, False)
                    mm(ob, V(arena, ucur.ap[:, c * 128:(c + 1) * 128], ucur.lo, ucur.hi), mc.c(gi * 128, (gi + 1) * 128), False, True)
                dT = A("dT")
                evac_copy(V(arena, dT.ap[:, 0:512], dT.lo, dT.hi), pb[4].c(0, 512))
                evac_copy(V(arena, dT.ap[:, 512:1024], dT.lo, dT.hi), pb[5].c(0, 512))
                for oc in range(8):
                    gi, jj = oc // 2, oc % 2
                    ob = pb[4 + oc // 4].c((oc % 4) * 128, (oc % 4 + 1) * 128)
                    for ci in range(2):
                        wv = wpool.c((gi * 2 + ci) * 256 + jj * 128, (gi * 2 + ci) * 256 + (jj + 1) * 128)
                        mm(ob, wv, V(arena, dT.ap[:, (gi * 2 + ci) * 128:(gi * 2 + ci + 1) * 128], dT.lo, dT.hi), ci == 0, ci == 1)
                for oc in range(8):
                    ob = pb[4 + oc // 4].c((oc % 4) * 128, (oc % 4 + 1) * 128)
                    evac_copy(mixT.c(oc * 512 + ti * 128, oc * 512 + (ti + 1) * 128), ob, psc.c(oc, oc + 1))
                P.op('pool', lambda h, ucur=ucur: h.tensor_copy(out=uprev.t[:, :], in_=ucur.ap), reads=[ucur], writes=[uprev.c(0, 1024)])

            if phase == 1:
                continue

            for cb in range(4):
                ws = wslot()
                load_w(ws.c(0, KC * 512).r("p (a b) -> p a b", b=512), w_out[:, cb * 512:(cb + 1) * 512].rearrange("(a p) c -> p a c", p=128))
                for ti in range(ST):
                    bank = pb[ti]
                    for kc in range(KC):
                        mm(bank.c(0, 512), mixT.c(kc * 512 + ti * 128, kc * 512 + (ti + 1) * 128), ws.c(kc * 512, (kc + 1) * 512),
                           kc == 0, kc == KC - 1)
                    xv = xt[ti].c(cb * 512, (cb + 1) * 512)
                    P.op('dve', lambda h, xv=xv, bank=bank: h.tensor_tensor(out=xv.ap, in0=bank.t[:, 0:512], in1=xv.ap, op=ALU.add),
                         reads=[bank.c(0, 512), xv], writes=[xv])

            if moe:
                for ti in range(ST):
                    moe_route_tile(ti, st * ST + ti)
                continue

            for ti in range(ST):
                norm_tile(xt[ti], g_ffn, ti)

            for e in range(NE):
                for half in range(2):
                    f0 = half * HALF
                    for fb in range(HALF // 2):
                        ws = wslot()
                        c0 = (f0 + fb * 2) * 128
                        gv = ws.c(0, KC * 256)
                        uv = ws.c(KC * 256, 2 * KC * 256)
                        load_w(gv.r("p (a b) -> p a b", b=256), wg_d[e * D:(e + 1) * D, c0:c0 + 256].rearrange("(a p) c -> p a c", p=128))
                        load_w(uv.r("p (a b) -> p a b", b=256), wu_d[e * D:(e + 1) * D, c0:c0 + 256].rearrange("(a p) c -> p a c", p=128))
                        for j in range(2):
                            fc = fb * 2 + j
                            gb_ = pb[(fc % 2) * 2]
                            ub_ = pb[(fc % 2) * 2 + 1]
                            for kc in range(KC):
                                mm(gb_.c(0, 512), ws.c(kc * 256 + j * 128, kc * 256 + (j + 1) * 128), hT.c(kc * 512, (kc + 1) * 512),
                                   kc == 0, kc == KC - 1)
                            for kc in range(KC):
                                mm(ub_.c(0, 512), ws.c(KC * 256 + kc * 256 + j * 128, KC * 256 + kc * 256 + (j + 1) * 128),
                                   hT.c(kc * 512, (kc + 1) * 512), kc == 0, kc == KC - 1)
                            sg = hs.c((fc % 2) * 512, (fc % 2) * 512 + 512)
                            P.op('act', lambda h, sg=sg, gb_=gb_: h.activation(out=sg.ap, in_=gb_.t[:, 0:512], func=AF.Silu),
                                 reads=[gb_.c(0, 512)], writes=[sg])
                            av = ACT_(fc)
                            P.op('dve', lambda h, av=av, sg=sg, ub_=ub_: h.tensor_tensor(out=av.ap, in0=ub_.t[:, 0:512], in1=sg.ap, op=ALU.mult),
                                 reads=[ub_.c(0, 512), sg], writes=[av])
                    PIECE = 11 if not moe else 14
                    for cb in range(4):
                        for pi in range(HALF // PIECE):
                            ws = wslot()
                            r0 = e * FF + (f0 + pi * PIECE) * 128
                            load_w(ws.c(0, PIECE * 512).r("p (a b) -> p a b", b=512),
                                   wd_d[r0:r0 + PIECE * 128, cb * 512:(cb + 1) * 512].rearrange("(a p) c -> p a c", p=128))
                            for ti in range(ST):
                                bank = pb[3 + ti]
                                for q in range(PIECE):
                                    fc = pi * PIECE + q
                                    mm(bank.c(0, 512), ACT_(fc, ti * 128, (ti + 1) * 128), ws.c(q * 512, (q + 1) * 512),
                                       fc == 0, fc == HALF - 1)
                        for ti in range(ST):
                            bank = pb[3 + ti]
                            xv = xt[ti].c(cb * 512, (cb + 1) * 512)
                            if moe:
                                wv = wgt.c(ti * NEXP + e, ti * NEXP + e + 1)
                                P.op('dve', lambda h, xv=xv, bank=bank, wv=wv: h.scalar_tensor_tensor(
                                    out=xv.ap, in0=bank.t[:, 0:512], scalar=wv.ap, in1=xv.ap, op0=ALU.mult, op1=ALU.add),
                                    reads=[bank.c(0, 512), wv, xv], writes=[xv])
                            else:
                                P.op('dve', lambda h, xv=xv, bank=bank: h.tensor_tensor(out=xv.ap, in0=bank.t[:, 0:512], in1=xv.ap, op=ALU.add),
                                     reads=[bank.c(0, 512), xv], writes=[xv])

            if final:
                load('sp', fgb.c(0, D), fing.partition_broadcast(128))
            for ti in range(ST):
                g = st * ST + ti
                xv = xt[ti].c(0, D)
                if final:
                    ssq = stat.c(0, 1)
                    rs = stat.c(1, 2)
                    hsv = hs.c(0, D)
                    P.op('act', lambda h, xv=xv: h.activation(out=hsv.ap, in_=xv.ap, func=AF.Square, accum_out=ssq.ap),
                         reads=[xv], writes=[hsv, ssq])
                    rstd_from_ssq(ssq, D, rs)
                    P.op('dve', lambda h, xv=xv: h.scalar_tensor_tensor(out=xv.ap, in0=xv.ap, scalar=rs.ap, in1=fgb.t[:, :], op0=ALU.mult, op1=ALU.mult),
                         reads=[xv, rs, fgb.c(0, D)], writes=[xv])
                out_sems.append(P.dma('sp', lambda h, xv=xv, g=g: h.dma_start(out=x_out[g * 128:(g + 1) * 128, :], in_=xv.ap), reads=[xv]))

        if moe and phase == 2:
            moe_experts()
            moe_final()
        if phase == 1:
            out_sems.append(P.dma('sp', lambda h: h.dma_start(out=lst_o[:, :], in_=Sf.t[:, :]), reads=[Sf.c(0, 1024)]))
            out_sems.append(P.dma('sp', lambda h: h.dma_start(out=bsum_o[:, :], in_=bsum.t[:, :]), reads=[bsum.c(0, 4)]))
            lastu = Z(ST - 1, 0, 1024)
            out_sems.append(P.dma('sp', lambda h: h.dma_start(out=ulast_o[:, :], in_=lastu.ap), reads=[lastu]))
        fin = {}
        for s, v in out_sems:
            fin[id(s)] = (s, max(v, fin.get(id(s), (s, 0))[1]))
        P.wait_all('sp', list(fin.values()))

        with nc.Block() as block:
            @block.tensor
            def _(h):
                P.emit_engine('pe', h)

            @block.scalar
            def _(h):
                P.emit_engine('act', h)

            @block.vector
            def _(h):
                P.emit_engine('dve', h)

            @block.gpsimd
            def _(h):
                P.emit_engine('pool', h)

            @block.sync
            def _(h):
                P.emit_engine('sp', h)
    return nc


_PROG_CACHE = {}


def _get(kind, phase, final):
    key = (kind, phase, final)
    if key not in _PROG_CACHE:
        _PROG_CACHE[key] = build(kind, phase, final)
    return _PROG_CACHE[key]


def kernel(x, mix_norm, w_in, w_pool, pool_scale, w_gate_up, b_gate, gla_norm, w_out,
           ffn_norm, dense_w_gate, dense_w_up, dense_w_down, w_router,
           exp_w_gate, exp_w_up, exp_w_down, final_norm, _nlayers=2):
    f = lambda a: np.ascontiguousarray(np.asarray(a, dtype=np.float32))
    x = f(x).reshape(SEQ, D)
    consts = [_consts(c) for c in range(NCORES)]
    xs = [np.ascontiguousarray(x[c * TPC:(c + 1) * TPC]) for c in range(NCORES)]
    bf = ml_dtypes.bfloat16
    for l in range(_nlayers):
        kind = 'dense' if l % 2 == 0 else 'moe'
        common = {
            "mixg": _fm(f(mix_norm[l])), "w_in": f(w_in[l]), "wgu": f(w_gate_up[l]),
            "bgate": f(b_gate[l]).reshape(1, 512),
        }
        nc1 = _get(kind, 1, False)
        maps = []
        for c in range(NCORES):
            m = dict(common)
            m["x_in"] = xs[c]
            for k in ("ident_f", "ident_b", "trin"):
                m[k] = consts[c][k]
            maps.append(m)
        r1 = run_bass_kernel_spmd(nc1, maps, core_ids=list(range(NCORES))).results
        lall = np.concatenate([np.asarray(r["lst"]) for r in r1], axis=0)
        ball = np.concatenate([np.asarray(r["bsum"]) for r in r1], axis=0)
        final = l == 1
        nc2 = _get(kind, 2, final)
        maps = []
        for c in range(NCORES):
            m = dict(common)
            m["x_in"] = xs[c]
            m.update(consts[c])
            m["lall"] = lall
            m["ball"] = ball
            m["uhalo"] = np.asarray(r1[c - 1]["ulast"]) if c > 0 else np.zeros((128, 1024), bf)
            m["w_pool"] = f(w_pool[l]).reshape(4 * 256, 256)
            m["pscale"] = _fm(f(pool_scale[l]).reshape(-1))
            m["gnorm"] = f(gla_norm[l]).reshape(1, 1024)
            m["w_out"] = f(w_out[l])
            m["ffng"] = _fm(f(ffn_norm[l]))
            if kind == 'moe':
                i = l // 2
                m["w_router"] = f(w_router[i])
                m["wg"] = f(exp_w_gate[i]).reshape(NEXP * D, FF_EXP)
                m["wu"] = f(exp_w_up[i]).reshape(NEXP * D, FF_EXP)
                m["wd"] = f(exp_w_down[i]).reshape(NEXP * FF_EXP, D)
            else:
                i = l // 2
                m["wg"] = f(dense_w_gate[i])
                m["wu"] = f(dense_w_up[i])
                m["wd"] = f(dense_w_down[i])
            if final:
                m["fing"] = f(final_norm).reshape(1, D)
            maps.append(m)
        r2 = run_bass_kernel_spmd(nc2, maps, core_ids=list(range(NCORES))).results
        xs = [np.asarray(r["x_out"]) for r in r2]
    out = np.concatenate(xs, axis=0).reshape(1, SEQ, D).astype(np.float32)
    return out
```

```python
import numpy as np
import ml_dtypes
import concourse.bass as bass
import concourse.mybir as mybir
from concourse.bass_utils import run_bass_kernel_spmd

F32 = mybir.dt.float32
BF16 = mybir.dt.bfloat16
AF = mybir.ActivationFunctionType
ALU = mybir.AluOpType
AX = mybir.AxisListType

NCORES = 8
D = 2048
SEQ = 16384
TPC = SEQ // NCORES
NT = TPC // 128
ST = 4
NST = NT // ST
KC = D // 128
INW = 4112
FF_DENSE = 5632
FF_EXP = 7168
NEXP = 8
EPS = 1e-6
POOL_WINDOWS = (2, 4, 8, 16)
WSLOT = 8192
NWSLOT = 3
STOP = 99
EVAC_ACT_SCALE = True


class TT:
    def __init__(self, t, esz, name, holder=None, base=0):
        self.t = t
        self.esz = esz
        self.name = name
        self.H = holder if holder is not None else [[]]
        self.base = base
        self.whole = False

    @property
    def hist(self):
        return self.H[0]

    @hist.setter
    def hist(self, v):
        self.H[0] = v

    def c(self, lo, hi, p0=0, p1=None):
        ap = self.t[p0:p1, lo:hi] if p1 is not None else (self.t[p0:, lo:hi] if p0 else self.t[:, lo:hi])
        if self.whole:
            return V(self, ap, 0, 1 << 20)
        return V(self, ap, self.base + lo * self.esz, self.base + hi * self.esz)

    def view(self, lo_bytes, n, dt, name=None):
        esz = 4 if dt == F32 else 2
        a = lo_bytes // self.esz
        b = a + (n * esz) // self.esz
        ap = self.t[:, a:b]
        if esz != self.esz or dt != getattr(self, "dt", None):
            ap = ap.bitcast(dt)
        tt = TT(ap, esz, name or self.name, holder=self.H, base=self.base + lo_bytes)
        tt.dt = dt
        return tt


class V:
    def __init__(self, tt, ap, lo, hi):
        self.tt = tt
        self.ap = ap
        self.lo = lo
        self.hi = hi

    def r(self, pat, **kw):
        return V(self.tt, self.ap.rearrange(pat, **kw), self.lo, self.hi)

    def s(self, *key):
        return V(self.tt, self.ap[key], self.lo, self.hi)


class Eng:
    def __init__(self, key, sem):
        self.key = key
        self.sem = sem
        self.count = 0
        self.known = {}
        self.ops = []
        self.lanes = []
        self.lane_val = []
        self.lane_next = 0


class Prog:
    def __init__(self, nc):
        self.nc = nc
        self.E = {}
        self.sems = {}

    def add_engine(self, key, sem, lanes=()):
        e = Eng(key, sem)
        self.sems[id(sem)] = sem
        e.lanes = list(lanes)
        e.lane_val = [0] * len(lanes)
        for s in lanes:
            self.sems[id(s)] = s
        self.E[key] = e

    def _need(self, e, reads, writes):
        need = {}

        def add(h):
            if h[2] == 'pe' and e.key == 'pe' and h[3] == id(e.sem):
                return
            if need.get(h[3], 0) < h[4]:
                need[h[3]] = h[4]
        for v in reads:
            whole = v.tt.whole
            for h in v.tt.hist:
                if (h[5] or (whole and h[2] != e.key)) and h[0] < v.hi and v.lo < h[1]:
                    add(h)
        for v in writes:
            for h in v.tt.hist:
                if h[0] < v.hi and v.lo < h[1]:
                    add(h)
        waits = []
        for sid, val in need.items():
            if e.known.get(sid, 0) < val:
                e.known[sid] = val
                waits.append((self.sems[sid], val))
        return waits

    def _record(self, e, reads, writes, sid, val):
        for v in writes:
            hist = v.tt.hist
            v.tt.hist = [h for h in hist if not (h[0] >= v.lo and h[1] <= v.hi)]
            v.tt.hist.append([v.lo, v.hi, e.key, sid, val, True])
        for v in reads:
            hist = v.tt.hist
            v.tt.hist = [h for h in hist if not ((not h[5]) and h[3] == sid and h[0] >= v.lo and h[1] <= v.hi)]
            v.tt.hist.append([v.lo, v.hi, e.key, sid, val, False])

    def op(self, ek, fn, reads=(), writes=(), sig=True):
        e = self.E[ek]
        waits = self._need(e, reads, writes)
        val = e.count + 1
        if sig:
            e.count = val
        e.ops.append((waits, fn, (e.sem, 1) if sig else None))
        self._record(e, reads, writes, id(e.sem), val)

    def dma(self, ek, fn, reads=(), writes=()):
        e = self.E[ek]
        li = e.lane_next
        e.lane_next = (li + 1) % len(e.lanes)
        sem = e.lanes[li]
        waits = self._need(e, reads, writes)
        prev = e.lane_val[li]
        if prev > 0 and e.known.get(id(sem), 0) < prev:
            e.known[id(sem)] = prev
            waits.append((sem, prev))
        val = prev + 16
        e.lane_val[li] = val
        e.ops.append((waits, fn, (sem, 16)))
        self._record(e, reads, writes, id(sem), val)
        return sem, val

    def wait_all(self, ek, items):
        e = self.E[ek]
        waits = [(s, v) for (s, v) in items]
        e.ops.append((waits, None, None))

    def emit_engine(self, ek, h):
        for waits, fn, inc in self.E[ek].ops:
            for s, v in waits:
                h.wait_ge(s, v)
            if fn is None:
                continue
            ins = fn(h)
            if inc is not None:
                ins.then_inc(inc[0], inc[1])


def _pool_mats(first_core):
    cur = np.zeros((4, 128, 128), np.float32)
    cur0 = np.zeros((4, 128, 128), np.float32)
    prev = np.zeros((4, 128, 128), np.float32)
    for g, w in enumerate(POOL_WINDOWS):
        for t in range(128):
            for s in range(t - w + 1, t + 1):
                if s >= 0:
                    cur[g, s, t] += 1.0 / w
                else:
                    prev[g, 128 + s, t] += 1.0 / w
            cur[g, t, t] -= 1.0
            cnt = min(t + 1, w)
            for s in range(max(0, t - w + 1), t + 1):
                cur0[g, s, t] += 1.0 / cnt
            cur0[g, t, t] -= 1.0
    return cur, (cur0 if first_core else cur), prev


def _consts(core):
    bf = ml_dtypes.bfloat16
    c = {}
    c["ident_f"] = np.eye(128, dtype=np.float32)
    c["ident_b"] = np.eye(128, dtype=np.float32).astype(bf)
    j = np.arange(128)[:, None]
    i = np.arange(128)[None, :]
    tri = (j <= i).astype(np.float32)
    c["trin"] = (tri * (-1.0 / 16.0)).astype(bf)
    c["mask4"] = np.tile(tri, (1, 4)).astype(bf)
    cur, cur0, prev = _pool_mats(core == 0)
    c["mcur"] = np.ascontiguousarray(cur.transpose(1, 0, 2).reshape(128, 512)).astype(bf)
    c["mcur0"] = np.ascontiguousarray(cur0.transpose(1, 0, 2).reshape(128, 512)).astype(bf)
    c["mprev"] = np.ascontiguousarray(prev.transpose(1, 0, 2).reshape(128, 512)).astype(bf)
    m = np.zeros((128, 8), np.float32)
    m[:, :core] = 1.0
    c["cmask"] = m
    c["iota"] = np.tile(np.arange(640, dtype=np.float32)[None, :], (128, 1))
    c["ustrict"] = (j < i).astype(np.float32).astype(bf)
    c["ones"] = np.ones((128, 128), np.float32).astype(bf)
    return c


def _fm(vec):
    return np.ascontiguousarray(vec.reshape(-1, 128).T)


def build(layer_kind, phase, final):
    nc = bass.Bass("TRN2", target_bir_lowering=False)
    moe = layer_kind == 'moe'
    FF = FF_EXP if moe else FF_DENSE
    NE = NEXP if moe else 1
    NFC = FF // 128
    HALF = NFC // 2

    def din(name, shape, dt=F32):
        return nc.dram_tensor(name, list(shape), dt, kind="ExternalInput").ap()

    def dout(name, shape, dt=F32):
        return nc.dram_tensor(name, list(shape), dt, kind="ExternalOutput").ap()

    x_in = din("x_in", [TPC, D])
    mixg = din("mixg", [128, KC])
    w_in = din("w_in", [D, INW])
    wgu = din("wgu", [16, 512])
    bgate = din("bgate", [1, 512])
    ident_f_d = din("ident_f", [128, 128])
    ident_b_d = din("ident_b", [128, 128], BF16)
    trin_d = din("trin", [128, 128], BF16)
    if phase == 1:
        lst_o = dout("lst", [128, 1024])
        bsum_o = dout("bsum", [128, 4])
        ulast_o = dout("ulast", [128, 1024], BF16)
    else:
        mask4_d = din("mask4", [128, 512], BF16)
        mcur_d = din("mcur", [128, 512], BF16)
        mcur0_d = din("mcur0", [128, 512], BF16)
        mprev_d = din("mprev", [128, 512], BF16)
        cmask_d = din("cmask", [128, 8])
        lall = din("lall", [NCORES * 128, 1024])
        ball = din("ball", [NCORES * 128, 4])
        uhalo = din("uhalo", [128, 1024], BF16)
        w_pool = din("w_pool", [4 * 256, 256])
        pscale = din("pscale", [128, 8])
        gnorm = din("gnorm", [1, 1024])
        w_out = din("w_out", [D, D])
        ffng = din("ffng", [128, KC])
        if moe:
            w_router = din("w_router", [D, NEXP])
            wg_d = din("wg", [NEXP * D, FF])
            wu_d = din("wu", [NEXP * D, FF])
            wd_d = din("wd", [NEXP * FF, D])
        else:
            wg_d = din("wg", [D, FF])
            wu_d = din("wu", [D, FF])
            wd_d = din("wd", [FF, D])
        if final:
            fing = din("fing", [1, D])
        x_out = dout("x_out", [TPC, D])
        if moe:
            iota_d = din("iota", [128, 640])
            ustrict_d = din("ustrict", [128, 128], BF16)
            ones_d = din("ones", [128, 128], BF16)
            h2s = nc.dram_tensor("h2s", [TPC, D], BF16, kind="Internal").ap()

    import contextlib
    es = contextlib.ExitStack()
    with es:
        def sb(name, n, dt=F32, p=128):
            t = es.enter_context(nc.sbuf_tensor("sb_" + name, [p, n], dt))
            tt = TT(t, 4 if dt == F32 else 2, name)
            tt.dt = dt
            return tt

        def ps(name, n, dt=F32):
            t = es.enter_context(nc.psum_tensor("ps_" + name, [128, n], dt))
            tt = TT(t, 4 if dt == F32 else 2, name)
            tt.whole = True
            return tt

        def sem(name):
            return es.enter_context(nc.semaphore(name))

        P = Prog(nc)
        P.add_engine('pe', sem("s_pe"))
        P.add_engine('act', sem("s_act"))
        P.add_engine('dve', sem("s_dve"))
        P.add_engine('pool', sem("s_pool"), [sem("lp%d" % i) for i in range(16)])
        P.add_engine('sp', sem("s_sp"), [sem("ls%d" % i) for i in range(16)])

        xtall = sb("xtall", ST * D)
        xt = [xtall.view(i * D * 4, D, F32, "xt%d" % i) for i in range(ST)]
        hsf = sb("hsf", 2 * D)
        hs = hsf.view(0, D, F32, "hs")
        mixc = sb("mixc", 5 * 1024)
        hT = sb("hT", KC * 512, BF16)
        wsl = [sb("wsl%d" % i, WSLOT, BF16) for i in range(NWSLOT)]
        arena = sb("arena", 27 * 1024, BF16)
        mixT = hT
        stat = sb("stat", 64)
        cst_identf = sb("identf", 128)
        cst_identb = sb("identb", 128, BF16)
        cst_trin = sb("trin", 128, BF16)
        g_mix = sb("g_mix", KC)
        wgu_a = sb("wgu_a", 512, BF16, p=32)
        Sf = mixc.view(0, 1024, F32, "Sf")
        Sb = mixc.view(4096, 1024, BF16, "Sb")
        glT = sb("glT", 512, BF16, p=32)
        if phase == 2:
            cst_mask4 = sb("mask4", 512, BF16)
            cst_mcur = sb("mcur", 512, BF16)
            cst_mcur0 = sb("mcur0", 512, BF16)
            cst_mprev = sb("mprev", 512, BF16)
            cmask = sb("cmask", 8)
            g_ffn = sb("g_ffn", KC)
            psc = sb("psc", 8)
            gnb = mixc.view(6144, 1024, F32, "gnb")
            wpool = mixc.view(10240, 8 * 256, BF16, "wpool")
            uprev = mixc.view(14336, 1024, BF16, "uprev")
            if moe:
                wr = sb("wr", KC * NEXP)
                hTf = hsf.view(D * 4, KC * 128, F32, "hTf")
                wgt_all = sb("wgt_all", NT * NEXP)
                mask_all = sb("mask_all", NT * NEXP)
                pos_all = sb("pos_all", NT * NEXP)
                carry = sb("carry", NEXP)
                maskb = sb("maskb", NEXP, BF16)
                cst_ustrict = sb("ustrict", 128, BF16)
                cst_ones = sb("ones", 128, BF16)
                rt = sb("rt", 64)
            if final:
                fgb = hTf if moe else sb("fgb", D)
        bsum = sb("bsum", 4)

        AO = {}
        off = 0

        def carve(name, n_bf16):
            nonlocal off
            AO[name] = (off, off + n_bf16)
            off += n_bf16
        carve("z", ST * 3072)
        carve("qT", ST * 512)
        carve("kT", ST * 512)
        carve("ysp", 1024)
        carve("sp", 512)
        carve("epos", 1024)
        carve("eneg", 1024)
        carve("qd", 512)
        carve("kd", 512)
        carve("kteT", 512)
        carve("kte", 512)
        carve("att", 512)
        carve("otmp", 2048)
        carve("sr", 1024)
        carve("og", 1024)
        carve("dT", 1024)
        assert off <= 27 * 1024, off

        def A(name, lo=0, hi=None, f32=False):
            a, b = AO[name]
            if f32:
                n = (b - a) // 2
                hi_ = n if hi is None else hi
                ap = arena.t[:, a:b].bitcast(F32)[:, lo:hi_]
                return V(arena, ap, a * 2 + lo * 4, a * 2 + hi_ * 4)
            hi_ = (b - a) if hi is None else hi
            return arena.c(a + lo, a + hi_)

        def ACT_(fc, lo=0, hi=512):
            base = fc * 512
            return arena.c(base + lo, base + hi)
        assert HALF * 512 <= 27 * 1024

        pb = [ps("pb%d" % i, 512) for i in range(7)]
        pbb = ps("pbb", 1024, BF16)

        def load(eng, dst, src_ap, **kw):
            P.dma(eng, lambda h: h.dma_start(out=dst.ap, in_=src_ap, **kw), writes=[dst])

        rr = [0]

        def evac_copy(dst, src, scale_ap=None):
            rr[0] ^= 1
            use_act = rr[0]
            if scale_ap is not None:
                bi = [i for i, b_ in enumerate(pb) if b_ is src.tt]
                use_act = EVAC_ACT_SCALE and bool(bi) and (bi[0] % 2 == 0)
            if use_act:
                if scale_ap is None:
                    P.op('act', lambda h: h.activation(out=dst.ap, in_=src.ap, func=AF.Copy), reads=[src], writes=[dst])
                else:
                    P.op('act', lambda h: h.activation(out=dst.ap, in_=src.ap, func=AF.Identity, scale=scale_ap.ap),
                         reads=[src, scale_ap], writes=[dst])
            else:
                if scale_ap is None:
                    P.op('dve', lambda h: h.tensor_copy(out=dst.ap, in_=src.ap), reads=[src], writes=[dst])
                else:
                    P.op('dve', lambda h: h.tensor_scalar(out=dst.ap, in0=src.ap, scalar1=scale_ap.ap, scalar2=None,
                                                          op0=ALU.mult), reads=[src, scale_ap], writes=[dst])

        def mm(out, lhsT, rhs, start, stop):
            P.op('pe', lambda h: h.matmul(out.ap, lhsT=lhsT.ap, rhs=rhs.ap, start=start, stop=stop),
                 reads=[lhsT, rhs], writes=[out], sig=stop)

        def tr(out, in_, ident):
            P.op('pe', lambda h: h.transpose(out.ap, in_.ap, ident.ap), reads=[in_, ident], writes=[out])

        wrr = [0]

        def wslot():
            s = wsl[wrr[0] % NWSLOT]
            wrr[0] += 1
            return s

        def load_w(dst_view, src_ap):
            P.dma('pool', lambda h: h.dma_start(out=dst_view.ap, in_=src_ap, max_dma_last_dim=8192), writes=[dst_view])

        def rstd_from_ssq(ssq, n, out):
            P.op('dve', lambda h: h.tensor_scalar(out=out.ap, in0=ssq.ap, scalar1=1.0 / n, scalar2=EPS,
                                                  op0=ALU.mult, op1=ALU.add), reads=[ssq], writes=[out])
            P.op('act', lambda h: h.activation(out=out.ap, in_=out.ap, func=AF.Sqrt), reads=[out], writes=[out])
            P.op('dve', lambda h: h.reciprocal(out=out.ap, in_=out.ap), reads=[out], writes=[out])

        def norm_tile(x_t, gain, ti, want_f32=False):
            ssq = stat.c(0, 1)
            rs = stat.c(1, 2)
            hsv = hs.c(0, D)
            xv = x_t.c(0, D)
            if STOP <= 0:
                return
            P.op('act', lambda h: h.activation(out=hsv.ap, in_=xv.ap, func=AF.Square, accum_out=ssq.ap),
                 reads=[xv], writes=[hsv, ssq])
            rstd_from_ssq(ssq, D, rs)
            P.op('dve', lambda h: h.tensor_scalar(out=hsv.ap, in0=xv.ap, scalar1=rs.ap, scalar2=None, op0=ALU.mult),
                 reads=[xv, rs], writes=[hsv])
            if STOP <= 0.3:
                return
            for b in range(4):
                for j in range(4):
                    kc = b * 4 + j
                    tr(pb[b].c(j * 128, (j + 1) * 128), hs.c(kc * 128, (kc + 1) * 128), cst_identf.c(0, 128))
            if STOP <= 0.6:
                return
            for b in range(4):
                for j in range(4):
                    kc = b * 4 + j
                    src = pb[b].c(j * 128, (j + 1) * 128)
                    evac_copy(hT.c(kc * 512 + ti * 128, kc * 512 + (ti + 1) * 128), src, gain.c(kc, kc + 1))
                    if want_f32:
                        evac_copy(hTf.c(kc * 128, (kc + 1) * 128), src, gain.c(kc, kc + 1))

        CAP = 640
        if moe and phase == 2:
            XO = TT(x_out, 1, "x_out_d")
            H2S = TT(h2s, 1, "h2s_d")

        def xo_cells(tt, dg=None):
            if dg is None:
                return tt * 4, tt * 4 + 4
            return tt * 4 + dg, tt * 4 + dg + 1

        def moe_route_tile(ti, g):
            x_t = xt[ti]
            ssq = stat.c(0, 1)
            rs = stat.c(1, 2)
            hsv = hs.c(0, D)
            xv = x_t.c(0, D)
            P.op('act', lambda h: h.activation(out=hsv.ap, in_=xv.ap, func=AF.Square, accum_out=ssq.ap),
                 reads=[xv], writes=[hsv, ssq])
            rstd_from_ssq(ssq, D, rs)
            P.op('dve', lambda h: h.tensor_scalar(out=hsv.ap, in0=xv.ap, scalar1=rs.ap, scalar2=None, op0=ALU.mult),
                 reads=[xv, rs], writes=[hsv])
            P.dma('pool', lambda h: h.dma_start(out=h2s[g * 128:(g + 1) * 128, :], in_=hsv.ap, max_dma_last_dim=8192),
                  reads=[hsv], writes=[V(H2S, None, 0, 1)])
            lo, hi = xo_cells(g)
            P.dma('sp', lambda h: h.dma_start(out=x_out[g * 128:(g + 1) * 128, :], in_=xv.ap), reads=[xv], writes=[V(XO, None, lo, hi)])
            for b in range(4):
                for j in range(4):
                    kc = b * 4 + j
                    tr(pb[b].c(j * 128, (j + 1) * 128), hs.c(kc * 128, (kc + 1) * 128), cst_identf.c(0, 128))
            for b in range(4):
                for j in range(4):
                    kc = b * 4 + j
                    evac_copy(hTf.c(kc * 128, (kc + 1) * 128), pb[b].c(j * 128, (j + 1) * 128), g_ffn.c(kc, kc + 1))
            lgp = pb[6].c(0, NEXP)
            for kc in range(KC):
                mm(lgp, hTf.c(kc * 128, (kc + 1) * 128), wr.c(kc * NEXP, (kc + 1) * NEXP), kc == 0, kc == KC - 1)
            lg = rt.c(0, 8)
            lg2 = rt.c(8, 16)
            eq1 = rt.c(16, 24)
            eq2 = rt.c(24, 32)
            m1 = rt.c(32, 33)
            m2 = rt.c(33, 34)
            ex = rt.c(34, 35)
            w1 = rt.c(35, 36)
            w2 = rt.c(36, 37)
            wv = wgt_all.c(g * NEXP, (g + 1) * NEXP)
            mk = mask_all.c(g * NEXP, (g + 1) * NEXP)
            pv = pos_all.c(g * NEXP, (g + 1) * NEXP)
            P.op('dve', lambda h: h.tensor_copy(out=lg.ap, in_=lgp.ap), reads=[lgp], writes=[lg])
            P.op('dve', lambda h: h.tensor_reduce(out=m1.ap, in_=lg.ap, axis=AX.X, op=ALU.max), reads=[lg], writes=[m1])
            P.op('dve', lambda h: h.tensor_scalar(out=eq1.ap, in0=lg.ap, scalar1=m1.ap, scalar2=None, op0=ALU.is_equal),
                 reads=[lg, m1], writes=[eq1])
            P.op('dve', lambda h: h.scalar_tensor_tensor(out=lg2.ap, in0=eq1.ap, scalar=-1e30, in1=lg.ap, op0=ALU.mult, op1=ALU.add),
                 reads=[eq1, lg], writes=[lg2])
            P.op('dve', lambda h: h.tensor_reduce(out=m2.ap, in_=lg2.ap, axis=AX.X, op=ALU.max), reads=[lg2], writes=[m2])
            P.op('dve', lambda h: h.tensor_scalar(out=eq2.ap, in0=lg2.ap, scalar1=m2.ap, scalar2=None, op0=ALU.is_equal),
                 reads=[lg2, m2], writes=[eq2])
            P.op('dve', lambda h: h.tensor_tensor(out=ex.ap, in0=m2.ap, in1=m1.ap, op=ALU.subtract), reads=[m1, m2], writes=[ex])
            P.op('act', lambda h: h.activation(out=ex.ap, in_=ex.ap, func=AF.Exp), reads=[ex], writes=[ex])
            P.op('dve', lambda h: h.tensor_scalar(out=w1.ap, in0=ex.ap, scalar1=1.0, scalar2=None, op0=ALU.add), reads=[ex], writes=[w1])
            P.op('dve', lambda h: h.reciprocal(out=w1.ap, in_=w1.ap), reads=[w1], writes=[w1])
            P.op('dve', lambda h: h.tensor_tensor(out=w2.ap, in0=ex.ap, in1=w1.ap, op=ALU.mult), reads=[ex, w1], writes=[w2])
            P.op('dve', lambda h: h.tensor_scalar(out=wv.ap, in0=eq1.ap, scalar1=w1.ap, scalar2=None, op0=ALU.mult),
                 reads=[eq1, w1], writes=[wv])
            P.op('dve', lambda h: h.scalar_tensor_tensor(out=wv.ap, in0=eq2.ap, scalar=w2.ap, in1=wv.ap, op0=ALU.mult, op1=ALU.add),
                 reads=[eq2, w2, wv], writes=[wv])
            P.op('dve', lambda h: h.tensor_tensor(out=mk.ap, in0=eq1.ap, in1=eq2.ap, op=ALU.add), reads=[eq1, eq2], writes=[mk])
            mb = maskb.c(0, NEXP)
            P.op('dve', lambda h: h.tensor_copy(out=mb.ap, in_=mk.ap), reads=[mk], writes=[mb])
            mm(pb[5].c(0, NEXP), cst_ustrict.c(0, 128), mb, True, True)
            mm(pb[5].c(NEXP, 2 * NEXP), cst_ones.c(0, 128), mb, True, True)
            cv = carry.c(0, NEXP)
            P.op('dve', lambda h: h.tensor_tensor(out=pv.ap, in0=pb[5].t[:, 0:NEXP], in1=cv.ap, op=ALU.add),
                 reads=[pb[5].c(0, NEXP), cv], writes=[pv])
            P.op('dve', lambda h: h.tensor_tensor(out=cv.ap, in0=pb[5].t[:, NEXP:2 * NEXP], in1=cv.ap, op=ALU.add),
                 reads=[pb[5].c(0, NEXP), cv], writes=[cv])

        def moe_experts():
            NPART = 8
            PCH = NFC // NPART
            Pe = xtall.view(0, NT * CAP, BF16, "Pe")
            sg = xtall.view(NT * CAP * 2, CAP, F32, "sg")
            actq = xtall.view(NT * CAP * 2 + CAP * 4, PCH * CAP, BF16, "actq")
            assert NT * CAP * 2 + CAP * 4 + PCH * CAP * 2 <= ST * D * 4
            yacc = arena.view(0, 5 * D, F32, "yacc")
            PT = arena.view(5 * D * 4, CAP, BF16, "PT")
            contrib = [arena.view(5 * D * 4 + CAP * 2 + i * 2048, 512, F32, "contrib%d" % i) for i in range(4)]
            iota = arena.view(5 * D * 4 + CAP * 2 + 4 * 2048, CAP, F32, "iota")
            PT2 = arena.view(5 * D * 4 + CAP * 2 + 4 * 2048 + CAP * 4, CAP, BF16, "PT2")
            PTs = [PT, PT2]
            assert 5 * D * 4 + CAP * 2 + 4 * 2048 + CAP * 4 + CAP * 2 <= 27 * 1024 * 2
            hTe = mixc.view(0, KC * CAP, BF16, "hTe")
            yb = mixc.view(0, 5 * D, BF16, "yb")
            h2c = [hT.view(0, NT * 512, BF16, "h2c0"), hsf.view(0, NT * 512, BF16, "h2c1")]
            load('sp', iota.c(0, CAP), iota_d[:, :])
            for e in range(NEXP):
                for tt in range(NT):
                    pv = pos_all.c(tt * NEXP + e, tt * NEXP + e + 1)
                    mk = mask_all.c(tt * NEXP + e, tt * NEXP + e + 1)
                    pe_ = Pe.c(tt * CAP, (tt + 1) * CAP)
                    P.op('dve', lambda h, pe_=pe_, pv=pv, mk=mk: h.tensor_scalar(
                        out=pe_.ap, in0=iota.t[:, :], scalar1=pv.ap, scalar2=mk.ap, op0=ALU.is_equal, op1=ALU.mult),
                        reads=[iota.c(0, CAP), pv, mk], writes=[pe_])
                for kg in range(4):
                    hc = h2c[kg % 2]
                    hcv = hc.c(0, NT * 512)
                    P.dma('sp', lambda h, hcv=hcv, kg=kg: h.dma_start(
                        out=hcv.ap.rearrange("p (a b) -> p a b", b=512),
                        in_=h2s[:, kg * 512:(kg + 1) * 512].rearrange("(a p) c -> p a c", p=128)),
                        reads=[V(H2S, None, 0, 1)], writes=[hcv])
                    for k4 in range(4):
                        kc = kg * 4 + k4
                        for cg, (c0, c1) in enumerate(((0, 512), (512, CAP))):
                            bank = pb[(kc * 2 + cg) % 4]
                            for tt in range(NT):
                                mm(bank.c(0, c1 - c0), hc.c(tt * 512 + k4 * 128, tt * 512 + (k4 + 1) * 128),
                                   Pe.c(tt * CAP + c0, tt * CAP + c1), tt == 0, tt == NT - 1)
                            evac_copy(hTe.c(kc * CAP + c0, kc * CAP + c1), bank.c(0, c1 - c0), g_ffn.c(kc, kc + 1))
                for qi in range(NPART):
                    f0 = qi * PCH
                    fl = 0
                    for nb in (2, 2, 2, 1):
                        ws = wslot()
                        c0 = (f0 + fl) * 128
                        gv = ws.c(0, KC * nb * 128)
                        uv = ws.c(KC * 256, KC * 256 + KC * nb * 128)
                        load_w(gv.r("p (a b) -> p a b", b=nb * 128), wg_d[e * D:(e + 1) * D, c0:c0 + nb * 128].rearrange("(a p) c -> p a c", p=128))
                        load_w(uv.r("p (a b) -> p a b", b=nb * 128), wu_d[e * D:(e + 1) * D, c0:c0 + nb * 128].rearrange("(a p) c -> p a c", p=128))
                        for j in range(nb):
                            fc = fl + j
                            par = fc % 2
                            g0, u0, xb = pb[3 * par], pb[3 * par + 1], pb[3 * par + 2]
                            wgs = lambda kc: ws.c(kc * nb * 128 + j * 128, kc * nb * 128 + (j + 1) * 128)
                            wus = lambda kc: ws.c(KC * 256 + kc * nb * 128 + j * 128, KC * 256 + kc * nb * 128 + (j + 1) * 128)
                            for kc in range(KC):
                                mm(g0.c(0, 512), wgs(kc), hTe.c(kc * CAP, kc * CAP + 512), kc == 0, kc == KC - 1)
                            for kc in range(KC):
                                mm(xb.c(0, 128), wgs(kc), hTe.c(kc * CAP + 512, (kc + 1) * CAP), kc == 0, kc == KC - 1)
                            for kc in range(KC):
                                mm(u0.c(0, 512), wus(kc), hTe.c(kc * CAP, kc * CAP + 512), kc == 0, kc == KC - 1)
                            for kc in range(KC):
                                mm(xb.c(128, 256), wus(kc), hTe.c(kc * CAP + 512, (kc + 1) * CAP), kc == 0, kc == KC - 1)
                            s0, s1 = sg.c(0, 512), sg.c(512, CAP)
                            P.op('act', lambda h, s0=s0, g0=g0: h.activation(out=s0.ap, in_=g0.t[:, 0:512], func=AF.Silu),
                                 reads=[g0.c(0, 512)], writes=[s0])
                            P.op('act', lambda h, s1=s1, xb=xb: h.activation(out=s1.ap, in_=xb.t[:, 0:128], func=AF.Silu),
                                 reads=[xb.c(0, 128)], writes=[s1])
                            a0 = actq.c(fc * CAP, fc * CAP + 512)
                            a1 = actq.c(fc * CAP + 512, (fc + 1) * CAP)
                            P.op('dve', lambda h, a0=a0, s0=s0, u0=u0: h.tensor_tensor(out=a0.ap, in0=u0.t[:, 0:512], in1=s0.ap, op=ALU.mult),
                                 reads=[u0.c(0, 512), s0], writes=[a0])
                            P.op('dve', lambda h, a1=a1, s1=s1, xb=xb: h.tensor_tensor(out=a1.ap, in0=xb.t[:, 128:256], in1=s1.ap, op=ALU.mult),
                                 reads=[xb.c(0, 256), s1], writes=[a1])
                        fl += nb
                    for dg in range(4):
                        ws = wslot()
                        r0 = e * FF + f0 * 128
                        load_w(ws.c(0, PCH * 512).r("p (a b) -> p a b", b=512),
                               wd_d[r0:r0 + PCH * 128, dg * 512:(dg + 1) * 512].rearrange("(a p) c -> p a c", p=128))
                        for s5 in range(5):
                            bank = pb[4 + (dg * 5 + s5) % 3]
                            for q in range(PCH):
                                mm(bank.c(0, 512), actq.c(q * CAP + s5 * 128, q * CAP + (s5 + 1) * 128), ws.c(q * 512, (q + 1) * 512),
                                   q == 0, q == PCH - 1)
                            yv = yacc.c(s5 * D + dg * 512, s5 * D + (dg + 1) * 512)
                            if qi == 0:
                                evac_copy(yv, bank.c(0, 512))
                            elif qi < NPART - 1:
                                P.op('dve', lambda h, yv=yv, bank=bank: h.tensor_tensor(out=yv.ap, in0=bank.t[:, 0:512], in1=yv.ap, op=ALU.add),
                                     reads=[bank.c(0, 512), yv], writes=[yv])
                            else:
                                ybv = yb.c(s5 * D + dg * 512, s5 * D + (dg + 1) * 512)
                                P.op('dve', lambda h, yv=yv, ybv=ybv, bank=bank: h.tensor_tensor(out=ybv.ap, in0=bank.t[:, 0:512], in1=yv.ap, op=ALU.add),
                                     reads=[bank.c(0, 512), yv], writes=[ybv])
                def prep(tt):
                    for s5 in range(5):
                        tr(pbb.c(s5 * 128, (s5 + 1) * 128), Pe.c(tt * CAP + s5 * 128, tt * CAP + (s5 + 1) * 128), cst_identb.c(0, 128))
                    ptv = PTs[tt % 2].c(0, CAP)
                    P.op('act', lambda h, ptv=ptv: h.activation(out=ptv.ap, in_=pbb.t[:, 0:CAP], func=AF.Copy),
                         reads=[pbb.c(0, CAP)], writes=[ptv])
                prep(0)
                for tt in range(NT):
                    if tt + 1 < NT:
                        prep(tt + 1)
                    PTc = PTs[tt % 2]
                    wv = wgt_all.c(tt * NEXP + e, tt * NEXP + e + 1)
                    for dg in range(4):
                        bank = pb[dg]
                        for s5 in range(5):
                            mm(bank.c(0, 512), PTc.c(s5 * 128, (s5 + 1) * 128), yb.c(s5 * D + dg * 512, s5 * D + (dg + 1) * 512),
                               s5 == 0, s5 == 4)
                        cvw = contrib[dg].c(0, 512)
                        P.op('dve', lambda h, cvw=cvw, bank=bank, wv=wv: h.tensor_scalar(out=cvw.ap, in0=bank.t[:, 0:512], scalar1=wv.ap, scalar2=None, op0=ALU.mult),
                             reads=[bank.c(0, 512), wv], writes=[cvw])
                        lo, hi = xo_cells(tt, dg)
                        P.dma('pool', lambda h, cvw=cvw, tt=tt, dg=dg: h.dma_start(
                            out=x_out[tt * 128:(tt + 1) * 128, dg * 512:(dg + 1) * 512], in_=cvw.ap, accum_op=ALU.add),
                            reads=[cvw], writes=[V(XO, None, lo, hi)])

        def moe_final():
            load('sp', fgb.c(0, D), fing.partition_broadcast(128))
            for tt in range(NT):
                xv = xt[tt % ST].c(0, D)
                lo, hi = xo_cells(tt)
                P.dma('sp', lambda h, xv=xv, tt=tt: h.dma_start(out=xv.ap, in_=x_out[tt * 128:(tt + 1) * 128, :]),
                      reads=[V(XO, None, lo, hi)], writes=[xv])
                ssq = stat.c(0, 1)
                rs = stat.c(1, 2)
                hsv = A("otmp", f32=True)
                for hf in range(2):
                    sq = stat.c(2 + hf, 3 + hf)
                    xh = xt[tt % ST].c(hf * 1024, (hf + 1) * 1024)
                    P.op('act', lambda h, xh=xh, sq=sq: h.activation(out=hsv.ap, in_=xh.ap, func=AF.Square, accum_out=sq.ap),
                         reads=[xh], writes=[hsv, sq])
                P.op('dve', lambda h: h.tensor_tensor(out=ssq.ap, in0=stat.t[:, 2:3], in1=stat.t[:, 3:4], op=ALU.add),
                     reads=[stat.c(2, 4)], writes=[ssq])
                rstd_from_ssq(ssq, D, rs)
                P.op('dve', lambda h, xv=xv: h.scalar_tensor_tensor(out=xv.ap, in0=xv.ap, scalar=rs.ap, in1=fgb.t[:, :], op0=ALU.mult, op1=ALU.mult),
                     reads=[xv, rs, fgb.c(0, D)], writes=[xv])
                out_sems.append(P.dma('sp', lambda h, xv=xv, tt=tt: h.dma_start(out=x_out[tt * 128:(tt + 1) * 128, :], in_=xv.ap),
                                      reads=[xv], writes=[V(XO, None, lo, hi)]))

        load('sp', cst_identf.c(0, 128), ident_f_d[:, :])
        load('sp', cst_identb.c(0, 128), ident_b_d[:, :])
        load('sp', cst_trin.c(0, 128), trin_d[:, :])
        load('sp', g_mix.c(0, KC), mixg[:, :])
        P.op('pool', lambda h: h.memset(glT.t[:, :], 1.0), writes=[glT.c(0, 512)])
        load_w(wgu_a.c(0, 512, 0, 16), wgu[:, :])
        load_w(wgu_a.c(0, 512, 16, 17), bgate[:, :])
        P.op('pool', lambda h: h.memset(bsum.t[:, :], 0.0), writes=[bsum.c(0, 4)])
        if phase == 1:
            P.op('pool', lambda h: h.memset(Sf.t[:, :], 0.0), writes=[Sf.c(0, 1024)])
            P.op('pool', lambda h: h.memset(Sb.t[:, :], 0.0), writes=[Sb.c(0, 1024)])
        else:
            load('sp', cst_mask4.c(0, 512), mask4_d[:, :])
            load('sp', cst_mcur.c(0, 512), mcur_d[:, :])
            load('sp', cst_mcur0.c(0, 512), mcur0_d[:, :])
            load('sp', cst_mprev.c(0, 512), mprev_d[:, :])
            load('sp', cmask.c(0, 8), cmask_d[:, :])
            load('sp', g_ffn.c(0, KC), ffng[:, :])
            load('sp', psc.c(0, 8), pscale[:, :])
            load('sp', gnb.c(0, 1024), gnorm.partition_broadcast(128))
            load('sp', uprev.c(0, 1024), uhalo[:, :])
            load_w(wpool.c(0, 2048).r("p (a b) -> p a b", b=256), w_pool.rearrange("(a p) d -> p a d", p=128))
            if moe:
                load('sp', wr.c(0, KC * NEXP).r("p (a b) -> p a b", b=NEXP), w_router.rearrange("(a p) e -> p a e", p=128))
                load('sp', cst_ustrict.c(0, 128), ustrict_d[:, :])
                load('sp', cst_ones.c(0, 128), ones_d[:, :])
                P.op('pool', lambda h: h.memset(carry.t[:, :], 0.0), writes=[carry.c(0, NEXP)])
            P.op('pool', lambda h: h.memset(Sf.t[:, :], 0.0), writes=[Sf.c(0, 1024)])
            Lb = A("otmp", f32=True)
            bj = stat.c(8, 12)
            aj = stat.c(12, 16)
            for j in range(NCORES):
                load('sp', Lb, lall[j * 128:(j + 1) * 128, :])
                load('sp', bj, ball[j * 128:(j + 1) * 128, :])
                mj = cmask.c(j, j + 1)
                P.op('dve', lambda h, mj=mj: h.tensor_scalar(out=aj.ap, in0=bj.ap, scalar1=mj.ap, scalar2=None, op0=ALU.mult),
                     reads=[bj, mj], writes=[aj])
                P.op('act', lambda h: h.activation(out=aj.ap, in_=aj.ap, func=AF.Exp), reads=[aj], writes=[aj])
                P.op('dve', lambda h, mj=mj: h.tensor_scalar(out=Lb.ap, in0=Lb.ap, scalar1=mj.ap, scalar2=None, op0=ALU.mult),
                     reads=[Lb, mj], writes=[Lb])
                for hh in range(4):
                    sv = Sf.c(hh * 256, (hh + 1) * 256)
                    lv = V(arena, Lb.ap[:, hh * 256:(hh + 1) * 256], Lb.lo, Lb.hi)
                    av = stat.c(12 + hh, 13 + hh)
                    P.op('dve', lambda h, sv=sv, lv=lv, av=av: h.scalar_tensor_tensor(
                        out=sv.ap, in0=sv.ap, scalar=av.ap, in1=lv.ap, op0=ALU.mult, op1=ALU.add),
                        reads=[sv, lv, av], writes=[sv])
            P.op('dve', lambda h: h.tensor_copy(out=Sb.t[:, :], in_=Sf.t[:, :]), reads=[Sf.c(0, 1024)], writes=[Sb.c(0, 1024)])

        out_sems = []
        zoff = AO["z"][0]

        def Z(ti, lo, hi):
            return arena.c(zoff + ti * 3072 + lo, zoff + ti * 3072 + hi)

        for st in range(NST):
            for ti in range(ST):
                g = st * ST + ti
                load('sp', xt[ti].c(0, D), x_in[g * 128:(g + 1) * 128, :])
                norm_tile(xt[ti], g_mix, ti)

            if STOP <= 1:
                continue
            blocks = [('u', 0, 512, 0), ('u', 512, 512, 512), ('q', 1024, 512, 0), ('k', 1536, 512, 0),
                      ('v', 2048, 512, 1024), ('v', 2560, 512, 1536), ('g', 3072, 16, 0),
                      ('r', 3088, 512, 2048), ('r', 3600, 512, 2560)]
            for kind, c0, ncol, zc in blocks:
                if phase == 1 and (kind in ('q', 'r') or (kind == 'u' and st != NST - 1)):
                    continue
                ws = wslot()
                wv = ws.c(0, KC * ncol)
                load_w(wv.r("p (a b) -> p a b", b=ncol), w_in[:, c0:c0 + ncol].rearrange("(a p) c -> p a c", p=128))
                if kind in ('u', 'v', 'r'):
                    for ti in range(ST):
                        bank = pb[ti % 4]
                        for kc in range(KC):
                            mm(bank.c(0, 512), hT.c(kc * 512 + ti * 128, kc * 512 + (ti + 1) * 128),
                               ws.c(kc * 512, (kc + 1) * 512), kc == 0, kc == KC - 1)
                        evac_copy(Z(ti, zc, zc + 512), bank.c(0, 512))
                elif kind in ('q', 'k'):
                    for hh in range(4):
                        bank = pb[hh]
                        for kc in range(KC):
                            mm(bank.c(0, 512), ws.c(kc * 512 + hh * 128, kc * 512 + (hh + 1) * 128),
                               hT.c(kc * 512, (kc + 1) * 512), kc == 0, kc == KC - 1)
                        dst = A("qT" if kind == 'q' else "kT").r("p (a b c) -> p a b c", a=ST, b=4).s(slice(None), slice(None), hh, slice(None))
                        evac_copy(dst, bank.c(0, 512).r("p (a c) -> p a c", a=ST))
                else:
                    bank = pb[4]
                    for kc in range(KC):
                        mm(bank.c(0, 512, 0, 16), ws.c(kc * 16, (kc + 1) * 16), hT.c(kc * 512, (kc + 1) * 512),
                           kc == 0, kc == KC - 1)
                    evac_copy(glT.c(0, 512, 0, 16), bank.c(0, 512, 0, 16))

            if STOP <= 2:
                continue
            for ti in range(ST):
                g = st * ST + ti
                kT_t = A("kT", ti * 512, (ti + 1) * 512)
                mm(pb[5].c(0, 512), glT.c(ti * 128, (ti + 1) * 128, 0, 17), wgu_a.c(0, 512, 0, 17), True, True)
                ysp = A("ysp", f32=True)
                spv = A("sp")
                P.op('act', lambda h, ysp=ysp: h.activation(out=ysp.ap, in_=pb[5].t[:, 0:512], func=AF.Exp, scale=-1.0),
                     reads=[pb[5].c(0, 512)], writes=[ysp])
                P.op('act', lambda h, ysp=ysp, spv=spv: h.activation(out=spv.ap, in_=ysp.ap, func=AF.Ln, bias=1.0),
                     reads=[ysp], writes=[spv])
                if STOP <= 2.2:
                    continue
                for hh in range(4):
                    mm(pb[6].c(hh * 128, (hh + 1) * 128), V(arena, spv.ap[:, hh * 128:(hh + 1) * 128], spv.lo, spv.hi),
                       cst_trin.c(0, 128), True, True)
                epos = A("epos", f32=True)
                eneg = A("eneg", f32=True)
                bcv = pb[6].c(0, 512)
                P.op('act', lambda h, epos=epos: h.activation(out=epos.ap, in_=pb[6].t[:, 0:512], func=AF.Exp),
                     reads=[bcv], writes=[epos])
                P.op('act', lambda h, eneg=eneg: h.activation(out=eneg.ap, in_=pb[6].t[:, 0:512], func=AF.Exp, scale=-1.0),
                     reads=[bcv], writes=[eneg])
                if STOP <= 2.3:
                    continue
                bl = V(pb[6], pb[6].t[:, 0:512].rearrange("p (a b) -> p a b", b=128)[:, :, 127], 0, 2048)
                P.op('dve', lambda h, bl=bl: h.tensor_tensor(out=bsum.t[:, :], in0=bl.ap, in1=bsum.t[:, :], op=ALU.add),
                     reads=[bsum.c(0, 4), bl], writes=[bsum.c(0, 4)])
                if STOP <= 2.4:
                    continue
                kd = A("kd")
                P.op('dve', lambda h, kd=kd, kT_t=kT_t, eneg=eneg: h.tensor_tensor(out=kd.ap, in0=kT_t.ap, in1=eneg.ap, op=ALU.mult),
                     reads=[kT_t, eneg], writes=[kd])
                if phase == 2:
                    qT_t = A("qT", ti * 512, (ti + 1) * 512)
                    qd = A("qd")
                    P.op('dve', lambda h, qd=qd, qT_t=qT_t, epos=epos: h.scalar_tensor_tensor(
                        out=qd.ap, in0=qT_t.ap, scalar=float(128 ** -0.5), in1=epos.ap, op0=ALU.mult, op1=ALU.mult),
                        reads=[qT_t, epos], writes=[qd])
                    for hh in range(4):
                        mm(pb[4].c(hh * 128, (hh + 1) * 128), V(arena, kd.ap[:, hh * 128:(hh + 1) * 128], kd.lo, kd.hi),
                           V(arena, qd.ap[:, hh * 128:(hh + 1) * 128], qd.lo, qd.hi), True, True)
                    att = A("att")
                    P.op('dve', lambda h, att=att: h.tensor_tensor(out=att.ap, in0=pb[4].t[:, 0:512], in1=cst_mask4.t[:, :], op=ALU.mult),
                         reads=[pb[4].c(0, 512), cst_mask4.c(0, 512)], writes=[att])
                    for hh in range(4):
                        ob = pb[hh // 2].c((hh % 2) * 256, (hh % 2) * 256 + 256)
                        mm(ob, V(arena, qd.ap[:, hh * 128:(hh + 1) * 128], qd.lo, qd.hi), Sb.c(hh * 256, (hh + 1) * 256), True, False)
                        mm(ob, V(arena, att.ap[:, hh * 128:(hh + 1) * 128], att.lo, att.hi), Z(ti, 1024 + hh * 256, 1024 + (hh + 1) * 256), False, True)
                kteT = A("kteT")
                elb = V(arena, epos.ap.rearrange("p (a b) -> p a b", b=128)[:, :, 127:128].to_broadcast([128, 4, 128]), epos.lo, epos.hi)
                P.op('dve', lambda h, kteT=kteT, kd=kd, elb=elb: h.tensor_tensor(
                    out=kteT.ap.rearrange("p (a b) -> p a b", b=128), in0=kd.ap.rearrange("p (a b) -> p a b", b=128), in1=elb.ap, op=ALU.mult),
                    reads=[kd, elb], writes=[kteT])
                if STOP <= 2.6:
                    continue
                for hh in range(4):
                    tr(pbb.c(hh * 128, (hh + 1) * 128), V(arena, kteT.ap[:, hh * 128:(hh + 1) * 128], kteT.lo, kteT.hi), cst_identb.c(0, 128))
                kte = A("kte")
                P.op('act', lambda h, kte=kte: h.activation(out=kte.ap, in_=pbb.t[:, 0:512], func=AF.Copy),
                     reads=[pbb.c(0, 512)], writes=[kte])
                if STOP <= 2.8:
                    continue
                for hh in range(4):
                    cb = pb[2 + hh // 2].c((hh % 2) * 256, (hh % 2) * 256 + 256)
                    mm(cb, V(arena, kte.ap[:, hh * 128:(hh + 1) * 128], kte.lo, kte.hi), Z(ti, 1024 + hh * 256, 1024 + (hh + 1) * 256), True, True)
                elb2 = V(arena, epos.ap.rearrange("p (a b) -> p a b", b=128)[:, :, 127:128].to_broadcast([128, 4, 256]), epos.lo, epos.hi)
                sall = Sf.c(0, 1024)
                P.op('dve', lambda h, elb2=elb2: h.tensor_tensor(out=Sf.t[:, :].rearrange("p (a b) -> p a b", b=256),
                                                                 in0=Sf.t[:, :].rearrange("p (a b) -> p a b", b=256), in1=elb2.ap, op=ALU.mult),
                     reads=[sall, elb2], writes=[sall])
                for half in range(2):
                    cbk = pb[2 + half].c(0, 512)
                    sv = Sf.c(half * 512, (half + 1) * 512)
                    P.op('dve', lambda h, sv=sv, half=half: h.tensor_tensor(out=sv.ap, in0=pb[2 + half].t[:, 0:512], in1=sv.ap, op=ALU.add),
                         reads=[sv, cbk], writes=[sv])
                P.op('dve', lambda h: h.tensor_copy(out=Sb.t[:, :], in_=Sf.t[:, :]), reads=[Sf.c(0, 1024)], writes=[Sb.c(0, 1024)])

                if phase == 1:
                    continue

                otmp = A("otmp", f32=True)
                ssq4 = stat.c(16, 20)
                rs4 = stat.c(20, 24)
                for hh in range(4):
                    ob = pb[hh // 2].c((hh % 2) * 256, (hh % 2) * 256 + 256)
                    sq = stat.c(16 + hh, 17 + hh)
                    ot = V(arena, otmp.ap[:, hh * 256:(hh + 1) * 256], otmp.lo, otmp.hi)
                    P.op('act', lambda h, ob=ob, sq=sq, ot=ot: h.activation(out=ot.ap, in_=ob.ap, func=AF.Square, accum_out=sq.ap),
                         reads=[ob], writes=[ot, sq])
                rstd_from_ssq(ssq4, 256, rs4)
                for hh in range(4):
                    ob = pb[hh // 2].c((hh % 2) * 256, (hh % 2) * 256 + 256)
                    rv = stat.c(20 + hh, 21 + hh)
                    ot = V(arena, otmp.ap[:, hh * 256:(hh + 1) * 256], otmp.lo, otmp.hi)
                    gv = gnb.c(hh * 256, (hh + 1) * 256)
                    P.op('dve', lambda h, ob=ob, rv=rv, ot=ot, gv=gv: h.scalar_tensor_tensor(
                        out=ot.ap, in0=ob.ap, scalar=rv.ap, in1=gv.ap, op0=ALU.mult, op1=ALU.mult),
                        reads=[ob, rv, gv], writes=[ot])
                sr = A("sr")
                rz = Z(ti, 2048, 3072)
                P.op('act', lambda h, sr=sr, rz=rz: h.activation(out=sr.ap, in_=rz.ap, func=AF.Silu), reads=[rz], writes=[sr])
                og = A("og")
                P.op('dve', lambda h, og=og, otmp=otmp, sr=sr: h.tensor_tensor(out=og.ap, in0=otmp.ap, in1=sr.ap, op=ALU.mult),
                     reads=[otmp, sr], writes=[og])
                for c in range(8):
                    tr(pbb.c(c * 128, (c + 1) * 128), V(arena, og.ap[:, c * 128:(c + 1) * 128], og.lo, og.hi), cst_identb.c(0, 128))
                for c in range(8):
                    evac_copy(mixT.c((8 + c) * 512 + ti * 128, (8 + c) * 512 + (ti + 1) * 128), pbb.c(c * 128, (c + 1) * 128))

                ucur = Z(ti, 0, 1024)
                mc = cst_mcur0 if g == 0 else cst_mcur
                for c in range(8):
                    gi = c // 2
                    ob = pb[4 + c // 4].c((c % 4) * 128, (c % 4 + 1) * 128)
                    mm(ob, uprev.c(c * 128, (c + 1) * 128), cst_mprev.c(gi * 128, (gi + 1) * 128), True, False)
                    mm(ob, V(arena, ucur.ap[:, c * 128:(c + 1) * 128], ucur.lo, ucur.hi), mc.c(gi * 128, (gi + 1) * 128), False, True)
                dT = A("dT")
                evac_copy(V(arena, dT.ap[:, 0:512], dT.lo, dT.hi), pb[4].c(0, 512))
                evac_copy(V(arena, dT.ap[:, 512:1024], dT.lo, dT.hi), pb[5].c(0, 512))
                for oc in range(8):
                    gi, jj = oc // 2, oc % 2
                    ob = pb[4 + oc // 4].c((oc % 4) * 128, (oc % 4 + 1) * 128)
                    for ci in range(2):
                        wv = wpool.c((gi * 2 + ci) * 256 + jj * 128, (gi * 2 + ci) * 256 + (jj + 1) * 128)
                        mm(ob, wv, V(arena, dT.ap[:, (gi * 2 + ci) * 128:(gi * 2 + ci + 1) * 128], dT.lo, dT.hi), ci == 0, ci == 1)
                for oc in range(8):
                    ob = pb[4 + oc // 4].c((oc % 4) * 128, (oc % 4 + 1) * 128)
                    evac_copy(mixT.c(oc * 512 + ti * 128, oc * 512 + (ti + 1) * 128), ob, psc.c(oc, oc + 1))
                P.op('pool', lambda h, ucur=ucur: h.tensor_copy(out=uprev.t[:, :], in_=ucur.ap), reads=[ucur], writes=[uprev.c(0, 1024)])

            if phase == 1:
                continue

            for cb in range(4):
                ws = wslot()
                load_w(ws.c(0, KC * 512).r("p (a b) -> p a b", b=512), w_out[:, cb * 512:(cb + 1) * 512].rearrange("(a p) c -> p a c", p=128))
                for ti in range(ST):
                    bank = pb[ti]
                    for kc in range(KC):
                        mm(bank.c(0, 512), mixT.c(kc * 512 + ti * 128, kc * 512 + (ti + 1) * 128), ws.c(kc * 512, (kc + 1) * 512),
                           kc == 0, kc == KC - 1)
                    xv = xt[ti].c(cb * 512, (cb + 1) * 512)
                    P.op('dve', lambda h, xv=xv, bank=bank: h.tensor_tensor(out=xv.ap, in0=bank.t[:, 0:512], in1=xv.ap, op=ALU.add),
                         reads=[bank.c(0, 512), xv], writes=[xv])

            if moe:
                for ti in range(ST):
                    moe_route_tile(ti, st * ST + ti)
                continue

            for ti in range(ST):
                norm_tile(xt[ti], g_ffn, ti)

            for e in range(NE):
                for half in range(2):
                    f0 = half * HALF
                    for fb in range(HALF // 2):
                        ws = wslot()
                        c0 = (f0 + fb * 2) * 128
                        gv = ws.c(0, KC * 256)
                        uv = ws.c(KC * 256, 2 * KC * 256)
                        load_w(gv.r("p (a b) -> p a b", b=256), wg_d[e * D:(e + 1) * D, c0:c0 + 256].rearrange("(a p) c -> p a c", p=128))
                        load_w(uv.r("p (a b) -> p a b", b=256), wu_d[e * D:(e + 1) * D, c0:c0 + 256].rearrange("(a p) c -> p a c", p=128))
                        for j in range(2):
                            fc = fb * 2 + j
                            gb_ = pb[(fc % 2) * 2]
                            ub_ = pb[(fc % 2) * 2 + 1]
                            for kc in range(KC):
                                mm(gb_.c(0, 512), ws.c(kc * 256 + j * 128, kc * 256 + (j + 1) * 128), hT.c(kc * 512, (kc + 1) * 512),
                                   kc == 0, kc == KC - 1)
                            for kc in range(KC):
                                mm(ub_.c(0, 512), ws.c(KC * 256 + kc * 256 + j * 128, KC * 256 + kc * 256 + (j + 1) * 128),
                                   hT.c(kc * 512, (kc + 1) * 512), kc == 0, kc == KC - 1)
                            sg = hs.c((fc % 2) * 512, (fc % 2) * 512 + 512)
                            P.op('act', lambda h, sg=sg, gb_=gb_: h.activation(out=sg.ap, in_=gb_.t[:, 0:512], func=AF.Silu),
                                 reads=[gb_.c(0, 512)], writes=[sg])
                            av = ACT_(fc)
                            P.op('dve', lambda h, av=av, sg=sg, ub_=ub_: h.tensor_tensor(out=av.ap, in0=ub_.t[:, 0:512], in1=sg.ap, op=ALU.mult),
                                 reads=[ub_.c(0, 512), sg], writes=[av])
                    PIECE = 11 if not moe else 14
                    for cb in range(4):
                        for pi in range(HALF // PIECE):
                            ws = wslot()
                            r0 = e * FF + (f0 + pi * PIECE) * 128
                            load_w(ws.c(0, PIECE * 512).r("p (a b) -> p a b", b=512),
                                   wd_d[r0:r0 + PIECE * 128, cb * 512:(cb + 1) * 512].rearrange("(a p) c -> p a c", p=128))
                            for ti in range(ST):
                                bank = pb[3 + ti]
                                for q in range(PIECE):
                                    fc = pi * PIECE + q
                                    mm(bank.c(0, 512), ACT_(fc, ti * 128, (ti + 1) * 128), ws.c(q * 512, (q + 1) * 512),
                                       fc == 0, fc == HALF - 1)
                        for ti in range(ST):
                            bank = pb[3 + ti]
                            xv = xt[ti].c(cb * 512, (cb + 1) * 512)
                            if moe:
                                wv = wgt.c(ti * NEXP + e, ti * NEXP + e + 1)
                                P.op('dve', lambda h, xv=xv, bank=bank, wv=wv: h.scalar_tensor_tensor(
                                    out=xv.ap, in0=bank.t[:, 0:512], scalar=wv.ap, in1=xv.ap, op0=ALU.mult, op1=ALU.add),
                                    reads=[bank.c(0, 512), wv, xv], writes=[xv])
                            else:
                                P.op('dve', lambda h, xv=xv, bank=bank: h.tensor_tensor(out=xv.ap, in0=bank.t[:, 0:512], in1=xv.ap, op=ALU.add),
                                     reads=[bank.c(0, 512), xv], writes=[xv])

            if final:
                load('sp', fgb.c(0, D), fing.partition_broadcast(128))
            for ti in range(ST):
                g = st * ST + ti
                xv = xt[ti].c(0, D)
                if final:
                    ssq = stat.c(0, 1)
                    rs = stat.c(1, 2)
                    hsv = hs.c(0, D)
                    P.op('act', lambda h, xv=xv: h.activation(out=hsv.ap, in_=xv.ap, func=AF.Square, accum_out=ssq.ap),
                         reads=[xv], writes=[hsv, ssq])
                    rstd_from_ssq(ssq, D, rs)
                    P.op('dve', lambda h, xv=xv: h.scalar_tensor_tensor(out=xv.ap, in0=xv.ap, scalar=rs.ap, in1=fgb.t[:, :], op0=ALU.mult, op1=ALU.mult),
                         reads=[xv, rs, fgb.c(0, D)], writes=[xv])
                out_sems.append(P.dma('sp', lambda h, xv=xv, g=g: h.dma_start(out=x_out[g * 128:(g + 1) * 128, :], in_=xv.ap), reads=[xv]))

        if moe and phase == 2:
            moe_experts()
            moe_final()
        if phase == 1:
            out_sems.append(P.dma('sp', lambda h: h.dma_start(out=lst_o[:, :], in_=Sf.t[:, :]), reads=[Sf.c(0, 1024)]))
            out_sems.append(P.dma('sp', lambda h: h.dma_start(out=bsum_o[:, :], in_=bsum.t[:, :]), reads=[bsum.c(0, 4)]))
            lastu = Z(ST - 1, 0, 1024)
            out_sems.append(P.dma('sp', lambda h: h.dma_start(out=ulast_o[:, :], in_=lastu.ap), reads=[lastu]))
        fin = {}
        for s, v in out_sems:
            fin[id(s)] = (s, max(v, fin.get(id(s), (s, 0))[1]))
        P.wait_all('sp', list(fin.values()))

        with nc.Block() as block:
            @block.tensor
            def _(h):
                P.emit_engine('pe', h)

            @block.scalar
            def _(h):
                P.emit_engine('act', h)

            @block.vector
            def _(h):
                P.emit_engine('dve', h)

            @block.gpsimd
            def _(h):
                P.emit_engine('pool', h)

            @block.sync
            def _(h):
                P.emit_engine('sp', h)
    return nc


_PROG_CACHE = {}


def _get(kind, phase, final):
    key = (kind, phase, final)
    if key not in _PROG_CACHE:
        _PROG_CACHE[key] = build(kind, phase, final)
    return _PROG_CACHE[key]


def kernel(x, mix_norm, w_in, w_pool, pool_scale, w_gate_up, b_gate, gla_norm, w_out,
           ffn_norm, dense_w_gate, dense_w_up, dense_w_down, w_router,
           exp_w_gate, exp_w_up, exp_w_down, final_norm, _nlayers=2):
    f = lambda a: np.ascontiguousarray(np.asarray(a, dtype=np.float32))
    x = f(x).reshape(SEQ, D)
    consts = [_consts(c) for c in range(NCORES)]
    xs = [np.ascontiguousarray(x[c * TPC:(c + 1) * TPC]) for c in range(NCORES)]
    bf = ml_dtypes.bfloat16
    for l in range(_nlayers):
        kind = 'dense' if l % 2 == 0 else 'moe'
        common = {
            "mixg": _fm(f(mix_norm[l])), "w_in": f(w_in[l]), "wgu": f(w_gate_up[l]),
            "bgate": f(b_gate[l]).reshape(1, 512),
        }
        nc1 = _get(kind, 1, False)
        maps = []
        for c in range(NCORES):
            m = dict(common)
            m["x_in"] = xs[c]
            for k in ("ident_f", "ident_b", "trin"):
                m[k] = consts[c][k]
            maps.append(m)
        r1 = run_bass_kernel_spmd(nc1, maps, core_ids=list(range(NCORES))).results
        lall = np.concatenate([np.asarray(r["lst"]) for r in r1], axis=0)
        ball = np.concatenate([np.asarray(r["bsum"]) for r in r1], axis=0)
        final = l == 1
        nc2 = _get(kind, 2, final)
        maps = []
        for c in range(NCORES):
            m = dict(common)
            m["x_in"] = xs[c]
            m.update(consts[c])
            m["lall"] = lall
            m["ball"] = ball
            m["uhalo"] = np.asarray(r1[c - 1]["ulast"]) if c > 0 else np.zeros((128, 1024), bf)
            m["w_pool"] = f(w_pool[l]).reshape(4 * 256, 256)
            m["pscale"] = _fm(f(pool_scale[l]).reshape(-1))
            m["gnorm"] = f(gla_norm[l]).reshape(1, 1024)
            m["w_out"] = f(w_out[l])
            m["ffng"] = _fm(f(ffn_norm[l]))
            if kind == 'moe':
                i = l // 2
                m["w_router"] = f(w_router[i])
                m["wg"] = f(exp_w_gate[i]).reshape(NEXP * D, FF_EXP)
                m["wu"] = f(exp_w_up[i]).reshape(NEXP * D, FF_EXP)
                m["wd"] = f(exp_w_down[i]).reshape(NEXP * FF_EXP, D)
            else:
                i = l // 2
                m["wg"] = f(dense_w_gate[i])
                m["wu"] = f(dense_w_up[i])
                m["wd"] = f(dense_w_down[i])
            if final:
                m["fing"] = f(final_norm).reshape(1, D)
            maps.append(m)
        r2 = run_bass_kernel_spmd(nc2, maps, core_ids=list(range(NCORES))).results
        xs = [np.asarray(r["x_out"]) for r in r2]
    out = np.concatenate(xs, axis=0).reshape(1, SEQ, D).astype(np.float32)
    return out
```

```python
import numpy as np
import ml_dtypes
import concourse.bass as bass
import concourse.mybir as mybir
from concourse.bass_utils import run_bass_kernel_spmd

F32 = mybir.dt.float32
BF16 = mybir.dt.bfloat16
AF = mybir.ActivationFunctionType
ALU = mybir.AluOpType
AX = mybir.AxisListType

NCORES = 8
D = 2048
SEQ = 16384
TPC = SEQ // NCORES
NT = TPC // 128
ST = 4
NST = NT // ST
KC = D // 128
INW = 4112
FF_DENSE = 5632
FF_EXP = 7168
NEXP = 8
EPS = 1e-6
POOL_WINDOWS = (2, 4, 8, 16)
WSLOT = 8192
NWSLOT = 3
STOP = 99
EVAC_ACT_SCALE = True


class TT:
    def __init__(self, t, esz, name, holder=None, base=0):
        self.t = t
        self.esz = esz
        self.name = name
        self.H = holder if holder is not None else [[]]
        self.base = base
        self.whole = False

    @property
    def hist(self):
        return self.H[0]

    @hist.setter
    def hist(self, v):
        self.H[0] = v

    def c(self, lo, hi, p0=0, p1=None):
        ap = self.t[p0:p1, lo:hi] if p1 is not None else (self.t[p0:, lo:hi] if p0 else self.t[:, lo:hi])
        if self.whole:
            return V(self, ap, 0, 1 << 20)
        return V(self, ap, self.base + lo * self.esz, self.base + hi * self.esz)

    def view(self, lo_bytes, n, dt, name=None):
        esz = 4 if dt == F32 else 2
        a = lo_bytes // self.esz
        b = a + (n * esz) // self.esz
        ap = self.t[:, a:b]
        if esz != self.esz or dt != getattr(self, "dt", None):
            ap = ap.bitcast(dt)
        tt = TT(ap, esz, name or self.name, holder=self.H, base=self.base + lo_bytes)
        tt.dt = dt
        return tt


class V:
    def __init__(self, tt, ap, lo, hi):
        self.tt = tt
        self.ap = ap
        self.lo = lo
        self.hi = hi

    def r(self, pat, **kw):
        return V(self.tt, self.ap.rearrange(pat, **kw), self.lo, self.hi)

    def s(self, *key):
        return V(self.tt, self.ap[key], self.lo, self.hi)


class Eng:
    def __init__(self, key, sem):
        self.key = key
        self.sem = sem
        self.count = 0
        self.known = {}
        self.ops = []
        self.lanes = []
        self.lane_val = []
        self.lane_next = 0


class Prog:
    def __init__(self, nc):
        self.nc = nc
        self.E = {}
        self.sems = {}

    def add_engine(self, key, sem, lanes=()):
        e = Eng(key, sem)
        self.sems[id(sem)] = sem
        e.lanes = list(lanes)
        e.lane_val = [0] * len(lanes)
        for s in lanes:
            self.sems[id(s)] = s
        self.E[key] = e

    def _need(self, e, reads, writes):
        need = {}

        def add(h):
            if h[2] == 'pe' and e.key == 'pe' and h[3] == id(e.sem):
                return
            if need.get(h[3], 0) < h[4]:
                need[h[3]] = h[4]
        for v in reads:
            whole = v.tt.whole
            for h in v.tt.hist:
                if (h[5] or (whole and h[2] != e.key)) and h[0] < v.hi and v.lo < h[1]:
                    add(h)
        for v in writes:
            for h in v.tt.hist:
                if h[0] < v.hi and v.lo < h[1]:
                    add(h)
        waits = []
        for sid, val in need.items():
            if e.known.get(sid, 0) < val:
                e.known[sid] = val
                waits.append((self.sems[sid], val))
        return waits

    def _record(self, e, reads, writes, sid, val):
        for v in writes:
            hist = v.tt.hist
            v.tt.hist = [h for h in hist if not (h[0] >= v.lo and h[1] <= v.hi)]
            v.tt.hist.append([v.lo, v.hi, e.key, sid, val, True])
        for v in reads:
            hist = v.tt.hist
            v.tt.hist = [h for h in hist if not ((not h[5]) and h[3] == sid and h[0] >= v.lo and h[1] <= v.hi)]
            v.tt.hist.append([v.lo, v.hi, e.key, sid, val, False])

    def op(self, ek, fn, reads=(), writes=(), sig=True):
        e = self.E[ek]
        waits = self._need(e, reads, writes)
        val = e.count + 1
        if sig:
            e.count = val
        e.ops.append((waits, fn, (e.sem, 1) if sig else None))
        self._record(e, reads, writes, id(e.sem), val)

    def dma(self, ek, fn, reads=(), writes=()):
        e = self.E[ek]
        li = e.lane_next
        e.lane_next = (li + 1) % len(e.lanes)
        sem = e.lanes[li]
        waits = self._need(e, reads, writes)
        prev = e.lane_val[li]
        if prev > 0 and e.known.get(id(sem), 0) < prev:
            e.known[id(sem)] = prev
            waits.append((sem, prev))
        val = prev + 16
        e.lane_val[li] = val
        e.ops.append((waits, fn, (sem, 16)))
        self._record(e, reads, writes, id(sem), val)
        return sem, val

    def wait_all(self, ek, items):
        e = self.E[ek]
        waits = [(s, v) for (s, v) in items]
        e.ops.append((waits, None, None))

    def emit_engine(self, ek, h):
        for waits, fn, inc in self.E[ek].ops:
            for s, v in waits:
                h.wait_ge(s, v)
            if fn is None:
                continue
            ins = fn(h)
            if inc is not None:
                ins.then_inc(inc[0], inc[1])


def _pool_mats(first_core):
    cur = np.zeros((4, 128, 128), np.float32)
    cur0 = np.zeros((4, 128, 128), np.float32)
    prev = np.zeros((4, 128, 128), np.float32)
    for g, w in enumerate(POOL_WINDOWS):
        for t in range(128):
            for s in range(t - w + 1, t + 1):
                if s >= 0:
                    cur[g, s, t] += 1.0 / w
                else:
                    prev[g, 128 + s, t] += 1.0 / w
            cur[g, t, t] -= 1.0
            cnt = min(t + 1, w)
            for s in range(max(0, t - w + 1), t + 1):
                cur0[g, s, t] += 1.0 / cnt
            cur0[g, t, t] -= 1.0
    return cur, (cur0 if first_core else cur), prev


def _consts(core):
    bf = ml_dtypes.bfloat16
    c = {}
    c["ident_f"] = np.eye(128, dtype=np.float32)
    c["ident_b"] = np.eye(128, dtype=np.float32).astype(bf)
    j = np.arange(128)[:, None]
    i = np.arange(128)[None, :]
    tri = (j <= i).astype(np.float32)
    c["trin"] = (tri * (-1.0 / 16.0)).astype(bf)
    c["mask4"] = np.tile(tri, (1, 4)).astype(bf)
    cur, cur0, prev = _pool_mats(core == 0)
    c["mcur"] = np.ascontiguousarray(cur.transpose(1, 0, 2).reshape(128, 512)).astype(bf)
    c["mcur0"] = np.ascontiguousarray(cur0.transpose(1, 0, 2).reshape(128, 512)).astype(bf)
    c["mprev"] = np.ascontiguousarray(prev.transpose(1, 0, 2).reshape(128, 512)).astype(bf)
    m = np.zeros((128, 8), np.float32)
    m[:, :core] = 1.0
    c["cmask"] = m
    c["iota"] = np.tile(np.arange(640, dtype=np.float32)[None, :], (128, 1))
    c["ustrict"] = (j < i).astype(np.float32).astype(bf)
    c["ones"] = np.ones((128, 128), np.float32).astype(bf)
    return c


def _fm(vec):
    return np.ascontiguousarray(vec.reshape(-1, 128).T)


def build(layer_kind, phase, final):
    nc = bass.Bass("TRN2", target_bir_lowering=False)
    moe = layer_kind == 'moe'
    FF = FF_EXP if moe else FF_DENSE
    NE = NEXP if moe else 1
    NFC = FF // 128
    HALF = NFC // 2

    def din(name, shape, dt=F32):
        return nc.dram_tensor(name, list(shape), dt, kind="ExternalInput").ap()

    def dout(name, shape, dt=F32):
        return nc.dram_tensor(name, list(shape), dt, kind="ExternalOutput").ap()

    x_in = din("x_in", [TPC, D])
    mixg = din("mixg", [128, KC])
    w_in = din("w_in", [D, INW])
    wgu = din("wgu", [16, 512])
    bgate = din("bgate", [1, 512])
    ident_f_d = din("ident_f", [128, 128])
    ident_b_d = din("ident_b", [128, 128], BF16)
    trin_d = din("trin", [128, 128], BF16)
    if phase == 1:
        lst_o = dout("lst", [128, 1024])
        bsum_o = dout("bsum", [128, 4])
        ulast_o = dout("ulast", [128, 1024], BF16)
        kT_o = dout("kT_o", [NST * 128, 2048], BF16)
        v_o = dout("v_o", [TPC, 1024], BF16)
        gl_o = dout("gl_o", [NST * 16, 512], BF16)
    else:
        mask4_d = din("mask4", [128, 512], BF16)
        mcur_d = din("mcur", [128, 512], BF16)
        mcur0_d = din("mcur0", [128, 512], BF16)
        mprev_d = din("mprev", [128, 512], BF16)
        cmask_d = din("cmask", [128, 8])
        lall = din("lall", [NCORES * 128, 1024])
        ball = din("ball", [NCORES * 128, 4])
        uhalo = din("uhalo", [128, 1024], BF16)
        kT_i = din("kT_i", [NST * 128, 2048], BF16)
        v_i = din("v_i", [TPC, 1024], BF16)
        gl_i = din("gl_i", [NST * 16, 512], BF16)
        w_pool = din("w_pool", [4 * 256, 256])
        pscale = din("pscale", [128, 8])
        gnorm = din("gnorm", [1, 1024])
        w_out = din("w_out", [D, D])
        ffng = din("ffng", [128, KC])
        if moe:
            w_router = din("w_router", [D, NEXP])
            wg_d = din("wg", [NEXP * D, FF])
            wu_d = din("wu", [NEXP * D, FF])
            wd_d = din("wd", [NEXP * FF, D])
        else:
            wg_d = din("wg", [D, FF])
            wu_d = din("wu", [D, FF])
            wd_d = din("wd", [FF, D])
        if final:
            fing = din("fing", [1, D])
        x_out = dout("x_out", [TPC, D])
        if moe:
            iota_d = din("iota", [128, 640])
            ustrict_d = din("ustrict", [128, 128], BF16)
            ones_d = din("ones", [128, 128], BF16)
            h2s = nc.dram_tensor("h2s", [TPC, D], BF16, kind="Internal").ap()

    import contextlib
    es = contextlib.ExitStack()
    with es:
        def sb(name, n, dt=F32, p=128):
            t = es.enter_context(nc.sbuf_tensor("sb_" + name, [p, n], dt))
            tt = TT(t, 4 if dt == F32 else 2, name)
            tt.dt = dt
            return tt

        def ps(name, n, dt=F32):
            t = es.enter_context(nc.psum_tensor("ps_" + name, [128, n], dt))
            tt = TT(t, 4 if dt == F32 else 2, name)
            tt.whole = True
            return tt

        def sem(name):
            return es.enter_context(nc.semaphore(name))

        P = Prog(nc)
        P.add_engine('pe', sem("s_pe"))
        P.add_engine('act', sem("s_act"))
        P.add_engine('dve', sem("s_dve"))
        P.add_engine('pool', sem("s_pool"), [sem("lp%d" % i) for i in range(16)])
        P.add_engine('sp', sem("s_sp"), [sem("ls%d" % i) for i in range(16)])

        xtall = sb("xtall", ST * D)
        xt = [xtall.view(i * D * 4, D, F32, "xt%d" % i) for i in range(ST)]
        hsf = sb("hsf", 2 * D)
        hs = hsf.view(0, D, F32, "hs")
        mixc = sb("mixc", 5 * 1024)
        hT = sb("hT", KC * 512, BF16)
        wsl = [sb("wsl%d" % i, WSLOT, BF16) for i in range(NWSLOT)]
        arena = sb("arena", 27 * 1024, BF16)
        mixT = hT
        stat = sb("stat", 64)
        cst_identf = sb("identf", 128)
        cst_identb = sb("identb", 128, BF16)
        cst_trin = sb("trin", 128, BF16)
        g_mix = sb("g_mix", KC)
        wgu_a = sb("wgu_a", 512, BF16, p=32)
        Sf = mixc.view(0, 1024, F32, "Sf")
        Sb = mixc.view(4096, 1024, BF16, "Sb")
        glT = sb("glT", 512, BF16, p=32)
        if phase == 2:
            cst_mask4 = sb("mask4", 512, BF16)
            cst_mcur = sb("mcur", 512, BF16)
            cst_mcur0 = sb("mcur0", 512, BF16)
            cst_mprev = sb("mprev", 512, BF16)
            cmask = sb("cmask", 8)
            g_ffn = sb("g_ffn", KC)
            psc = sb("psc", 8)
            gnb = mixc.view(6144, 1024, F32, "gnb")
            wpool = mixc.view(10240, 8 * 256, BF16, "wpool")
            uprev = mixc.view(14336, 1024, BF16, "uprev")
            if moe:
                wr = sb("wr", KC * NEXP)
                hTf = hsf.view(D * 4, KC * 128, F32, "hTf")
                wgt_all = sb("wgt_all", NT * NEXP)
                mask_all = sb("mask_all", NT * NEXP)
                pos_all = sb("pos_all", NT * NEXP)
                carry = sb("carry", NEXP)
                maskb = sb("maskb", NEXP, BF16)
                cst_ustrict = sb("ustrict", 128, BF16)
                cst_ones = sb("ones", 128, BF16)
                rt = sb("rt", 64)
            if final:
                fgb = hTf if moe else sb("fgb", D)
        bsum = sb("bsum", 4)

        AO = {}
        off = 0

        def carve(name, n_bf16):
            nonlocal off
            AO[name] = (off, off + n_bf16)
            off += n_bf16
        carve("z", ST * 3072)
        carve("qT", ST * 512)
        carve("kT", ST * 512)
        carve("ysp", 1024)
        carve("sp", 512)
        carve("epos", 1024)
        carve("eneg", 1024)
        carve("qd", 512)
        carve("kd", 512)
        carve("kteT", 512)
        carve("kte", 512)
        carve("att", 512)
        carve("otmp", 2048)
        carve("sr", 1024)
        carve("og", 1024)
        carve("dT", 1024)
        assert off <= 27 * 1024, off

        def A(name, lo=0, hi=None, f32=False):
            a, b = AO[name]
            if f32:
                n = (b - a) // 2
                hi_ = n if hi is None else hi
                ap = arena.t[:, a:b].bitcast(F32)[:, lo:hi_]
                return V(arena, ap, a * 2 + lo * 4, a * 2 + hi_ * 4)
            hi_ = (b - a) if hi is None else hi
            return arena.c(a + lo, a + hi_)

        def ACT_(fc, lo=0, hi=512):
            base = fc * 512
            return arena.c(base + lo, base + hi)
        assert HALF * 512 <= 27 * 1024

        pb = [ps("pb%d" % i, 512) for i in range(7)]
        pbb = ps("pbb", 1024, BF16)

        def load(eng, dst, src_ap, **kw):
            P.dma(eng, lambda h: h.dma_start(out=dst.ap, in_=src_ap, **kw), writes=[dst])

        rr = [0]

        def evac_copy(dst, src, scale_ap=None):
            rr[0] ^= 1
            use_act = rr[0]
            if scale_ap is not None:
                bi = [i for i, b_ in enumerate(pb) if b_ is src.tt]
                use_act = EVAC_ACT_SCALE and bool(bi) and (bi[0] % 2 == 0)
            if use_act:
                if scale_ap is None:
                    P.op('act', lambda h: h.activation(out=dst.ap, in_=src.ap, func=AF.Copy), reads=[src], writes=[dst])
                else:
                    P.op('act', lambda h: h.activation(out=dst.ap, in_=src.ap, func=AF.Identity, scale=scale_ap.ap),
                         reads=[src, scale_ap], writes=[dst])
            else:
                if scale_ap is None:
                    P.op('dve', lambda h: h.tensor_copy(out=dst.ap, in_=src.ap), reads=[src], writes=[dst])
                else:
                    P.op('dve', lambda h: h.tensor_scalar(out=dst.ap, in0=src.ap, scalar1=scale_ap.ap, scalar2=None,
                                                          op0=ALU.mult), reads=[src, scale_ap], writes=[dst])

        def mm(out, lhsT, rhs, start, stop, sgc=False):
            P.op('pe', lambda h: h.matmul(out.ap, lhsT=lhsT.ap, rhs=rhs.ap, start=start, stop=stop, skip_group_check=sgc),
                 reads=[lhsT, rhs], writes=[out], sig=stop)

        def tr(out, in_, ident):
            P.op('pe', lambda h: h.transpose(out.ap, in_.ap, ident.ap), reads=[in_, ident], writes=[out])

        wrr = [0]

        def wslot():
            s = wsl[wrr[0] % NWSLOT]
            wrr[0] += 1
            return s

        def load_w(dst_view, src_ap):
            P.dma('pool', lambda h: h.dma_start(out=dst_view.ap, in_=src_ap, max_dma_last_dim=8192), writes=[dst_view])

        def rstd_from_ssq(ssq, n, out):
            P.op('dve', lambda h: h.tensor_scalar(out=out.ap, in0=ssq.ap, scalar1=1.0 / n, scalar2=EPS,
                                                  op0=ALU.mult, op1=ALU.add), reads=[ssq], writes=[out])
            P.op('act', lambda h: h.activation(out=out.ap, in_=out.ap, func=AF.Sqrt), reads=[out], writes=[out])
            P.op('dve', lambda h: h.reciprocal(out=out.ap, in_=out.ap), reads=[out], writes=[out])

        def norm_tile(x_t, gain, ti, want_f32=False):
            ssq = stat.c(0, 1)
            rs = stat.c(1, 2)
            hsv = hs.c(0, D)
            xv = x_t.c(0, D)
            if STOP <= 0:
                return
            P.op('act', lambda h: h.activation(out=hsv.ap, in_=xv.ap, func=AF.Square, accum_out=ssq.ap),
                 reads=[xv], writes=[hsv, ssq])
            rstd_from_ssq(ssq, D, rs)
            P.op('dve', lambda h: h.tensor_scalar(out=hsv.ap, in0=xv.ap, scalar1=rs.ap, scalar2=None, op0=ALU.mult),
                 reads=[xv, rs], writes=[hsv])
            if STOP <= 0.3:
                return
            for b in range(4):
                for j in range(4):
                    kc = b * 4 + j
                    tr(pb[b].c(j * 128, (j + 1) * 128), hs.c(kc * 128, (kc + 1) * 128), cst_identf.c(0, 128))
            if STOP <= 0.6:
                return
            for b in range(4):
                for j in range(4):
                    kc = b * 4 + j
                    src = pb[b].c(j * 128, (j + 1) * 128)
                    evac_copy(hT.c(kc * 512 + ti * 128, kc * 512 + (ti + 1) * 128), src, gain.c(kc, kc + 1))
                    if want_f32:
                        evac_copy(hTf.c(kc * 128, (kc + 1) * 128), src, gain.c(kc, kc + 1))

        CAP = 640
        if moe and phase == 2:
            XO = TT(x_out, 1, "x_out_d")
            H2S = TT(h2s, 1, "h2s_d")

        def xo_cells(tt, dg=None):
            if dg is None:
                return tt * 4, tt * 4 + 4
            return tt * 4 + dg, tt * 4 + dg + 1

        def moe_route_tile(ti, g):
            x_t = xt[ti]
            ssq = stat.c(0, 1)
            rs = stat.c(1, 2)
            hsv = hs.c(0, D)
            xv = x_t.c(0, D)
            P.op('act', lambda h: h.activation(out=hsv.ap, in_=xv.ap, func=AF.Square, accum_out=ssq.ap),
                 reads=[xv], writes=[hsv, ssq])
            rstd_from_ssq(ssq, D, rs)
            P.op('dve', lambda h: h.tensor_scalar(out=hsv.ap, in0=xv.ap, scalar1=rs.ap, scalar2=None, op0=ALU.mult),
                 reads=[xv, rs], writes=[hsv])
            P.dma('pool', lambda h: h.dma_start(out=h2s[g * 128:(g + 1) * 128, :], in_=hsv.ap, max_dma_last_dim=8192),
                  reads=[hsv], writes=[V(H2S, None, 0, 1)])
            lo, hi = xo_cells(g)
            P.dma('sp', lambda h: h.dma_start(out=x_out[g * 128:(g + 1) * 128, :], in_=xv.ap), reads=[xv], writes=[V(XO, None, lo, hi)])
            for b in range(4):
                for j in range(4):
                    kc = b * 4 + j
                    tr(pb[b].c(j * 128, (j + 1) * 128), hs.c(kc * 128, (kc + 1) * 128), cst_identf.c(0, 128))
            for b in range(4):
                for j in range(4):
                    kc = b * 4 + j
                    evac_copy(hTf.c(kc * 128, (kc + 1) * 128), pb[b].c(j * 128, (j + 1) * 128), g_ffn.c(kc, kc + 1))
            lgp = pb[6].c(0, NEXP)
            for kc in range(KC):
                mm(lgp, hTf.c(kc * 128, (kc + 1) * 128), wr.c(kc * NEXP, (kc + 1) * NEXP), kc == 0, kc == KC - 1)
            lg = rt.c(0, 8)
            lg2 = rt.c(8, 16)
            eq1 = rt.c(16, 24)
            eq2 = rt.c(24, 32)
            m1 = rt.c(32, 33)
            m2 = rt.c(33, 34)
            ex = rt.c(34, 35)
            w1 = rt.c(35, 36)
            w2 = rt.c(36, 37)
            wv = wgt_all.c(g * NEXP, (g + 1) * NEXP)
            mk = mask_all.c(g * NEXP, (g + 1) * NEXP)
            pv = pos_all.c(g * NEXP, (g + 1) * NEXP)
            P.op('dve', lambda h: h.tensor_copy(out=lg.ap, in_=lgp.ap), reads=[lgp], writes=[lg])
            P.op('dve', lambda h: h.tensor_reduce(out=m1.ap, in_=lg.ap, axis=AX.X, op=ALU.max), reads=[lg], writes=[m1])
            P.op('dve', lambda h: h.tensor_scalar(out=eq1.ap, in0=lg.ap, scalar1=m1.ap, scalar2=None, op0=ALU.is_equal),
                 reads=[lg, m1], writes=[eq1])
            P.op('dve', lambda h: h.scalar_tensor_tensor(out=lg2.ap, in0=eq1.ap, scalar=-1e30, in1=lg.ap, op0=ALU.mult, op1=ALU.add),
                 reads=[eq1, lg], writes=[lg2])
            P.op('dve', lambda h: h.tensor_reduce(out=m2.ap, in_=lg2.ap, axis=AX.X, op=ALU.max), reads=[lg2], writes=[m2])
            P.op('dve', lambda h: h.tensor_scalar(out=eq2.ap, in0=lg2.ap, scalar1=m2.ap, scalar2=None, op0=ALU.is_equal),
                 reads=[lg2, m2], writes=[eq2])
            P.op('dve', lambda h: h.tensor_tensor(out=ex.ap, in0=m2.ap, in1=m1.ap, op=ALU.subtract), reads=[m1, m2], writes=[ex])
            P.op('act', lambda h: h.activation(out=ex.ap, in_=ex.ap, func=AF.Exp), reads=[ex], writes=[ex])
            P.op('dve', lambda h: h.tensor_scalar(out=w1.ap, in0=ex.ap, scalar1=1.0, scalar2=None, op0=ALU.add), reads=[ex], writes=[w1])
            P.op('dve', lambda h: h.reciprocal(out=w1.ap, in_=w1.ap), reads=[w1], writes=[w1])
            P.op('dve', lambda h: h.tensor_tensor(out=w2.ap, in0=ex.ap, in1=w1.ap, op=ALU.mult), reads=[ex, w1], writes=[w2])
            P.op('dve', lambda h: h.tensor_scalar(out=wv.ap, in0=eq1.ap, scalar1=w1.ap, scalar2=None, op0=ALU.mult),
                 reads=[eq1, w1], writes=[wv])
            P.op('dve', lambda h: h.scalar_tensor_tensor(out=wv.ap, in0=eq2.ap, scalar=w2.ap, in1=wv.ap, op0=ALU.mult, op1=ALU.add),
                 reads=[eq2, w2, wv], writes=[wv])
            P.op('dve', lambda h: h.tensor_tensor(out=mk.ap, in0=eq1.ap, in1=eq2.ap, op=ALU.add), reads=[eq1, eq2], writes=[mk])
            mb = maskb.c(0, NEXP)
            P.op('dve', lambda h: h.tensor_copy(out=mb.ap, in_=mk.ap), reads=[mk], writes=[mb])
            mm(pb[5].c(0, NEXP), cst_ustrict.c(0, 128), mb, True, True)
            mm(pb[5].c(NEXP, 2 * NEXP), cst_ones.c(0, 128), mb, True, True)
            cv = carry.c(0, NEXP)
            P.op('dve', lambda h: h.tensor_tensor(out=pv.ap, in0=pb[5].t[:, 0:NEXP], in1=cv.ap, op=ALU.add),
                 reads=[pb[5].c(0, NEXP), cv], writes=[pv])
            P.op('dve', lambda h: h.tensor_tensor(out=cv.ap, in0=pb[5].t[:, NEXP:2 * NEXP], in1=cv.ap, op=ALU.add),
                 reads=[pb[5].c(0, NEXP), cv], writes=[cv])

        def moe_experts():
            NPART = 8
            PCH = NFC // NPART
            Pe = xtall.view(0, NT * CAP, BF16, "Pe")
            sg = xtall.view(NT * CAP * 2, CAP, F32, "sg")
            actq = xtall.view(NT * CAP * 2 + CAP * 4, PCH * CAP, BF16, "actq")
            assert NT * CAP * 2 + CAP * 4 + PCH * CAP * 2 <= ST * D * 4
            yacc = arena.view(0, 5 * D, F32, "yacc")
            PT = arena.view(5 * D * 4, CAP, BF16, "PT")
            contrib = [arena.view(5 * D * 4 + CAP * 2 + i * 2048, 512, F32, "contrib%d" % i) for i in range(4)]
            iota = arena.view(5 * D * 4 + CAP * 2 + 4 * 2048, CAP, F32, "iota")
            PT2 = arena.view(5 * D * 4 + CAP * 2 + 4 * 2048 + CAP * 4, CAP, BF16, "PT2")
            PTs = [PT, PT2]
            assert 5 * D * 4 + CAP * 2 + 4 * 2048 + CAP * 4 + CAP * 2 <= 27 * 1024 * 2
            hTe = mixc.view(0, KC * CAP, BF16, "hTe")
            yb = mixc.view(0, 5 * D, BF16, "yb")
            h2c = [hT.view(0, NT * 512, BF16, "h2c0"), hsf.view(0, NT * 512, BF16, "h2c1")]
            load('sp', iota.c(0, CAP), iota_d[:, :])
            for e in range(NEXP):
                for tt in range(NT):
                    pv = pos_all.c(tt * NEXP + e, tt * NEXP + e + 1)
                    mk = mask_all.c(tt * NEXP + e, tt * NEXP + e + 1)
                    pe_ = Pe.c(tt * CAP, (tt + 1) * CAP)
                    P.op('dve', lambda h, pe_=pe_, pv=pv, mk=mk: h.tensor_scalar(
                        out=pe_.ap, in0=iota.t[:, :], scalar1=pv.ap, scalar2=mk.ap, op0=ALU.is_equal, op1=ALU.mult),
                        reads=[iota.c(0, CAP), pv, mk], writes=[pe_])
                for kg in range(4):
                    hc = h2c[kg % 2]
                    hcv = hc.c(0, NT * 512)
                    P.dma('sp', lambda h, hcv=hcv, kg=kg: h.dma_start(
                        out=hcv.ap.rearrange("p (a b) -> p a b", b=512),
                        in_=h2s[:, kg * 512:(kg + 1) * 512].rearrange("(a p) c -> p a c", p=128)),
                        reads=[V(H2S, None, 0, 1)], writes=[hcv])
                    for k4 in range(4):
                        kc = kg * 4 + k4
                        for cg, (c0, c1) in enumerate(((0, 512), (512, CAP))):
                            bank = pb[(kc * 2 + cg) % 4]
                            tts = [tt for tt in range(NT) if 128 * (tt + 1) > c0] or list(range(NT))
                            for tt in tts:
                                ce = max(min(c1, 128 * (tt + 1)), c0 + 128) if NT * 128 >= CAP else c1
                                mm(bank.c(0, ce - c0), hc.c(tt * 512 + k4 * 128, tt * 512 + (k4 + 1) * 128),
                                   Pe.c(tt * CAP + c0, tt * CAP + ce), tt == tts[0], tt == tts[-1], sgc=True)
                            evac_copy(hTe.c(kc * CAP + c0, kc * CAP + c1), bank.c(0, c1 - c0), g_ffn.c(kc, kc + 1))
                for qi in range(NPART):
                    f0 = qi * PCH
                    fl = 0
                    for nb in (2, 2, 2, 1):
                        ws = wslot()
                        c0 = (f0 + fl) * 128
                        gv = ws.c(0, KC * nb * 128)
                        uv = ws.c(KC * 256, KC * 256 + KC * nb * 128)
                        load_w(gv.r("p (a b) -> p a b", b=nb * 128), wg_d[e * D:(e + 1) * D, c0:c0 + nb * 128].rearrange("(a p) c -> p a c", p=128))
                        load_w(uv.r("p (a b) -> p a b", b=nb * 128), wu_d[e * D:(e + 1) * D, c0:c0 + nb * 128].rearrange("(a p) c -> p a c", p=128))
                        for j in range(nb):
                            fc = fl + j
                            par = fc % 2
                            g0, u0, xb = pb[3 * par], pb[3 * par + 1], pb[3 * par + 2]
                            wgs = lambda kc: ws.c(kc * nb * 128 + j * 128, kc * nb * 128 + (j + 1) * 128)
                            wus = lambda kc: ws.c(KC * 256 + kc * nb * 128 + j * 128, KC * 256 + kc * nb * 128 + (j + 1) * 128)
                            for kc in range(KC):
                                mm(g0.c(0, 512), wgs(kc), hTe.c(kc * CAP, kc * CAP + 512), kc == 0, kc == KC - 1)
                            for kc in range(KC):
                                mm(xb.c(0, 128), wgs(kc), hTe.c(kc * CAP + 512, (kc + 1) * CAP), kc == 0, kc == KC - 1)
                            for kc in range(KC):
                                mm(u0.c(0, 512), wus(kc), hTe.c(kc * CAP, kc * CAP + 512), kc == 0, kc == KC - 1)
                            for kc in range(KC):
                                mm(xb.c(128, 256), wus(kc), hTe.c(kc * CAP + 512, (kc + 1) * CAP), kc == 0, kc == KC - 1)
                            s0, s1 = sg.c(0, 512), sg.c(512, CAP)
                            P.op('act', lambda h, s0=s0, g0=g0: h.activation(out=s0.ap, in_=g0.t[:, 0:512], func=AF.Silu),
                                 reads=[g0.c(0, 512)], writes=[s0])
                            P.op('act', lambda h, s1=s1, xb=xb: h.activation(out=s1.ap, in_=xb.t[:, 0:128], func=AF.Silu),
                                 reads=[xb.c(0, 128)], writes=[s1])
                            a0 = actq.c(fc * CAP, fc * CAP + 512)
                            a1 = actq.c(fc * CAP + 512, (fc + 1) * CAP)
                            P.op('dve', lambda h, a0=a0, s0=s0, u0=u0: h.tensor_tensor(out=a0.ap, in0=u0.t[:, 0:512], in1=s0.ap, op=ALU.mult),
                                 reads=[u0.c(0, 512), s0], writes=[a0])
                            P.op('dve', lambda h, a1=a1, s1=s1, xb=xb: h.tensor_tensor(out=a1.ap, in0=xb.t[:, 128:256], in1=s1.ap, op=ALU.mult),
                                 reads=[xb.c(0, 256), s1], writes=[a1])
                        fl += nb
                    for dg in range(4):
                        ws = wslot()
                        r0 = e * FF + f0 * 128
                        load_w(ws.c(0, PCH * 512).r("p (a b) -> p a b", b=512),
                               wd_d[r0:r0 + PCH * 128, dg * 512:(dg + 1) * 512].rearrange("(a p) c -> p a c", p=128))
                        for s5 in range(5):
                            bank = pb[4 + (dg * 5 + s5) % 3]
                            for q in range(PCH):
                                mm(bank.c(0, 512), actq.c(q * CAP + s5 * 128, q * CAP + (s5 + 1) * 128), ws.c(q * 512, (q + 1) * 512),
                                   q == 0, q == PCH - 1)
                            yv = yacc.c(s5 * D + dg * 512, s5 * D + (dg + 1) * 512)
                            if qi == 0:
                                evac_copy(yv, bank.c(0, 512))
                            elif qi < NPART - 1:
                                P.op('dve', lambda h, yv=yv, bank=bank: h.tensor_tensor(out=yv.ap, in0=bank.t[:, 0:512], in1=yv.ap, op=ALU.add),
                                     reads=[bank.c(0, 512), yv], writes=[yv])
                            else:
                                ybv = yb.c(s5 * D + dg * 512, s5 * D + (dg + 1) * 512)
                                P.op('dve', lambda h, yv=yv, ybv=ybv, bank=bank: h.tensor_tensor(out=ybv.ap, in0=bank.t[:, 0:512], in1=yv.ap, op=ALU.add),
                                     reads=[bank.c(0, 512), yv], writes=[ybv])
                def s5s(tt):
                    return [q for q in range(5) if q <= tt] if NT * 128 >= CAP else list(range(5))

                def prep(tt):
                    ss = s5s(tt)
                    for s5 in ss:
                        tr(pbb.c(s5 * 128, (s5 + 1) * 128), Pe.c(tt * CAP + s5 * 128, tt * CAP + (s5 + 1) * 128), cst_identb.c(0, 128))
                    ptv = PTs[tt % 2].c(0, len(ss) * 128)
                    P.op('act', lambda h, ptv=ptv, n=len(ss) * 128: h.activation(out=ptv.ap, in_=pbb.t[:, 0:n], func=AF.Copy),
                         reads=[pbb.c(0, CAP)], writes=[ptv])
                prep(0)
                for tt in range(NT):
                    if tt + 1 < NT:
                        prep(tt + 1)
                    PTc = PTs[tt % 2]
                    wv = wgt_all.c(tt * NEXP + e, tt * NEXP + e + 1)
                    for dg in range(4):
                        bank = pb[dg]
                        ss = s5s(tt)
                        for s5 in ss:
                            mm(bank.c(0, 512), PTc.c(s5 * 128, (s5 + 1) * 128), yb.c(s5 * D + dg * 512, s5 * D + (dg + 1) * 512),
                               s5 == ss[0], s5 == ss[-1])
                        cvw = contrib[dg].c(0, 512)
                        P.op('dve', lambda h, cvw=cvw, bank=bank, wv=wv: h.tensor_scalar(out=cvw.ap, in0=bank.t[:, 0:512], scalar1=wv.ap, scalar2=None, op0=ALU.mult),
                             reads=[bank.c(0, 512), wv], writes=[cvw])
                        lo, hi = xo_cells(tt, dg)
                        P.dma('pool', lambda h, cvw=cvw, tt=tt, dg=dg: h.dma_start(
                            out=x_out[tt * 128:(tt + 1) * 128, dg * 512:(dg + 1) * 512], in_=cvw.ap, accum_op=ALU.add),
                            reads=[cvw], writes=[V(XO, None, lo, hi)])

        def moe_final():
            load('sp', fgb.c(0, D), fing.partition_broadcast(128))
            for tt in range(NT):
                xv = xt[tt % ST].c(0, D)
                lo, hi = xo_cells(tt)
                P.dma('sp', lambda h, xv=xv, tt=tt: h.dma_start(out=xv.ap, in_=x_out[tt * 128:(tt + 1) * 128, :]),
                      reads=[V(XO, None, lo, hi)], writes=[xv])
                ssq = stat.c(0, 1)
                rs = stat.c(1, 2)
                hsv = A("otmp", f32=True)
                for hf in range(2):
                    sq = stat.c(2 + hf, 3 + hf)
                    xh = xt[tt % ST].c(hf * 1024, (hf + 1) * 1024)
                    P.op('act', lambda h, xh=xh, sq=sq: h.activation(out=hsv.ap, in_=xh.ap, func=AF.Square, accum_out=sq.ap),
                         reads=[xh], writes=[hsv, sq])
                P.op('dve', lambda h: h.tensor_tensor(out=ssq.ap, in0=stat.t[:, 2:3], in1=stat.t[:, 3:4], op=ALU.add),
                     reads=[stat.c(2, 4)], writes=[ssq])
                rstd_from_ssq(ssq, D, rs)
                P.op('dve', lambda h, xv=xv: h.scalar_tensor_tensor(out=xv.ap, in0=xv.ap, scalar=rs.ap, in1=fgb.t[:, :], op0=ALU.mult, op1=ALU.mult),
                     reads=[xv, rs, fgb.c(0, D)], writes=[xv])
                out_sems.append(P.dma('sp', lambda h, xv=xv, tt=tt: h.dma_start(out=x_out[tt * 128:(tt + 1) * 128, :], in_=xv.ap),
                                      reads=[xv], writes=[V(XO, None, lo, hi)]))

        load('sp', cst_identf.c(0, 128), ident_f_d[:, :])
        load('sp', cst_identb.c(0, 128), ident_b_d[:, :])
        load('sp', cst_trin.c(0, 128), trin_d[:, :])
        load('sp', g_mix.c(0, KC), mixg[:, :])
        P.op('pool', lambda h: h.memset(glT.t[:, :], 1.0), writes=[glT.c(0, 512)])
        load_w(wgu_a.c(0, 512, 0, 16), wgu[:, :])
        load_w(wgu_a.c(0, 512, 16, 17), bgate[:, :])
        P.op('pool', lambda h: h.memset(bsum.t[:, :], 0.0), writes=[bsum.c(0, 4)])
        if phase == 1:
            P.op('pool', lambda h: h.memset(Sf.t[:, :], 0.0), writes=[Sf.c(0, 1024)])
            P.op('pool', lambda h: h.memset(Sb.t[:, :], 0.0), writes=[Sb.c(0, 1024)])
        else:
            load('sp', cst_mask4.c(0, 512), mask4_d[:, :])
            load('sp', cst_mcur.c(0, 512), mcur_d[:, :])
            load('sp', cst_mcur0.c(0, 512), mcur0_d[:, :])
            load('sp', cst_mprev.c(0, 512), mprev_d[:, :])
            load('sp', cmask.c(0, 8), cmask_d[:, :])
            load('sp', g_ffn.c(0, KC), ffng[:, :])
            load('sp', psc.c(0, 8), pscale[:, :])
            load('sp', gnb.c(0, 1024), gnorm.partition_broadcast(128))
            load('sp', uprev.c(0, 1024), uhalo[:, :])
            load_w(wpool.c(0, 2048).r("p (a b) -> p a b", b=256), w_pool.rearrange("(a p) d -> p a d", p=128))
            if moe:
                load('sp', wr.c(0, KC * NEXP).r("p (a b) -> p a b", b=NEXP), w_router.rearrange("(a p) e -> p a e", p=128))
                load('sp', cst_ustrict.c(0, 128), ustrict_d[:, :])
                load('sp', cst_ones.c(0, 128), ones_d[:, :])
                P.op('pool', lambda h: h.memset(carry.t[:, :], 0.0), writes=[carry.c(0, NEXP)])
            P.op('pool', lambda h: h.memset(Sf.t[:, :], 0.0), writes=[Sf.c(0, 1024)])
            Lb = A("otmp", f32=True)
            bj = stat.c(8, 12)
            aj = stat.c(12, 16)
            for j in range(NCORES):
                load('sp', Lb, lall[j * 128:(j + 1) * 128, :])
                load('sp', bj, ball[j * 128:(j + 1) * 128, :])
                mj = cmask.c(j, j + 1)
                P.op('dve', lambda h, mj=mj: h.tensor_scalar(out=aj.ap, in0=bj.ap, scalar1=mj.ap, scalar2=None, op0=ALU.mult),
                     reads=[bj, mj], writes=[aj])
                P.op('act', lambda h: h.activation(out=aj.ap, in_=aj.ap, func=AF.Exp), reads=[aj], writes=[aj])
                P.op('dve', lambda h, mj=mj: h.tensor_scalar(out=Lb.ap, in0=Lb.ap, scalar1=mj.ap, scalar2=None, op0=ALU.mult),
                     reads=[Lb, mj], writes=[Lb])
                for hh in range(4):
                    sv = Sf.c(hh * 256, (hh + 1) * 256)
                    lv = V(arena, Lb.ap[:, hh * 256:(hh + 1) * 256], Lb.lo, Lb.hi)
                    av = stat.c(12 + hh, 13 + hh)
                    P.op('dve', lambda h, sv=sv, lv=lv, av=av: h.scalar_tensor_tensor(
                        out=sv.ap, in0=sv.ap, scalar=av.ap, in1=lv.ap, op0=ALU.mult, op1=ALU.add),
                        reads=[sv, lv, av], writes=[sv])
            P.op('dve', lambda h: h.tensor_copy(out=Sb.t[:, :], in_=Sf.t[:, :]), reads=[Sf.c(0, 1024)], writes=[Sb.c(0, 1024)])

        out_sems = []
        zoff = AO["z"][0]

        def Z(ti, lo, hi):
            return arena.c(zoff + ti * 3072 + lo, zoff + ti * 3072 + hi)

        for st in range(NST):
            for ti in range(ST):
                g = st * ST + ti
                load('sp', xt[ti].c(0, D), x_in[g * 128:(g + 1) * 128, :])
                norm_tile(xt[ti], g_mix, ti)

            if STOP <= 1:
                continue
            blocks = [('u', 0, 512, 0), ('u', 512, 512, 512), ('q', 1024, 512, 0), ('k', 1536, 512, 0),
                      ('v', 2048, 512, 1024), ('v', 2560, 512, 1536), ('g', 3072, 16, 0),
                      ('r', 3088, 512, 2048), ('r', 3600, 512, 2560)]
            for kind, c0, ncol, zc in blocks:
                if phase == 1 and (kind in ('q', 'r') or (kind == 'u' and st != NST - 1)):
                    continue
                if phase == 2 and kind in ('k', 'v', 'g'):
                    continue
                ws = wslot()
                wv = ws.c(0, KC * ncol)
                load_w(wv.r("p (a b) -> p a b", b=ncol), w_in[:, c0:c0 + ncol].rearrange("(a p) c -> p a c", p=128))
                if kind in ('u', 'v', 'r'):
                    for ti in range(ST):
                        bank = pb[ti % 4]
                        for kc in range(KC):
                            mm(bank.c(0, 512), hT.c(kc * 512 + ti * 128, kc * 512 + (ti + 1) * 128),
                               ws.c(kc * 512, (kc + 1) * 512), kc == 0, kc == KC - 1)
                        evac_copy(Z(ti, zc, zc + 512), bank.c(0, 512))
                elif kind in ('q', 'k'):
                    for hh in range(4):
                        bank = pb[hh]
                        for kc in range(KC):
                            mm(bank.c(0, 512), ws.c(kc * 512 + hh * 128, kc * 512 + (hh + 1) * 128),
                               hT.c(kc * 512, (kc + 1) * 512), kc == 0, kc == KC - 1)
                        dst = A("qT" if kind == 'q' else "kT").r("p (a b c) -> p a b c", a=ST, b=4).s(slice(None), slice(None), hh, slice(None))
                        evac_copy(dst, bank.c(0, 512).r("p (a c) -> p a c", a=ST))
                else:
                    bank = pb[4]
                    for kc in range(KC):
                        mm(bank.c(0, 512, 0, 16), ws.c(kc * 16, (kc + 1) * 16), hT.c(kc * 512, (kc + 1) * 512),
                           kc == 0, kc == KC - 1)
                    evac_copy(glT.c(0, 512, 0, 16), bank.c(0, 512, 0, 16))

            if phase == 2:
                kv_ = A("kT")
                load('sp', kv_, kT_i[st * 128:(st + 1) * 128, :])
                for ti in range(ST):
                    g = st * ST + ti
                    load('sp', Z(ti, 1024, 2048), v_i[g * 128:(g + 1) * 128, :])
                load('sp', glT.c(0, 512, 0, 16), gl_i[st * 16:(st + 1) * 16, :])
            else:
                kv_ = A("kT")
                out_sems.append(P.dma('sp', lambda h, kv_=kv_, st=st: h.dma_start(out=kT_o[st * 128:(st + 1) * 128, :], in_=kv_.ap), reads=[kv_]))
                for ti in range(ST):
                    g = st * ST + ti
                    vv_ = Z(ti, 1024, 2048)
                    out_sems.append(P.dma('sp', lambda h, vv_=vv_, g=g: h.dma_start(out=v_o[g * 128:(g + 1) * 128, :], in_=vv_.ap), reads=[vv_]))
                gv_ = glT.c(0, 512, 0, 16)
                out_sems.append(P.dma('sp', lambda h, gv_=gv_, st=st: h.dma_start(out=gl_o[st * 16:(st + 1) * 16, :], in_=gv_.ap), reads=[gv_]))
            if STOP <= 2:
                continue
            for ti in range(ST):
                g = st * ST + ti
                kT_t = A("kT", ti * 512, (ti + 1) * 512)
                mm(pb[5].c(0, 512), glT.c(ti * 128, (ti + 1) * 128, 0, 17), wgu_a.c(0, 512, 0, 17), True, True)
                ysp = A("ysp", f32=True)
                spv = A("sp")
                P.op('act', lambda h, ysp=ysp: h.activation(out=ysp.ap, in_=pb[5].t[:, 0:512], func=AF.Exp, scale=-1.0),
                     reads=[pb[5].c(0, 512)], writes=[ysp])
                P.op('act', lambda h, ysp=ysp, spv=spv: h.activation(out=spv.ap, in_=ysp.ap, func=AF.Ln, bias=1.0),
                     reads=[ysp], writes=[spv])
                if STOP <= 2.2:
                    continue
                for hh in range(4):
                    mm(pb[6].c(hh * 128, (hh + 1) * 128), V(arena, spv.ap[:, hh * 128:(hh + 1) * 128], spv.lo, spv.hi),
                       cst_trin.c(0, 128), True, True)
                epos = A("epos", f32=True)
                eneg = A("eneg", f32=True)
                bcv = pb[6].c(0, 512)
                P.op('act', lambda h, epos=epos: h.activation(out=epos.ap, in_=pb[6].t[:, 0:512], func=AF.Exp),
                     reads=[bcv], writes=[epos])
                P.op('act', lambda h, eneg=eneg: h.activation(out=eneg.ap, in_=pb[6].t[:, 0:512], func=AF.Exp, scale=-1.0),
                     reads=[bcv], writes=[eneg])
                if STOP <= 2.3:
                    continue
                bl = V(pb[6], pb[6].t[:, 0:512].rearrange("p (a b) -> p a b", b=128)[:, :, 127], 0, 2048)
                P.op('dve', lambda h, bl=bl: h.tensor_tensor(out=bsum.t[:, :], in0=bl.ap, in1=bsum.t[:, :], op=ALU.add),
                     reads=[bsum.c(0, 4), bl], writes=[bsum.c(0, 4)])
                if STOP <= 2.4:
                    continue
                kd = A("kd")
                P.op('dve', lambda h, kd=kd, kT_t=kT_t, eneg=eneg: h.tensor_tensor(out=kd.ap, in0=kT_t.ap, in1=eneg.ap, op=ALU.mult),
                     reads=[kT_t, eneg], writes=[kd])
                if phase == 2:
                    qT_t = A("qT", ti * 512, (ti + 1) * 512)
                    qd = A("qd")
                    P.op('dve', lambda h, qd=qd, qT_t=qT_t, epos=epos: h.scalar_tensor_tensor(
                        out=qd.ap, in0=qT_t.ap, scalar=float(128 ** -0.5), in1=epos.ap, op0=ALU.mult, op1=ALU.mult),
                        reads=[qT_t, epos], writes=[qd])
                    for hh in range(4):
                        mm(pb[4].c(hh * 128, (hh + 1) * 128), V(arena, kd.ap[:, hh * 128:(hh + 1) * 128], kd.lo, kd.hi),
                           V(arena, qd.ap[:, hh * 128:(hh + 1) * 128], qd.lo, qd.hi), True, True)
                    att = A("att")
                    P.op('dve', lambda h, att=att: h.tensor_tensor(out=att.ap, in0=pb[4].t[:, 0:512], in1=cst_mask4.t[:, :], op=ALU.mult),
                         reads=[pb[4].c(0, 512), cst_mask4.c(0, 512)], writes=[att])
                    for hh in range(4):
                        ob = pb[hh // 2].c((hh % 2) * 256, (hh % 2) * 256 + 256)
                        mm(ob, V(arena, qd.ap[:, hh * 128:(hh + 1) * 128], qd.lo, qd.hi), Sb.c(hh * 256, (hh + 1) * 256), True, False)
                        mm(ob, V(arena, att.ap[:, hh * 128:(hh + 1) * 128], att.lo, att.hi), Z(ti, 1024 + hh * 256, 1024 + (hh + 1) * 256), False, True)
                kteT = A("kteT")
                elb = V(arena, epos.ap.rearrange("p (a b) -> p a b", b=128)[:, :, 127:128].to_broadcast([128, 4, 128]), epos.lo, epos.hi)
                P.op('dve', lambda h, kteT=kteT, kd=kd, elb=elb: h.tensor_tensor(
                    out=kteT.ap.rearrange("p (a b) -> p a b", b=128), in0=kd.ap.rearrange("p (a b) -> p a b", b=128), in1=elb.ap, op=ALU.mult),
                    reads=[kd, elb], writes=[kteT])
                if STOP <= 2.6:
                    continue
                for hh in range(4):
                    tr(pbb.c(hh * 128, (hh + 1) * 128), V(arena, kteT.ap[:, hh * 128:(hh + 1) * 128], kteT.lo, kteT.hi), cst_identb.c(0, 128))
                kte = A("kte")
                P.op('act', lambda h, kte=kte: h.activation(out=kte.ap, in_=pbb.t[:, 0:512], func=AF.Copy),
                     reads=[pbb.c(0, 512)], writes=[kte])
                if STOP <= 2.8:
                    continue
                for hh in range(4):
                    cb = pb[2 + hh // 2].c((hh % 2) * 256, (hh % 2) * 256 + 256)
                    mm(cb, V(arena, kte.ap[:, hh * 128:(hh + 1) * 128], kte.lo, kte.hi), Z(ti, 1024 + hh * 256, 1024 + (hh + 1) * 256), True, True)
                elb2 = V(arena, epos.ap.rearrange("p (a b) -> p a b", b=128)[:, :, 127:128].to_broadcast([128, 4, 256]), epos.lo, epos.hi)
                sall = Sf.c(0, 1024)
                P.op('dve', lambda h, elb2=elb2: h.tensor_tensor(out=Sf.t[:, :].rearrange("p (a b) -> p a b", b=256),
                                                                 in0=Sf.t[:, :].rearrange("p (a b) -> p a b", b=256), in1=elb2.ap, op=ALU.mult),
                     reads=[sall, elb2], writes=[sall])
                for half in range(2):
                    cbk = pb[2 + half].c(0, 512)
                    sv = Sf.c(half * 512, (half + 1) * 512)
                    P.op('dve', lambda h, sv=sv, half=half: h.tensor_tensor(out=sv.ap, in0=pb[2 + half].t[:, 0:512], in1=sv.ap, op=ALU.add),
                         reads=[sv, cbk], writes=[sv])
                P.op('dve', lambda h: h.tensor_copy(out=Sb.t[:, :], in_=Sf.t[:, :]), reads=[Sf.c(0, 1024)], writes=[Sb.c(0, 1024)])

                if phase == 1:
                    continue

                otmp = A("otmp", f32=True)
                ssq4 = stat.c(16, 20)
                rs4 = stat.c(20, 24)
                for hh in range(4):
                    ob = pb[hh // 2].c((hh % 2) * 256, (hh % 2) * 256 + 256)
                    sq = stat.c(16 + hh, 17 + hh)
                    ot = V(arena, otmp.ap[:, hh * 256:(hh + 1) * 256], otmp.lo, otmp.hi)
                    P.op('act', lambda h, ob=ob, sq=sq, ot=ot: h.activation(out=ot.ap, in_=ob.ap, func=AF.Square, accum_out=sq.ap),
                         reads=[ob], writes=[ot, sq])
                rstd_from_ssq(ssq4, 256, rs4)
                for hh in range(4):
                    ob = pb[hh // 2].c((hh % 2) * 256, (hh % 2) * 256 + 256)
                    rv = stat.c(20 + hh, 21 + hh)
                    ot = V(arena, otmp.ap[:, hh * 256:(hh + 1) * 256], otmp.lo, otmp.hi)
                    gv = gnb.c(hh * 256, (hh + 1) * 256)
                    P.op('dve', lambda h, ob=ob, rv=rv, ot=ot, gv=gv: h.scalar_tensor_tensor(
                        out=ot.ap, in0=ob.ap, scalar=rv.ap, in1=gv.ap, op0=ALU.mult, op1=ALU.mult),
                        reads=[ob, rv, gv], writes=[ot])
                sr = A("sr")
                rz = Z(ti, 2048, 3072)
                P.op('act', lambda h, sr=sr, rz=rz: h.activation(out=sr.ap, in_=rz.ap, func=AF.Silu), reads=[rz], writes=[sr])
                og = A("og")
                P.op('dve', lambda h, og=og, otmp=otmp, sr=sr: h.tensor_tensor(out=og.ap, in0=otmp.ap, in1=sr.ap, op=ALU.mult),
                     reads=[otmp, sr], writes=[og])
                for c in range(8):
                    tr(pbb.c(c * 128, (c + 1) * 128), V(arena, og.ap[:, c * 128:(c + 1) * 128], og.lo, og.hi), cst_identb.c(0, 128))
                for c in range(8):
                    evac_copy(mixT.c((8 + c) * 512 + ti * 128, (8 + c) * 512 + (ti + 1) * 128), pbb.c(c * 128, (c + 1) * 128))

                ucur = Z(ti, 0, 1024)
                mc = cst_mcur0 if g == 0 else cst_mcur
                for c in range(8):
                    gi = c // 2
                    ob = pb[4 + c // 4].c((c % 4) * 128, (c % 4 + 1) * 128)
                    mm(ob, uprev.c(c * 128, (c + 1) * 128), cst_mprev.c(gi * 128, (gi + 1) * 128), True, False)
                    mm(ob, V(arena, ucur.ap[:, c * 128:(c + 1) * 128], ucur.lo, ucur.hi), mc.c(gi * 128, (gi + 1) * 128), False, True)
                dT = A("dT")
                evac_copy(V(arena, dT.ap[:, 0:512], dT.lo, dT.hi), pb[4].c(0, 512))
                evac_copy(V(arena, dT.ap[:, 512:1024], dT.lo, dT.hi), pb[5].c(0, 512))
                for oc in range(8):
                    gi, jj = oc // 2, oc % 2
                    ob = pb[4 + oc // 4].c((oc % 4) * 128, (oc % 4 + 1) * 128)
                    for ci in range(2):
                        wv = wpool.c((gi * 2 + ci) * 256 + jj * 128, (gi * 2 + ci) * 256 + (jj + 1) * 128)
                        mm(ob, wv, V(arena, dT.ap[:, (gi * 2 + ci) * 128:(gi * 2 + ci + 1) * 128], dT.lo, dT.hi), ci == 0, ci == 1)
                for oc in range(8):
                    ob = pb[4 + oc // 4].c((oc % 4) * 128, (oc % 4 + 1) * 128)
                    evac_copy(mixT.c(oc * 512 + ti * 128, oc * 512 + (ti + 1) * 128), ob, psc.c(oc, oc + 1))
                P.op('pool', lambda h, ucur=ucur: h.tensor_copy(out=uprev.t[:, :], in_=ucur.ap), reads=[ucur], writes=[uprev.c(0, 1024)])

            if phase == 1:
                continue

            for cb in range(4):
                ws = wslot()
                load_w(ws.c(0, KC * 512).r("p (a b) -> p a b", b=512), w_out[:, cb * 512:(cb + 1) * 512].rearrange("(a p) c -> p a c", p=128))
                for ti in range(ST):
                    bank = pb[ti]
                    for kc in range(KC):
                        mm(bank.c(0, 512), mixT.c(kc * 512 + ti * 128, kc * 512 + (ti + 1) * 128), ws.c(kc * 512, (kc + 1) * 512),
                           kc == 0, kc == KC - 1)
                    xv = xt[ti].c(cb * 512, (cb + 1) * 512)
                    P.op('dve', lambda h, xv=xv, bank=bank: h.tensor_tensor(out=xv.ap, in0=bank.t[:, 0:512], in1=xv.ap, op=ALU.add),
                         reads=[bank.c(0, 512), xv], writes=[xv])

            if moe:
                for ti in range(ST):
                    moe_route_tile(ti, st * ST + ti)
                continue

            for ti in range(ST):
                norm_tile(xt[ti], g_ffn, ti)

            for e in range(NE):
                for half in range(2):
                    f0 = half * HALF
                    for fb in range(HALF // 2):
                        ws = wslot()
                        c0 = (f0 + fb * 2) * 128
                        gv = ws.c(0, KC * 256)
                        uv = ws.c(KC * 256, 2 * KC * 256)
                        load_w(gv.r("p (a b) -> p a b", b=256), wg_d[e * D:(e + 1) * D, c0:c0 + 256].rearrange("(a p) c -> p a c", p=128))
                        load_w(uv.r("p (a b) -> p a b", b=256), wu_d[e * D:(e + 1) * D, c0:c0 + 256].rearrange("(a p) c -> p a c", p=128))
                        for j in range(2):
                            fc = fb * 2 + j
                            gb_ = pb[(fc % 2) * 2]
                            ub_ = pb[(fc % 2) * 2 + 1]
                            for kc in range(KC):
                                mm(gb_.c(0, 512), ws.c(kc * 256 + j * 128, kc * 256 + (j + 1) * 128), hT.c(kc * 512, (kc + 1) * 512),
                                   kc == 0, kc == KC - 1)
                            for kc in range(KC):
                                mm(ub_.c(0, 512), ws.c(KC * 256 + kc * 256 + j * 128, KC * 256 + kc * 256 + (j + 1) * 128),
                                   hT.c(kc * 512, (kc + 1) * 512), kc == 0, kc == KC - 1)
                            sg = hs.c((fc % 2) * 512, (fc % 2) * 512 + 512)
                            P.op('act', lambda h, sg=sg, gb_=gb_: h.activation(out=sg.ap, in_=gb_.t[:, 0:512], func=AF.Silu),
                                 reads=[gb_.c(0, 512)], writes=[sg])
                            av = ACT_(fc)
                            P.op('dve', lambda h, av=av, sg=sg, ub_=ub_: h.tensor_tensor(out=av.ap, in0=ub_.t[:, 0:512], in1=sg.ap, op=ALU.mult),
                                 reads=[ub_.c(0, 512), sg], writes=[av])
                    PIECE = 11 if not moe else 14
                    for cb in range(4):
                        for pi in range(HALF // PIECE):
                            ws = wslot()
                            r0 = e * FF + (f0 + pi * PIECE) * 128
                            load_w(ws.c(0, PIECE * 512).r("p (a b) -> p a b", b=512),
                                   wd_d[r0:r0 + PIECE * 128, cb * 512:(cb + 1) * 512].rearrange("(a p) c -> p a c", p=128))
                            for ti in range(ST):
                                bank = pb[3 + ti]
                                for q in range(PIECE):
                                    fc = pi * PIECE + q
                                    mm(bank.c(0, 512), ACT_(fc, ti * 128, (ti + 1) * 128), ws.c(q * 512, (q + 1) * 512),
                                       fc == 0, fc == HALF - 1)
                        for ti in range(ST):
                            bank = pb[3 + ti]
                            xv = xt[ti].c(cb * 512, (cb + 1) * 512)
                            if moe:
                                wv = wgt.c(ti * NEXP + e, ti * NEXP + e + 1)
                                P.op('dve', lambda h, xv=xv, bank=bank, wv=wv: h.scalar_tensor_tensor(
                                    out=xv.ap, in0=bank.t[:, 0:512], scalar=wv.ap, in1=xv.ap, op0=ALU.mult, op1=ALU.add),
                                    reads=[bank.c(0, 512), wv, xv], writes=[xv])
                            else:
                                P.op('dve', lambda h, xv=xv, bank=bank: h.tensor_tensor(out=xv.ap, in0=bank.t[:, 0:512], in1=xv.ap, op=ALU.add),
                                     reads=[bank.c(0, 512), xv], writes=[xv])

            if final:
                load('sp', fgb.c(0, D), fing.partition_broadcast(128))
            for ti in range(ST):
                g = st * ST + ti
                xv = xt[ti].c(0, D)
                if final:
                    ssq = stat.c(0, 1)
                    rs = stat.c(1, 2)
                    hsv = hs.c(0, D)
                    P.op('act', lambda h, xv=xv: h.activation(out=hsv.ap, in_=xv.ap, func=AF.Square, accum_out=ssq.ap),
                         reads=[xv], writes=[hsv, ssq])
                    rstd_from_ssq(ssq, D, rs)
                    P.op('dve', lambda h, xv=xv: h.scalar_tensor_tensor(out=xv.ap, in0=xv.ap, scalar=rs.ap, in1=fgb.t[:, :], op0=ALU.mult, op1=ALU.mult),
                         reads=[xv, rs, fgb.c(0, D)], writes=[xv])
                out_sems.append(P.dma('sp', lambda h, xv=xv, g=g: h.dma_start(out=x_out[g * 128:(g + 1) * 128, :], in_=xv.ap), reads=[xv]))

        if moe and phase == 2:
            moe_experts()
            moe_final()
        if phase == 1:
            out_sems.append(P.dma('sp', lambda h: h.dma_start(out=lst_o[:, :], in_=Sf.t[:, :]), reads=[Sf.c(0, 1024)]))
            out_sems.append(P.dma('sp', lambda h: h.dma_start(out=bsum_o[:, :], in_=bsum.t[:, :]), reads=[bsum.c(0, 4)]))
            lastu = Z(ST - 1, 0, 1024)
            out_sems.append(P.dma('sp', lambda h: h.dma_start(out=ulast_o[:, :], in_=lastu.ap), reads=[lastu]))
        fin = {}
        for s, v in out_sems:
            fin[id(s)] = (s, max(v, fin.get(id(s), (s, 0))[1]))
        P.wait_all('sp', list(fin.values()))

        with nc.Block() as block:
            @block.tensor
            def _(h):
                P.emit_engine('pe', h)

            @block.scalar
            def _(h):
                P.emit_engine('act', h)

            @block.vector
            def _(h):
                P.emit_engine('dve', h)

            @block.gpsimd
            def _(h):
                P.emit_engine('pool', h)

            @block.sync
            def _(h):
                P.emit_engine('sp', h)
    return nc


_PROG_CACHE = {}


def _get(kind, phase, final):
    key = (kind, phase, final)
    if key not in _PROG_CACHE:
        _PROG_CACHE[key] = build(kind, phase, final)
    return _PROG_CACHE[key]


def kernel(x, mix_norm, w_in, w_pool, pool_scale, w_gate_up, b_gate, gla_norm, w_out,
           ffn_norm, dense_w_gate, dense_w_up, dense_w_down, w_router,
           exp_w_gate, exp_w_up, exp_w_down, final_norm, _nlayers=2):
    f = lambda a: np.ascontiguousarray(np.asarray(a, dtype=np.float32))
    x = f(x).reshape(SEQ, D)
    consts = [_consts(c) for c in range(NCORES)]
    xs = [np.ascontiguousarray(x[c * TPC:(c + 1) * TPC]) for c in range(NCORES)]
    bf = ml_dtypes.bfloat16
    for l in range(_nlayers):
        kind = 'dense' if l % 2 == 0 else 'moe'
        common = {
            "mixg": _fm(f(mix_norm[l])), "w_in": f(w_in[l]), "wgu": f(w_gate_up[l]),
            "bgate": f(b_gate[l]).reshape(1, 512),
        }
        nc1 = _get(kind, 1, False)
        maps = []
        for c in range(NCORES):
            m = dict(common)
            m["x_in"] = xs[c]
            for k in ("ident_f", "ident_b", "trin"):
                m[k] = consts[c][k]
            maps.append(m)
        r1 = run_bass_kernel_spmd(nc1, maps, core_ids=list(range(NCORES))).results
        lall = np.concatenate([np.asarray(r["lst"]) for r in r1], axis=0)
        ball = np.concatenate([np.asarray(r["bsum"]) for r in r1], axis=0)
        final = l == 1
        nc2 = _get(kind, 2, final)
        maps = []
        for c in range(NCORES):
            m = dict(common)
            m["x_in"] = xs[c]
            m.update(consts[c])
            m["lall"] = lall
            m["ball"] = ball
            m["uhalo"] = np.asarray(r1[c - 1]["ulast"]) if c > 0 else np.zeros((128, 1024), bf)
            m["kT_i"] = np.asarray(r1[c]["kT_o"])
            m["v_i"] = np.asarray(r1[c]["v_o"])
            m["gl_i"] = np.asarray(r1[c]["gl_o"])
            m["w_pool"] = f(w_pool[l]).reshape(4 * 256, 256)
            m["pscale"] = _fm(f(pool_scale[l]).reshape(-1))
            m["gnorm"] = f(gla_norm[l]).reshape(1, 1024)
            m["w_out"] = f(w_out[l])
            m["ffng"] = _fm(f(ffn_norm[l]))
            if kind == 'moe':
                i = l // 2
                m["w_router"] = f(w_router[i])
                m["wg"] = f(exp_w_gate[i]).reshape(NEXP * D, FF_EXP)
                m["wu"] = f(exp_w_up[i]).reshape(NEXP * D, FF_EXP)
                m["wd"] = f(exp_w_down[i]).reshape(NEXP * FF_EXP, D)
            else:
                i = l // 2
                m["wg"] = f(dense_w_gate[i])
                m["wu"] = f(dense_w_up[i])
                m["wd"] = f(dense_w_down[i])
            if final:
                m["fing"] = f(final_norm).reshape(1, D)
            maps.append(m)
        r2 = run_bass_kernel_spmd(nc2, maps, core_ids=list(range(NCORES))).results
        xs = [np.asarray(r["x_out"]) for r in r2]
    out = np.concatenate(xs, axis=0).reshape(1, SEQ, D).astype(np.float32)
    return out
```

```python
import numpy as np
import ml_dtypes
import concourse.bass as bass
import concourse.mybir as mybir
from concourse.bass_utils import run_bass_kernel_spmd

F32 = mybir.dt.float32
BF16 = mybir.dt.bfloat16
AF = mybir.ActivationFunctionType
ALU = mybir.AluOpType
AX = mybir.AxisListType

NCORES = 8
D = 2048
SEQ = 16384
TPC = SEQ // NCORES
NT = TPC // 128
ST = 4
NST = NT // ST
KC = D // 128
INW = 4112
FF_DENSE = 5632
FF_EXP = 7168
NEXP = 8
EPS = 1e-6
POOL_WINDOWS = (2, 4, 8, 16)
WSLOT = 8192
NWSLOT = 3
STOP = 99
EVAC_ACT_SCALE = True
STATIC_BOUNDS = False


class TT:
    def __init__(self, t, esz, name, holder=None, base=0):
        self.t = t
        self.esz = esz
        self.name = name
        self.H = holder if holder is not None else [[]]
        self.base = base
        self.whole = False

    @property
    def hist(self):
        return self.H[0]

    @hist.setter
    def hist(self, v):
        self.H[0] = v

    def c(self, lo, hi, p0=0, p1=None):
        ap = self.t[p0:p1, lo:hi] if p1 is not None else (self.t[p0:, lo:hi] if p0 else self.t[:, lo:hi])
        if self.whole:
            return V(self, ap, 0, 1 << 20)
        return V(self, ap, self.base + lo * self.esz, self.base + hi * self.esz)

    def view(self, lo_bytes, n, dt, name=None):
        esz = 4 if dt == F32 else 2
        a = lo_bytes // self.esz
        b = a + (n * esz) // self.esz
        ap = self.t[:, a:b]
        if esz != self.esz or dt != getattr(self, "dt", None):
            ap = ap.bitcast(dt)
        tt = TT(ap, esz, name or self.name, holder=self.H, base=self.base + lo_bytes)
        tt.dt = dt
        return tt


class V:
    def __init__(self, tt, ap, lo, hi):
        self.tt = tt
        self.ap = ap
        self.lo = lo
        self.hi = hi

    def r(self, pat, **kw):
        return V(self.tt, self.ap.rearrange(pat, **kw), self.lo, self.hi)

    def s(self, *key):
        return V(self.tt, self.ap[key], self.lo, self.hi)


class Eng:
    def __init__(self, key, sem):
        self.key = key
        self.sem = sem
        self.count = 0
        self.known = {}
        self.ops = []
        self.lanes = []
        self.lane_val = []
        self.lane_next = 0


class Prog:
    def __init__(self, nc):
        self.nc = nc
        self.E = {}
        self.sems = {}

    def add_engine(self, key, sem, lanes=()):
        e = Eng(key, sem)
        self.sems[id(sem)] = sem
        e.lanes = list(lanes)
        e.lane_val = [0] * len(lanes)
        for s in lanes:
            self.sems[id(s)] = s
        self.E[key] = e

    def _need(self, e, reads, writes):
        need = {}

        def add(h):
            if h[2] == 'pe' and e.key == 'pe' and h[3] == id(e.sem):
                return
            if need.get(h[3], 0) < h[4]:
                need[h[3]] = h[4]
        for v in reads:
            whole = v.tt.whole
            for h in v.tt.hist:
                if (h[5] or (whole and h[2] != e.key)) and h[0] < v.hi and v.lo < h[1]:
                    add(h)
        for v in writes:
            for h in v.tt.hist:
                if h[0] < v.hi and v.lo < h[1]:
                    add(h)
        waits = []
        for sid, val in need.items():
            if e.known.get(sid, 0) < val:
                e.known[sid] = val
                waits.append((self.sems[sid], val))
        return waits

    def _record(self, e, reads, writes, sid, val):
        for v in writes:
            hist = v.tt.hist
            v.tt.hist = [h for h in hist if not (h[0] >= v.lo and h[1] <= v.hi)]
            v.tt.hist.append([v.lo, v.hi, e.key, sid, val, True])
        for v in reads:
            hist = v.tt.hist
            v.tt.hist = [h for h in hist if not ((not h[5]) and h[3] == sid and h[0] >= v.lo and h[1] <= v.hi)]
            v.tt.hist.append([v.lo, v.hi, e.key, sid, val, False])

    def op(self, ek, fn, reads=(), writes=(), sig=True):
        e = self.E[ek]
        waits = self._need(e, reads, writes)
        val = e.count + 1
        if sig:
            e.count = val
        e.ops.append((waits, fn, (e.sem, 1) if sig else None))
        self._record(e, reads, writes, id(e.sem), val)

    def dma(self, ek, fn, reads=(), writes=()):
        e = self.E[ek]
        li = e.lane_next
        e.lane_next = (li + 1) % len(e.lanes)
        sem = e.lanes[li]
        waits = self._need(e, reads, writes)
        prev = e.lane_val[li]
        if prev > 0 and e.known.get(id(sem), 0) < prev:
            e.known[id(sem)] = prev
            waits.append((sem, prev))
        val = prev + 16
        e.lane_val[li] = val
        e.ops.append((waits, fn, (sem, 16)))
        self._record(e, reads, writes, id(sem), val)
        return sem, val

    def wait_all(self, ek, items):
        e = self.E[ek]
        waits = [(s, v) for (s, v) in items]
        e.ops.append((waits, None, None))

    def emit_engine(self, ek, h):
        for waits, fn, inc in self.E[ek].ops:
            for s, v in waits:
                h.wait_ge(s, v)
            if fn is None:
                continue
            ins = fn(h)
            if inc is not None:
                ins.then_inc(inc[0], inc[1])


def _pool_mats(first_core):
    cur = np.zeros((4, 128, 128), np.float32)
    cur0 = np.zeros((4, 128, 128), np.float32)
    prev = np.zeros((4, 128, 128), np.float32)
    for g, w in enumerate(POOL_WINDOWS):
        for t in range(128):
            for s in range(t - w + 1, t + 1):
                if s >= 0:
                    cur[g, s, t] += 1.0 / w
                else:
                    prev[g, 128 + s, t] += 1.0 / w
            cur[g, t, t] -= 1.0
            cnt = min(t + 1, w)
            for s in range(max(0, t - w + 1), t + 1):
                cur0[g, s, t] += 1.0 / cnt
            cur0[g, t, t] -= 1.0
    return cur, (cur0 if first_core else cur), prev


def _consts(core):
    bf = ml_dtypes.bfloat16
    c = {}
    c["ident_f"] = np.eye(128, dtype=np.float32)
    c["ident_b"] = np.eye(128, dtype=np.float32).astype(bf)
    j = np.arange(128)[:, None]
    i = np.arange(128)[None, :]
    tri = (j <= i).astype(np.float32)
    c["trin"] = (tri * (-1.0 / 16.0)).astype(bf)
    c["mask4"] = np.tile(tri, (1, 4)).astype(bf)
    cur, cur0, prev = _pool_mats(core == 0)
    c["mcur"] = np.ascontiguousarray(cur.transpose(1, 0, 2).reshape(128, 512)).astype(bf)
    c["mcur0"] = np.ascontiguousarray(cur0.transpose(1, 0, 2).reshape(128, 512)).astype(bf)
    c["mprev"] = np.ascontiguousarray(prev.transpose(1, 0, 2).reshape(128, 512)).astype(bf)
    m = np.zeros((128, 8), np.float32)
    m[:, :core] = 1.0
    c["cmask"] = m
    c["iota"] = np.tile(np.arange(640, dtype=np.float32)[None, :], (128, 1))
    c["ustrict"] = (j < i).astype(np.float32).astype(bf)
    c["ones"] = np.ones((128, 128), np.float32).astype(bf)
    return c


def _fm(vec):
    return np.ascontiguousarray(vec.reshape(-1, 128).T)


def build(layer_kind, phase, final):
    nc = bass.Bass("TRN2", target_bir_lowering=False)
    moe = layer_kind == 'moe'
    FF = FF_EXP if moe else FF_DENSE
    NE = NEXP if moe else 1
    NFC = FF // 128
    HALF = NFC // 2

    def din(name, shape, dt=F32):
        return nc.dram_tensor(name, list(shape), dt, kind="ExternalInput").ap()

    def dout(name, shape, dt=F32):
        return nc.dram_tensor(name, list(shape), dt, kind="ExternalOutput").ap()

    x_in = din("x_in", [TPC, D])
    mixg = din("mixg", [128, KC])
    w_in = din("w_in", [D, INW])
    wgu = din("wgu", [16, 512])
    bgate = din("bgate", [1, 512])
    ident_f_d = din("ident_f", [128, 128])
    ident_b_d = din("ident_b", [128, 128], BF16)
    trin_d = din("trin", [128, 128], BF16)
    if phase == 1:
        lst_o = dout("lst", [128, 1024])
        bsum_o = dout("bsum", [128, 4])
        ulast_o = dout("ulast", [128, 1024], BF16)
        kT_o = dout("kT_o", [NST * 128, 2048], BF16)
        v_o = dout("v_o", [TPC, 1024], BF16)
        gl_o = dout("gl_o", [NST * 16, 512], BF16)
    else:
        mask4_d = din("mask4", [128, 512], BF16)
        mcur_d = din("mcur", [128, 512], BF16)
        mcur0_d = din("mcur0", [128, 512], BF16)
        mprev_d = din("mprev", [128, 512], BF16)
        cmask_d = din("cmask", [128, 8])
        lall = din("lall", [NCORES * 128, 1024])
        ball = din("ball", [NCORES * 128, 4])
        uhalo = din("uhalo", [128, 1024], BF16)
        kT_i = din("kT_i", [NST * 128, 2048], BF16)
        v_i = din("v_i", [TPC, 1024], BF16)
        gl_i = din("gl_i", [NST * 16, 512], BF16)
        w_pool = din("w_pool", [4 * 256, 256])
        pscale = din("pscale", [128, 8])
        gnorm = din("gnorm", [1, 1024])
        w_out = din("w_out", [D, D])
        ffng = din("ffng", [128, KC])
        if moe:
            w_router = din("w_router", [D, NEXP])
            wg_d = din("wg", [NEXP * D, FF])
            wu_d = din("wu", [NEXP * D, FF])
            wd_d = din("wd", [NEXP * FF, D])
        else:
            wg_d = din("wg", [D, FF])
            wu_d = din("wu", [D, FF])
            wd_d = din("wd", [FF, D])
        if final:
            fing = din("fing", [1, D])
        x_out = dout("x_out", [TPC, D])
        if moe:
            iota_d = din("iota", [128, 640])
            ustrict_d = din("ustrict", [128, 128], BF16)
            ones_d = din("ones", [128, 128], BF16)
            h2s = nc.dram_tensor("h2s", [TPC, D], BF16, kind="Internal").ap()

    import contextlib
    es = contextlib.ExitStack()
    with es:
        def sb(name, n, dt=F32, p=128):
            t = es.enter_context(nc.sbuf_tensor("sb_" + name, [p, n], dt))
            tt = TT(t, 4 if dt == F32 else 2, name)
            tt.dt = dt
            return tt

        def ps(name, n, dt=F32):
            t = es.enter_context(nc.psum_tensor("ps_" + name, [128, n], dt))
            tt = TT(t, 4 if dt == F32 else 2, name)
            tt.whole = True
            return tt

        def sem(name):
            return es.enter_context(nc.semaphore(name))

        P = Prog(nc)
        P.add_engine('pe', sem("s_pe"))
        P.add_engine('act', sem("s_act"))
        P.add_engine('dve', sem("s_dve"))
        P.add_engine('pool', sem("s_pool"), [sem("lp%d" % i) for i in range(16)])
        P.add_engine('sp', sem("s_sp"), [sem("ls%d" % i) for i in range(16)])

        xtall = sb("xtall", ST * D)
        xt = [xtall.view(i * D * 4, D, F32, "xt%d" % i) for i in range(ST)]
        hsf = sb("hsf", 2 * D)
        hs = hsf.view(0, D, F32, "hs")
        mixc = sb("mixc", 5 * 1024)
        hT = sb("hT", KC * 512, BF16)
        wsl = [sb("wsl%d" % i, WSLOT, BF16) for i in range(NWSLOT)]
        arena = sb("arena", 27 * 1024, BF16)
        mixT = hT
        stat = sb("stat", 64)
        cst_identf = sb("identf", 128)
        cst_identb = sb("identb", 128, BF16)
        cst_trin = sb("trin", 128, BF16)
        g_mix = sb("g_mix", KC)
        wgu_a = sb("wgu_a", 512, BF16, p=32)
        Sf = mixc.view(0, 1024, F32, "Sf")
        Sb = mixc.view(4096, 1024, BF16, "Sb")
        glT = sb("glT", 512, BF16, p=32)
        if phase == 2:
            cst_mask4 = sb("mask4", 512, BF16)
            cst_mcur = sb("mcur", 512, BF16)
            cst_mcur0 = sb("mcur0", 512, BF16)
            cst_mprev = sb("mprev", 512, BF16)
            cmask = sb("cmask", 8)
            g_ffn = sb("g_ffn", KC)
            psc = sb("psc", 8)
            gnb = mixc.view(6144, 1024, F32, "gnb")
            wpool = mixc.view(10240, 8 * 256, BF16, "wpool")
            uprev = mixc.view(14336, 1024, BF16, "uprev")
            if moe:
                wr = sb("wr", KC * NEXP)
                hTf = hsf.view(D * 4, KC * 128, F32, "hTf")
                wgt_all = sb("wgt_all", NT * NEXP)
                mask_all = sb("mask_all", NT * NEXP)
                pos_all = sb("pos_all", NT * NEXP)
                carry = sb("carry", NEXP)
                maskb = sb("maskb", NEXP, BF16)
                cst_ustrict = sb("ustrict", 128, BF16)
                cst_ones = sb("ones", 128, BF16)
                rt = sb("rt", 64)
            if final:
                fgb = hTf if moe else sb("fgb", D)
        bsum = sb("bsum", 4)

        AO = {}
        off = 0

        def carve(name, n_bf16):
            nonlocal off
            AO[name] = (off, off + n_bf16)
            off += n_bf16
        carve("z", ST * 3072)
        carve("qT", ST * 512)
        carve("kT", ST * 512)
        carve("ysp", 1024)
        carve("sp", 512)
        carve("epos", 1024)
        carve("eneg", 1024)
        carve("qd", 512)
        carve("kd", 512)
        carve("kteT", 512)
        carve("kte", 512)
        carve("att", 512)
        carve("otmp", 2048)
        carve("sr", 1024)
        carve("og", 1024)
        carve("dT", 1024)
        assert off <= 27 * 1024, off

        def A(name, lo=0, hi=None, f32=False):
            a, b = AO[name]
            if f32:
                n = (b - a) // 2
                hi_ = n if hi is None else hi
                ap = arena.t[:, a:b].bitcast(F32)[:, lo:hi_]
                return V(arena, ap, a * 2 + lo * 4, a * 2 + hi_ * 4)
            hi_ = (b - a) if hi is None else hi
            return arena.c(a + lo, a + hi_)

        def ACT_(fc, lo=0, hi=512):
            base = fc * 512
            return arena.c(base + lo, base + hi)
        assert HALF * 512 <= 27 * 1024

        pb = [ps("pb%d" % i, 512) for i in range(7)]
        pbb = ps("pbb", 1024, BF16)

        def load(eng, dst, src_ap, **kw):
            P.dma(eng, lambda h: h.dma_start(out=dst.ap, in_=src_ap, **kw), writes=[dst])

        rr = [0]

        def evac_copy(dst, src, scale_ap=None):
            rr[0] ^= 1
            use_act = rr[0]
            if scale_ap is not None:
                bi = [i for i, b_ in enumerate(pb) if b_ is src.tt]
                use_act = EVAC_ACT_SCALE and bool(bi) and (bi[0] % 2 == 0)
            if use_act:
                if scale_ap is None:
                    P.op('act', lambda h: h.activation(out=dst.ap, in_=src.ap, func=AF.Copy), reads=[src], writes=[dst])
                else:
                    P.op('act', lambda h: h.activation(out=dst.ap, in_=src.ap, func=AF.Identity, scale=scale_ap.ap),
                         reads=[src, scale_ap], writes=[dst])
            else:
                if scale_ap is None:
                    P.op('dve', lambda h: h.tensor_copy(out=dst.ap, in_=src.ap), reads=[src], writes=[dst])
                else:
                    P.op('dve', lambda h: h.tensor_scalar(out=dst.ap, in0=src.ap, scalar1=scale_ap.ap, scalar2=None,
                                                          op0=ALU.mult), reads=[src, scale_ap], writes=[dst])

        def mm(out, lhsT, rhs, start, stop, sgc=False):
            P.op('pe', lambda h: h.matmul(out.ap, lhsT=lhsT.ap, rhs=rhs.ap, start=start, stop=stop, skip_group_check=sgc),
                 reads=[lhsT, rhs], writes=[out], sig=stop)

        def tr(out, in_, ident):
            P.op('pe', lambda h: h.transpose(out.ap, in_.ap, ident.ap), reads=[in_, ident], writes=[out])

        wrr = [0]

        def wslot():
            s = wsl[wrr[0] % NWSLOT]
            wrr[0] += 1
            return s

        def load_w(dst_view, src_ap):
            P.dma('pool', lambda h: h.dma_start(out=dst_view.ap, in_=src_ap, max_dma_last_dim=8192), writes=[dst_view])

        def rstd_from_ssq(ssq, n, out):
            P.op('dve', lambda h: h.tensor_scalar(out=out.ap, in0=ssq.ap, scalar1=1.0 / n, scalar2=EPS,
                                                  op0=ALU.mult, op1=ALU.add), reads=[ssq], writes=[out])
            P.op('act', lambda h: h.activation(out=out.ap, in_=out.ap, func=AF.Sqrt), reads=[out], writes=[out])
            P.op('dve', lambda h: h.reciprocal(out=out.ap, in_=out.ap), reads=[out], writes=[out])

        def norm_tile(x_t, gain, ti, want_f32=False):
            ssq = stat.c(0, 1)
            rs = stat.c(1, 2)
            hsv = hs.c(0, D)
            xv = x_t.c(0, D)
            if STOP <= 0:
                return
            P.op('act', lambda h: h.activation(out=hsv.ap, in_=xv.ap, func=AF.Square, accum_out=ssq.ap),
                 reads=[xv], writes=[hsv, ssq])
            rstd_from_ssq(ssq, D, rs)
            P.op('dve', lambda h: h.tensor_scalar(out=hsv.ap, in0=xv.ap, scalar1=rs.ap, scalar2=None, op0=ALU.mult),
                 reads=[xv, rs], writes=[hsv])
            if STOP <= 0.3:
                return
            for b in range(4):
                for j in range(4):
                    kc = b * 4 + j
                    tr(pb[b].c(j * 128, (j + 1) * 128), hs.c(kc * 128, (kc + 1) * 128), cst_identf.c(0, 128))
            if STOP <= 0.6:
                return
            for b in range(4):
                for j in range(4):
                    kc = b * 4 + j
                    src = pb[b].c(j * 128, (j + 1) * 128)
                    evac_copy(hT.c(kc * 512 + ti * 128, kc * 512 + (ti + 1) * 128), src, gain.c(kc, kc + 1))
                    if want_f32:
                        evac_copy(hTf.c(kc * 128, (kc + 1) * 128), src, gain.c(kc, kc + 1))

        CAP = 640
        if moe and phase == 2:
            XO = TT(x_out, 1, "x_out_d")
            H2S = TT(h2s, 1, "h2s_d")

        def xo_cells(tt, dg=None):
            if dg is None:
                return tt * 4, tt * 4 + 4
            return tt * 4 + dg, tt * 4 + dg + 1

        def moe_route_tile(ti, g):
            x_t = xt[ti]
            ssq = stat.c(0, 1)
            rs = stat.c(1, 2)
            hsv = hs.c(0, D)
            xv = x_t.c(0, D)
            P.op('act', lambda h: h.activation(out=hsv.ap, in_=xv.ap, func=AF.Square, accum_out=ssq.ap),
                 reads=[xv], writes=[hsv, ssq])
            rstd_from_ssq(ssq, D, rs)
            P.op('dve', lambda h: h.tensor_scalar(out=hsv.ap, in0=xv.ap, scalar1=rs.ap, scalar2=None, op0=ALU.mult),
                 reads=[xv, rs], writes=[hsv])
            P.dma('pool', lambda h: h.dma_start(out=h2s[g * 128:(g + 1) * 128, :], in_=hsv.ap, max_dma_last_dim=8192),
                  reads=[hsv], writes=[V(H2S, None, 0, 1)])
            lo, hi = xo_cells(g)
            P.dma('sp', lambda h: h.dma_start(out=x_out[g * 128:(g + 1) * 128, :], in_=xv.ap), reads=[xv], writes=[V(XO, None, lo, hi)])
            for b in range(4):
                for j in range(4):
                    kc = b * 4 + j
                    tr(pb[b].c(j * 128, (j + 1) * 128), hs.c(kc * 128, (kc + 1) * 128), cst_identf.c(0, 128))
            for b in range(4):
                for j in range(4):
                    kc = b * 4 + j
                    evac_copy(hTf.c(kc * 128, (kc + 1) * 128), pb[b].c(j * 128, (j + 1) * 128), g_ffn.c(kc, kc + 1))
            lgp = pb[6].c(0, NEXP)
            for kc in range(KC):
                mm(lgp, hTf.c(kc * 128, (kc + 1) * 128), wr.c(kc * NEXP, (kc + 1) * NEXP), kc == 0, kc == KC - 1)
            lg = rt.c(0, 8)
            lg2 = rt.c(8, 16)
            eq1 = rt.c(16, 24)
            eq2 = rt.c(24, 32)
            m1 = rt.c(32, 33)
            m2 = rt.c(33, 34)
            ex = rt.c(34, 35)
            w1 = rt.c(35, 36)
            w2 = rt.c(36, 37)
            wv = wgt_all.c(g * NEXP, (g + 1) * NEXP)
            mk = mask_all.c(g * NEXP, (g + 1) * NEXP)
            pv = pos_all.c(g * NEXP, (g + 1) * NEXP)
            P.op('dve', lambda h: h.tensor_copy(out=lg.ap, in_=lgp.ap), reads=[lgp], writes=[lg])
            P.op('dve', lambda h: h.tensor_reduce(out=m1.ap, in_=lg.ap, axis=AX.X, op=ALU.max), reads=[lg], writes=[m1])
            P.op('dve', lambda h: h.tensor_scalar(out=eq1.ap, in0=lg.ap, scalar1=m1.ap, scalar2=None, op0=ALU.is_equal),
                 reads=[lg, m1], writes=[eq1])
            P.op('dve', lambda h: h.scalar_tensor_tensor(out=lg2.ap, in0=eq1.ap, scalar=-1e30, in1=lg.ap, op0=ALU.mult, op1=ALU.add),
                 reads=[eq1, lg], writes=[lg2])
            P.op('dve', lambda h: h.tensor_reduce(out=m2.ap, in_=lg2.ap, axis=AX.X, op=ALU.max), reads=[lg2], writes=[m2])
            P.op('dve', lambda h: h.tensor_scalar(out=eq2.ap, in0=lg2.ap, scalar1=m2.ap, scalar2=None, op0=ALU.is_equal),
                 reads=[lg2, m2], writes=[eq2])
            P.op('dve', lambda h: h.tensor_tensor(out=ex.ap, in0=m2.ap, in1=m1.ap, op=ALU.subtract), reads=[m1, m2], writes=[ex])
            P.op('act', lambda h: h.activation(out=ex.ap, in_=ex.ap, func=AF.Exp), reads=[ex], writes=[ex])
            P.op('dve', lambda h: h.tensor_scalar(out=w1.ap, in0=ex.ap, scalar1=1.0, scalar2=None, op0=ALU.add), reads=[ex], writes=[w1])
            P.op('dve', lambda h: h.reciprocal(out=w1.ap, in_=w1.ap), reads=[w1], writes=[w1])
            P.op('dve', lambda h: h.tensor_tensor(out=w2.ap, in0=ex.ap, in1=w1.ap, op=ALU.mult), reads=[ex, w1], writes=[w2])
            P.op('dve', lambda h: h.tensor_scalar(out=wv.ap, in0=eq1.ap, scalar1=w1.ap, scalar2=None, op0=ALU.mult),
                 reads=[eq1, w1], writes=[wv])
            P.op('dve', lambda h: h.scalar_tensor_tensor(out=wv.ap, in0=eq2.ap, scalar=w2.ap, in1=wv.ap, op0=ALU.mult, op1=ALU.add),
                 reads=[eq2, w2, wv], writes=[wv])
            P.op('dve', lambda h: h.tensor_tensor(out=mk.ap, in0=eq1.ap, in1=eq2.ap, op=ALU.add), reads=[eq1, eq2], writes=[mk])
            mb = maskb.c(0, NEXP)
            P.op('dve', lambda h: h.tensor_copy(out=mb.ap, in_=mk.ap), reads=[mk], writes=[mb])
            mm(pb[5].c(0, NEXP), cst_ustrict.c(0, 128), mb, True, True)
            mm(pb[5].c(NEXP, 2 * NEXP), cst_ones.c(0, 128), mb, True, True)
            cv = carry.c(0, NEXP)
            P.op('dve', lambda h: h.tensor_tensor(out=pv.ap, in0=pb[5].t[:, 0:NEXP], in1=cv.ap, op=ALU.add),
                 reads=[pb[5].c(0, NEXP), cv], writes=[pv])
            P.op('dve', lambda h: h.tensor_tensor(out=cv.ap, in0=pb[5].t[:, NEXP:2 * NEXP], in1=cv.ap, op=ALU.add),
                 reads=[pb[5].c(0, NEXP), cv], writes=[cv])

        def moe_experts():
            NPART = 8
            PCH = NFC // NPART
            Pe = xtall.view(0, NT * CAP, BF16, "Pe")
            sg = xtall.view(NT * CAP * 2, CAP, F32, "sg")
            actq = xtall.view(NT * CAP * 2 + CAP * 4, PCH * CAP, BF16, "actq")
            assert NT * CAP * 2 + CAP * 4 + PCH * CAP * 2 <= ST * D * 4
            yacc = arena.view(0, 5 * D, F32, "yacc")
            PT = arena.view(5 * D * 4, CAP, BF16, "PT")
            contrib = [arena.view(5 * D * 4 + CAP * 2 + i * 2048, 512, F32, "contrib%d" % i) for i in range(4)]
            iota = arena.view(5 * D * 4 + CAP * 2 + 4 * 2048, CAP, F32, "iota")
            PT2 = arena.view(5 * D * 4 + CAP * 2 + 4 * 2048 + CAP * 4, CAP, BF16, "PT2")
            PTs = [PT, PT2]
            assert 5 * D * 4 + CAP * 2 + 4 * 2048 + CAP * 4 + CAP * 2 <= 27 * 1024 * 2
            hTe = mixc.view(0, KC * CAP, BF16, "hTe")
            yb = mixc.view(0, 5 * D, BF16, "yb")
            h2c = [hT.view(0, NT * 512, BF16, "h2c0"), hsf.view(0, NT * 512, BF16, "h2c1")]
            load('sp', iota.c(0, CAP), iota_d[:, :])
            for e in range(NEXP):
                for tt in range(NT):
                    pv = pos_all.c(tt * NEXP + e, tt * NEXP + e + 1)
                    mk = mask_all.c(tt * NEXP + e, tt * NEXP + e + 1)
                    pe_ = Pe.c(tt * CAP, (tt + 1) * CAP)
                    P.op('dve', lambda h, pe_=pe_, pv=pv, mk=mk: h.tensor_scalar(
                        out=pe_.ap, in0=iota.t[:, :], scalar1=pv.ap, scalar2=mk.ap, op0=ALU.is_equal, op1=ALU.mult),
                        reads=[iota.c(0, CAP), pv, mk], writes=[pe_])
                for kg in range(4):
                    hc = h2c[kg % 2]
                    hcv = hc.c(0, NT * 512)
                    P.dma('sp', lambda h, hcv=hcv, kg=kg: h.dma_start(
                        out=hcv.ap.rearrange("p (a b) -> p a b", b=512),
                        in_=h2s[:, kg * 512:(kg + 1) * 512].rearrange("(a p) c -> p a c", p=128)),
                        reads=[V(H2S, None, 0, 1)], writes=[hcv])
                    for k4 in range(4):
                        kc = kg * 4 + k4
                        for cg, (c0, c1) in enumerate(((0, 512), (512, CAP))):
                            bank = pb[(kc * 2 + cg) % 4]
                            tts = ([tt for tt in range(NT) if 128 * (tt + 1) > c0] if STATIC_BOUNDS else []) or list(range(NT))
                            for tt in tts:
                                ce = max(min(c1, 128 * (tt + 1)), c0 + 128) if (STATIC_BOUNDS and NT * 128 >= CAP) else c1
                                mm(bank.c(0, ce - c0), hc.c(tt * 512 + k4 * 128, tt * 512 + (k4 + 1) * 128),
                                   Pe.c(tt * CAP + c0, tt * CAP + ce), tt == tts[0], tt == tts[-1], sgc=STATIC_BOUNDS)
                            evac_copy(hTe.c(kc * CAP + c0, kc * CAP + c1), bank.c(0, c1 - c0), g_ffn.c(kc, kc + 1))
                for qi in range(NPART):
                    f0 = qi * PCH
                    fl = 0
                    for nb in (2, 2, 2, 1):
                        ws = wslot()
                        c0 = (f0 + fl) * 128
                        gv = ws.c(0, KC * nb * 128)
                        uv = ws.c(KC * 256, KC * 256 + KC * nb * 128)
                        load_w(gv.r("p (a b) -> p a b", b=nb * 128), wg_d[e * D:(e + 1) * D, c0:c0 + nb * 128].rearrange("(a p) c -> p a c", p=128))
                        load_w(uv.r("p (a b) -> p a b", b=nb * 128), wu_d[e * D:(e + 1) * D, c0:c0 + nb * 128].rearrange("(a p) c -> p a c", p=128))
                        for j in range(nb):
                            fc = fl + j
                            par = fc % 2
                            g0, u0, xb = pb[3 * par], pb[3 * par + 1], pb[3 * par + 2]
                            wgs = lambda kc: ws.c(kc * nb * 128 + j * 128, kc * nb * 128 + (j + 1) * 128)
                            wus = lambda kc: ws.c(KC * 256 + kc * nb * 128 + j * 128, KC * 256 + kc * nb * 128 + (j + 1) * 128)
                            for kc in range(KC):
                                mm(g0.c(0, 512), wgs(kc), hTe.c(kc * CAP, kc * CAP + 512), kc == 0, kc == KC - 1)
                            for kc in range(KC):
                                mm(xb.c(0, 128), wgs(kc), hTe.c(kc * CAP + 512, (kc + 1) * CAP), kc == 0, kc == KC - 1)
                            for kc in range(KC):
                                mm(u0.c(0, 512), wus(kc), hTe.c(kc * CAP, kc * CAP + 512), kc == 0, kc == KC - 1)
                            for kc in range(KC):
                                mm(xb.c(128, 256), wus(kc), hTe.c(kc * CAP + 512, (kc + 1) * CAP), kc == 0, kc == KC - 1)
                            s0, s1 = sg.c(0, 512), sg.c(512, CAP)
                            P.op('act', lambda h, s0=s0, g0=g0: h.activation(out=s0.ap, in_=g0.t[:, 0:512], func=AF.Silu),
                                 reads=[g0.c(0, 512)], writes=[s0])
                            P.op('act', lambda h, s1=s1, xb=xb: h.activation(out=s1.ap, in_=xb.t[:, 0:128], func=AF.Silu),
                                 reads=[xb.c(0, 128)], writes=[s1])
                            a0 = actq.c(fc * CAP, fc * CAP + 512)
                            a1 = actq.c(fc * CAP + 512, (fc + 1) * CAP)
                            P.op('dve', lambda h, a0=a0, s0=s0, u0=u0: h.tensor_tensor(out=a0.ap, in0=u0.t[:, 0:512], in1=s0.ap, op=ALU.mult),
                                 reads=[u0.c(0, 512), s0], writes=[a0])
                            P.op('dve', lambda h, a1=a1, s1=s1, xb=xb: h.tensor_tensor(out=a1.ap, in0=xb.t[:, 128:256], in1=s1.ap, op=ALU.mult),
                                 reads=[xb.c(0, 256), s1], writes=[a1])
                        fl += nb
                    for dg in range(4):
                        ws = wslot()
                        r0 = e * FF + f0 * 128
                        load_w(ws.c(0, PCH * 512).r("p (a b) -> p a b", b=512),
                               wd_d[r0:r0 + PCH * 128, dg * 512:(dg + 1) * 512].rearrange("(a p) c -> p a c", p=128))
                        for s5 in range(5):
                            bank = pb[4 + (dg * 5 + s5) % 3]
                            for q in range(PCH):
                                mm(bank.c(0, 512), actq.c(q * CAP + s5 * 128, q * CAP + (s5 + 1) * 128), ws.c(q * 512, (q + 1) * 512),
                                   q == 0, q == PCH - 1)
                            yv = yacc.c(s5 * D + dg * 512, s5 * D + (dg + 1) * 512)
                            if qi == 0:
                                evac_copy(yv, bank.c(0, 512))
                            elif qi < NPART - 1:
                                P.op('dve', lambda h, yv=yv, bank=bank: h.tensor_tensor(out=yv.ap, in0=bank.t[:, 0:512], in1=yv.ap, op=ALU.add),
                                     reads=[bank.c(0, 512), yv], writes=[yv])
                            else:
                                ybv = yb.c(s5 * D + dg * 512, s5 * D + (dg + 1) * 512)
                                P.op('dve', lambda h, yv=yv, ybv=ybv, bank=bank: h.tensor_tensor(out=ybv.ap, in0=bank.t[:, 0:512], in1=yv.ap, op=ALU.add),
                                     reads=[bank.c(0, 512), yv], writes=[ybv])
                def s5s(tt):
                    return [q for q in range(5) if q <= tt] if (STATIC_BOUNDS and NT * 128 >= CAP) else list(range(5))

                def prep(tt):
                    ss = s5s(tt)
                    for s5 in ss:
                        tr(pbb.c(s5 * 128, (s5 + 1) * 128), Pe.c(tt * CAP + s5 * 128, tt * CAP + (s5 + 1) * 128), cst_identb.c(0, 128))
                    ptv = PTs[tt % 2].c(0, len(ss) * 128)
                    P.op('act', lambda h, ptv=ptv, n=len(ss) * 128: h.activation(out=ptv.ap, in_=pbb.t[:, 0:n], func=AF.Copy),
                         reads=[pbb.c(0, CAP)], writes=[ptv])
                prep(0)
                for tt in range(NT):
                    if tt + 1 < NT:
                        prep(tt + 1)
                    PTc = PTs[tt % 2]
                    wv = wgt_all.c(tt * NEXP + e, tt * NEXP + e + 1)
                    for dg in range(4):
                        bank = pb[dg]
                        ss = s5s(tt)
                        for s5 in ss:
                            mm(bank.c(0, 512), PTc.c(s5 * 128, (s5 + 1) * 128), yb.c(s5 * D + dg * 512, s5 * D + (dg + 1) * 512),
                               s5 == ss[0], s5 == ss[-1])
                        cvw = contrib[dg].c(0, 512)
                        P.op('dve', lambda h, cvw=cvw, bank=bank, wv=wv: h.tensor_scalar(out=cvw.ap, in0=bank.t[:, 0:512], scalar1=wv.ap, scalar2=None, op0=ALU.mult),
                             reads=[bank.c(0, 512), wv], writes=[cvw])
                        lo, hi = xo_cells(tt, dg)
                        P.dma('pool', lambda h, cvw=cvw, tt=tt, dg=dg: h.dma_start(
                            out=x_out[tt * 128:(tt + 1) * 128, dg * 512:(dg + 1) * 512], in_=cvw.ap, accum_op=ALU.add),
                            reads=[cvw], writes=[V(XO, None, lo, hi)])

        def moe_final():
            load('sp', fgb.c(0, D), fing.partition_broadcast(128))
            for tt in range(NT):
                xv = xt[tt % ST].c(0, D)
                lo, hi = xo_cells(tt)
                P.dma('sp', lambda h, xv=xv, tt=tt: h.dma_start(out=xv.ap, in_=x_out[tt * 128:(tt + 1) * 128, :]),
                      reads=[V(XO, None, lo, hi)], writes=[xv])
                ssq = stat.c(0, 1)
                rs = stat.c(1, 2)
                hsv = A("otmp", f32=True)
                for hf in range(2):
                    sq = stat.c(2 + hf, 3 + hf)
                    xh = xt[tt % ST].c(hf * 1024, (hf + 1) * 1024)
                    P.op('act', lambda h, xh=xh, sq=sq: h.activation(out=hsv.ap, in_=xh.ap, func=AF.Square, accum_out=sq.ap),
                         reads=[xh], writes=[hsv, sq])
                P.op('dve', lambda h: h.tensor_tensor(out=ssq.ap, in0=stat.t[:, 2:3], in1=stat.t[:, 3:4], op=ALU.add),
                     reads=[stat.c(2, 4)], writes=[ssq])
                rstd_from_ssq(ssq, D, rs)
                P.op('dve', lambda h, xv=xv: h.scalar_tensor_tensor(out=xv.ap, in0=xv.ap, scalar=rs.ap, in1=fgb.t[:, :], op0=ALU.mult, op1=ALU.mult),
                     reads=[xv, rs, fgb.c(0, D)], writes=[xv])
                out_sems.append(P.dma('sp', lambda h, xv=xv, tt=tt: h.dma_start(out=x_out[tt * 128:(tt + 1) * 128, :], in_=xv.ap),
                                      reads=[xv], writes=[V(XO, None, lo, hi)]))

        load('sp', cst_identf.c(0, 128), ident_f_d[:, :])
        load('sp', cst_identb.c(0, 128), ident_b_d[:, :])
        load('sp', cst_trin.c(0, 128), trin_d[:, :])
        load('sp', g_mix.c(0, KC), mixg[:, :])
        P.op('pool', lambda h: h.memset(glT.t[:, :], 1.0), writes=[glT.c(0, 512)])
        load_w(wgu_a.c(0, 512, 0, 16), wgu[:, :])
        load_w(wgu_a.c(0, 512, 16, 17), bgate[:, :])
        P.op('pool', lambda h: h.memset(bsum.t[:, :], 0.0), writes=[bsum.c(0, 4)])
        if phase == 1:
            P.op('pool', lambda h: h.memset(Sf.t[:, :], 0.0), writes=[Sf.c(0, 1024)])
            P.op('pool', lambda h: h.memset(Sb.t[:, :], 0.0), writes=[Sb.c(0, 1024)])
        else:
            load('sp', cst_mask4.c(0, 512), mask4_d[:, :])
            load('sp', cst_mcur.c(0, 512), mcur_d[:, :])
            load('sp', cst_mcur0.c(0, 512), mcur0_d[:, :])
            load('sp', cst_mprev.c(0, 512), mprev_d[:, :])
            load('sp', cmask.c(0, 8), cmask_d[:, :])
            load('sp', g_ffn.c(0, KC), ffng[:, :])
            load('sp', psc.c(0, 8), pscale[:, :])
            load('sp', gnb.c(0, 1024), gnorm.partition_broadcast(128))
            load('sp', uprev.c(0, 1024), uhalo[:, :])
            load_w(wpool.c(0, 2048).r("p (a b) -> p a b", b=256), w_pool.rearrange("(a p) d -> p a d", p=128))
            if moe:
                load('sp', wr.c(0, KC * NEXP).r("p (a b) -> p a b", b=NEXP), w_router.rearrange("(a p) e -> p a e", p=128))
                load('sp', cst_ustrict.c(0, 128), ustrict_d[:, :])
                load('sp', cst_ones.c(0, 128), ones_d[:, :])
                P.op('pool', lambda h: h.memset(carry.t[:, :], 0.0), writes=[carry.c(0, NEXP)])
            P.op('pool', lambda h: h.memset(Sf.t[:, :], 0.0), writes=[Sf.c(0, 1024)])
            Lb = A("otmp", f32=True)
            bj = stat.c(8, 12)
            aj = stat.c(12, 16)
            for j in range(NCORES):
                load('sp', Lb, lall[j * 128:(j + 1) * 128, :])
                load('sp', bj, ball[j * 128:(j + 1) * 128, :])
                mj = cmask.c(j, j + 1)
                P.op('dve', lambda h, mj=mj: h.tensor_scalar(out=aj.ap, in0=bj.ap, scalar1=mj.ap, scalar2=None, op0=ALU.mult),
                     reads=[bj, mj], writes=[aj])
                P.op('act', lambda h: h.activation(out=aj.ap, in_=aj.ap, func=AF.Exp), reads=[aj], writes=[aj])
                P.op('dve', lambda h, mj=mj: h.tensor_scalar(out=Lb.ap, in0=Lb.ap, scalar1=mj.ap, scalar2=None, op0=ALU.mult),
                     reads=[Lb, mj], writes=[Lb])
                for hh in range(4):
                    sv = Sf.c(hh * 256, (hh + 1) * 256)
                    lv = V(arena, Lb.ap[:, hh * 256:(hh + 1) * 256], Lb.lo, Lb.hi)
                    av = stat.c(12 + hh, 13 + hh)
                    P.op('dve', lambda h, sv=sv, lv=lv, av=av: h.scalar_tensor_tensor(
                        out=sv.ap, in0=sv.ap, scalar=av.ap, in1=lv.ap, op0=ALU.mult, op1=ALU.add),
                        reads=[sv, lv, av], writes=[sv])
            P.op('dve', lambda h: h.tensor_copy(out=Sb.t[:, :], in_=Sf.t[:, :]), reads=[Sf.c(0, 1024)], writes=[Sb.c(0, 1024)])

        out_sems = []
        zoff = AO["z"][0]

        def Z(ti, lo, hi):
            return arena.c(zoff + ti * 3072 + lo, zoff + ti * 3072 + hi)

        for st in range(NST):
            for ti in range(ST):
                g = st * ST + ti
                load('sp', xt[ti].c(0, D), x_in[g * 128:(g + 1) * 128, :])
                norm_tile(xt[ti], g_mix, ti)

            if STOP <= 1:
                continue
            blocks = [('u', 0, 512, 0), ('u', 512, 512, 512), ('q', 1024, 512, 0), ('k', 1536, 512, 0),
                      ('v', 2048, 512, 1024), ('v', 2560, 512, 1536), ('g', 3072, 16, 0),
                      ('r', 3088, 512, 2048), ('r', 3600, 512, 2560)]
            for kind, c0, ncol, zc in blocks:
                if phase == 1 and (kind in ('q', 'r') or (kind == 'u' and st != NST - 1)):
                    continue
                if phase == 2 and kind in ('k', 'v', 'g'):
                    continue
                ws = wslot()
                wv = ws.c(0, KC * ncol)
                load_w(wv.r("p (a b) -> p a b", b=ncol), w_in[:, c0:c0 + ncol].rearrange("(a p) c -> p a c", p=128))
                if kind in ('u', 'v', 'r'):
                    for ti in range(ST):
                        bank = pb[ti % 4]
                        for kc in range(KC):
                            mm(bank.c(0, 512), hT.c(kc * 512 + ti * 128, kc * 512 + (ti + 1) * 128),
                               ws.c(kc * 512, (kc + 1) * 512), kc == 0, kc == KC - 1)
                        evac_copy(Z(ti, zc, zc + 512), bank.c(0, 512))
                elif kind in ('q', 'k'):
                    for hh in range(4):
                        bank = pb[hh]
                        for kc in range(KC):
                            mm(bank.c(0, 512), ws.c(kc * 512 + hh * 128, kc * 512 + (hh + 1) * 128),
                               hT.c(kc * 512, (kc + 1) * 512), kc == 0, kc == KC - 1)
                        dst = A("qT" if kind == 'q' else "kT").r("p (a b c) -> p a b c", a=ST, b=4).s(slice(None), slice(None), hh, slice(None))
                        evac_copy(dst, bank.c(0, 512).r("p (a c) -> p a c", a=ST))
                else:
                    bank = pb[4]
                    for kc in range(KC):
                        mm(bank.c(0, 512, 0, 16), ws.c(kc * 16, (kc + 1) * 16), hT.c(kc * 512, (kc + 1) * 512),
                           kc == 0, kc == KC - 1)
                    evac_copy(glT.c(0, 512, 0, 16), bank.c(0, 512, 0, 16))

            if phase == 2:
                kv_ = A("kT")
                load('sp', kv_, kT_i[st * 128:(st + 1) * 128, :])
                for ti in range(ST):
                    g = st * ST + ti
                    load('sp', Z(ti, 1024, 2048), v_i[g * 128:(g + 1) * 128, :])
                load('sp', glT.c(0, 512, 0, 16), gl_i[st * 16:(st + 1) * 16, :])
            else:
                kv_ = A("kT")
                out_sems.append(P.dma('sp', lambda h, kv_=kv_, st=st: h.dma_start(out=kT_o[st * 128:(st + 1) * 128, :], in_=kv_.ap), reads=[kv_]))
                for ti in range(ST):
                    g = st * ST + ti
                    vv_ = Z(ti, 1024, 2048)
                    out_sems.append(P.dma('sp', lambda h, vv_=vv_, g=g: h.dma_start(out=v_o[g * 128:(g + 1) * 128, :], in_=vv_.ap), reads=[vv_]))
                gv_ = glT.c(0, 512, 0, 16)
                out_sems.append(P.dma('sp', lambda h, gv_=gv_, st=st: h.dma_start(out=gl_o[st * 16:(st + 1) * 16, :], in_=gv_.ap), reads=[gv_]))
            if STOP <= 2:
                continue
            for ti in range(ST):
                g = st * ST + ti
                kT_t = A("kT", ti * 512, (ti + 1) * 512)
                mm(pb[5].c(0, 512), glT.c(ti * 128, (ti + 1) * 128, 0, 17), wgu_a.c(0, 512, 0, 17), True, True)
                ysp = A("ysp", f32=True)
                spv = A("sp")
                P.op('act', lambda h, ysp=ysp: h.activation(out=ysp.ap, in_=pb[5].t[:, 0:512], func=AF.Exp, scale=-1.0),
                     reads=[pb[5].c(0, 512)], writes=[ysp])
                P.op('act', lambda h, ysp=ysp, spv=spv: h.activation(out=spv.ap, in_=ysp.ap, func=AF.Ln, bias=1.0),
                     reads=[ysp], writes=[spv])
                if STOP <= 2.2:
                    continue
                for hh in range(4):
                    mm(pb[6].c(hh * 128, (hh + 1) * 128), V(arena, spv.ap[:, hh * 128:(hh + 1) * 128], spv.lo, spv.hi),
                       cst_trin.c(0, 128), True, True)
                epos = A("epos", f32=True)
                eneg = A("eneg", f32=True)
                bcv = pb[6].c(0, 512)
                P.op('act', lambda h, epos=epos: h.activation(out=epos.ap, in_=pb[6].t[:, 0:512], func=AF.Exp),
                     reads=[bcv], writes=[epos])
                P.op('act', lambda h, eneg=eneg: h.activation(out=eneg.ap, in_=pb[6].t[:, 0:512], func=AF.Exp, scale=-1.0),
                     reads=[bcv], writes=[eneg])
                if STOP <= 2.3:
                    continue
                bl = V(pb[6], pb[6].t[:, 0:512].rearrange("p (a b) -> p a b", b=128)[:, :, 127], 0, 2048)
                P.op('dve', lambda h, bl=bl: h.tensor_tensor(out=bsum.t[:, :], in0=bl.ap, in1=bsum.t[:, :], op=ALU.add),
                     reads=[bsum.c(0, 4), bl], writes=[bsum.c(0, 4)])
                if STOP <= 2.4:
                    continue
                kd = A("kd")
                P.op('dve', lambda h, kd=kd, kT_t=kT_t, eneg=eneg: h.tensor_tensor(out=kd.ap, in0=kT_t.ap, in1=eneg.ap, op=ALU.mult),
                     reads=[kT_t, eneg], writes=[kd])
                if phase == 2:
                    qT_t = A("qT", ti * 512, (ti + 1) * 512)
                    qd = A("qd")
                    P.op('dve', lambda h, qd=qd, qT_t=qT_t, epos=epos: h.scalar_tensor_tensor(
                        out=qd.ap, in0=qT_t.ap, scalar=float(128 ** -0.5), in1=epos.ap, op0=ALU.mult, op1=ALU.mult),
                        reads=[qT_t, epos], writes=[qd])
                    for hh in range(4):
                        mm(pb[4].c(hh * 128, (hh + 1) * 128), V(arena, kd.ap[:, hh * 128:(hh + 1) * 128], kd.lo, kd.hi),
                           V(arena, qd.ap[:, hh * 128:(hh + 1) * 128], qd.lo, qd.hi), True, True)
                    att = A("att")
                    P.op('dve', lambda h, att=att: h.tensor_tensor(out=att.ap, in0=pb[4].t[:, 0:512], in1=cst_mask4.t[:, :], op=ALU.mult),
                         reads=[pb[4].c(0, 512), cst_mask4.c(0, 512)], writes=[att])
                    for hh in range(4):
                        ob = pb[hh // 2].c((hh % 2) * 256, (hh % 2) * 256 + 256)
                        mm(ob, V(arena, qd.ap[:, hh * 128:(hh + 1) * 128], qd.lo, qd.hi), Sb.c(hh * 256, (hh + 1) * 256), True, False)
                        mm(ob, V(arena, att.ap[:, hh * 128:(hh + 1) * 128], att.lo, att.hi), Z(ti, 1024 + hh * 256, 1024 + (hh + 1) * 256), False, True)
                kteT = A("kteT")
                elb = V(arena, epos.ap.rearrange("p (a b) -> p a b", b=128)[:, :, 127:128].to_broadcast([128, 4, 128]), epos.lo, epos.hi)
                P.op('dve', lambda h, kteT=kteT, kd=kd, elb=elb: h.tensor_tensor(
                    out=kteT.ap.rearrange("p (a b) -> p a b", b=128), in0=kd.ap.rearrange("p (a b) -> p a b", b=128), in1=elb.ap, op=ALU.mult),
                    reads=[kd, elb], writes=[kteT])
                if STOP <= 2.6:
                    continue
                for hh in range(4):
                    tr(pbb.c(hh * 128, (hh + 1) * 128), V(arena, kteT.ap[:, hh * 128:(hh + 1) * 128], kteT.lo, kteT.hi), cst_identb.c(0, 128))
                kte = A("kte")
                P.op('act', lambda h, kte=kte: h.activation(out=kte.ap, in_=pbb.t[:, 0:512], func=AF.Copy),
                     reads=[pbb.c(0, 512)], writes=[kte])
                if STOP <= 2.8:
                    continue
                for hh in range(4):
                    cb = pb[2 + hh // 2].c((hh % 2) * 256, (hh % 2) * 256 + 256)
                    mm(cb, V(arena, kte.ap[:, hh * 128:(hh + 1) * 128], kte.lo, kte.hi), Z(ti, 1024 + hh * 256, 1024 + (hh + 1) * 256), True, True)
                elb2 = V(arena, epos.ap.rearrange("p (a b) -> p a b", b=128)[:, :, 127:128].to_broadcast([128, 4, 256]), epos.lo, epos.hi)
                sall = Sf.c(0, 1024)
                P.op('dve', lambda h, elb2=elb2: h.tensor_tensor(out=Sf.t[:, :].rearrange("p (a b) -> p a b", b=256),
                                                                 in0=Sf.t[:, :].rearrange("p (a b) -> p a b", b=256), in1=elb2.ap, op=ALU.mult),
                     reads=[sall, elb2], writes=[sall])
                for half in range(2):
                    cbk = pb[2 + half].c(0, 512)
                    sv = Sf.c(half * 512, (half + 1) * 512)
                    P.op('dve', lambda h, sv=sv, half=half: h.tensor_tensor(out=sv.ap, in0=pb[2 + half].t[:, 0:512], in1=sv.ap, op=ALU.add),
                         reads=[sv, cbk], writes=[sv])
                P.op('dve', lambda h: h.tensor_copy(out=Sb.t[:, :], in_=Sf.t[:, :]), reads=[Sf.c(0, 1024)], writes=[Sb.c(0, 1024)])

                if phase == 1:
                    continue

                otmp = A("otmp", f32=True)
                ssq4 = stat.c(16, 20)
                rs4 = stat.c(20, 24)
                for hh in range(4):
                    ob = pb[hh // 2].c((hh % 2) * 256, (hh % 2) * 256 + 256)
                    sq = stat.c(16 + hh, 17 + hh)
                    ot = V(arena, otmp.ap[:, hh * 256:(hh + 1) * 256], otmp.lo, otmp.hi)
                    P.op('act', lambda h, ob=ob, sq=sq, ot=ot: h.activation(out=ot.ap, in_=ob.ap, func=AF.Square, accum_out=sq.ap),
                         reads=[ob], writes=[ot, sq])
                rstd_from_ssq(ssq4, 256, rs4)
                for hh in range(4):
                    ob = pb[hh // 2].c((hh % 2) * 256, (hh % 2) * 256 + 256)
                    rv = stat.c(20 + hh, 21 + hh)
                    ot = V(arena, otmp.ap[:, hh * 256:(hh + 1) * 256], otmp.lo, otmp.hi)
                    gv = gnb.c(hh * 256, (hh + 1) * 256)
                    P.op('dve', lambda h, ob=ob, rv=rv, ot=ot, gv=gv: h.scalar_tensor_tensor(
                        out=ot.ap, in0=ob.ap, scalar=rv.ap, in1=gv.ap, op0=ALU.mult, op1=ALU.mult),
                        reads=[ob, rv, gv], writes=[ot])
                sr = A("sr")
                rz = Z(ti, 2048, 3072)
                P.op('act', lambda h, sr=sr, rz=rz: h.activation(out=sr.ap, in_=rz.ap, func=AF.Silu), reads=[rz], writes=[sr])
                og = A("og")
                P.op('dve', lambda h, og=og, otmp=otmp, sr=sr: h.tensor_tensor(out=og.ap, in0=otmp.ap, in1=sr.ap, op=ALU.mult),
                     reads=[otmp, sr], writes=[og])
                for c in range(8):
                    tr(pbb.c(c * 128, (c + 1) * 128), V(arena, og.ap[:, c * 128:(c + 1) * 128], og.lo, og.hi), cst_identb.c(0, 128))
                for c in range(8):
                    evac_copy(mixT.c((8 + c) * 512 + ti * 128, (8 + c) * 512 + (ti + 1) * 128), pbb.c(c * 128, (c + 1) * 128))

                ucur = Z(ti, 0, 1024)
                mc = cst_mcur0 if g == 0 else cst_mcur
                for c in range(8):
                    gi = c // 2
                    ob = pb[4 + c // 4].c((c % 4) * 128, (c % 4 + 1) * 128)
                    mm(ob, uprev.c(c * 128, (c + 1) * 128), cst_mprev.c(gi * 128, (gi + 1) * 128), True, False)
                    mm(ob, V(arena, ucur.ap[:, c * 128:(c + 1) * 128], ucur.lo, ucur.hi), mc.c(gi * 128, (gi + 1) * 128), False, True)
                dT = A("dT")
                evac_copy(V(arena, dT.ap[:, 0:512], dT.lo, dT.hi), pb[4].c(0, 512))
                evac_copy(V(arena, dT.ap[:, 512:1024], dT.lo, dT.hi), pb[5].c(0, 512))
                for oc in range(8):
                    gi, jj = oc // 2, oc % 2
                    ob = pb[4 + oc // 4].c((oc % 4) * 128, (oc % 4 + 1) * 128)
                    for ci in range(2):
                        wv = wpool.c((gi * 2 + ci) * 256 + jj * 128, (gi * 2 + ci) * 256 + (jj + 1) * 128)
                        mm(ob, wv, V(arena, dT.ap[:, (gi * 2 + ci) * 128:(gi * 2 + ci + 1) * 128], dT.lo, dT.hi), ci == 0, ci == 1)
                for oc in range(8):
                    ob = pb[4 + oc // 4].c((oc % 4) * 128, (oc % 4 + 1) * 128)
                    evac_copy(mixT.c(oc * 512 + ti * 128, oc * 512 + (ti + 1) * 128), ob, psc.c(oc, oc + 1))
                P.op('pool', lambda h, ucur=ucur: h.tensor_copy(out=uprev.t[:, :], in_=ucur.ap), reads=[ucur], writes=[uprev.c(0, 1024)])

            if phase == 1:
                continue

            for cb in range(4):
                ws = wslot()
                load_w(ws.c(0, KC * 512).r("p (a b) -> p a b", b=512), w_out[:, cb * 512:(cb + 1) * 512].rearrange("(a p) c -> p a c", p=128))
                for ti in range(ST):
                    bank = pb[ti]
                    for kc in range(KC):
                        mm(bank.c(0, 512), mixT.c(kc * 512 + ti * 128, kc * 512 + (ti + 1) * 128), ws.c(kc * 512, (kc + 1) * 512),
                           kc == 0, kc == KC - 1)
                    xv = xt[ti].c(cb * 512, (cb + 1) * 512)
                    P.op('dve', lambda h, xv=xv, bank=bank: h.tensor_tensor(out=xv.ap, in0=bank.t[:, 0:512], in1=xv.ap, op=ALU.add),
                         reads=[bank.c(0, 512), xv], writes=[xv])

            if moe:
                for ti in range(ST):
                    moe_route_tile(ti, st * ST + ti)
                continue

            for ti in range(ST):
                norm_tile(xt[ti], g_ffn, ti)

            for e in range(NE):
                for half in range(2):
                    f0 = half * HALF
                    for fb in range(HALF // 2):
                        ws = wslot()
                        c0 = (f0 + fb * 2) * 128
                        gv = ws.c(0, KC * 256)
                        uv = ws.c(KC * 256, 2 * KC * 256)
                        load_w(gv.r("p (a b) -> p a b", b=256), wg_d[e * D:(e + 1) * D, c0:c0 + 256].rearrange("(a p) c -> p a c", p=128))
                        load_w(uv.r("p (a b) -> p a b", b=256), wu_d[e * D:(e + 1) * D, c0:c0 + 256].rearrange("(a p) c -> p a c", p=128))
                        for j in range(2):
                            fc = fb * 2 + j
                            gb_ = pb[(fc % 2) * 2]
                            ub_ = pb[(fc % 2) * 2 + 1]
                            for kc in range(KC):
                                mm(gb_.c(0, 512), ws.c(kc * 256 + j * 128, kc * 256 + (j + 1) * 128), hT.c(kc * 512, (kc + 1) * 512),
                                   kc == 0, kc == KC - 1)
                            for kc in range(KC):
                                mm(ub_.c(0, 512), ws.c(KC * 256 + kc * 256 + j * 128, KC * 256 + kc * 256 + (j + 1) * 128),
                                   hT.c(kc * 512, (kc + 1) * 512), kc == 0, kc == KC - 1)
                            sg = hs.c((fc % 2) * 512, (fc % 2) * 512 + 512)
                            P.op('act', lambda h, sg=sg, gb_=gb_: h.activation(out=sg.ap, in_=gb_.t[:, 0:512], func=AF.Silu),
                                 reads=[gb_.c(0, 512)], writes=[sg])
                            av = ACT_(fc)
                            P.op('dve', lambda h, av=av, sg=sg, ub_=ub_: h.tensor_tensor(out=av.ap, in0=ub_.t[:, 0:512], in1=sg.ap, op=ALU.mult),
                                 reads=[ub_.c(0, 512), sg], writes=[av])
                    PIECE = 11 if not moe else 14
                    for cb in range(4):
                        for pi in range(HALF // PIECE):
                            ws = wslot()
                            r0 = e * FF + (f0 + pi * PIECE) * 128
                            load_w(ws.c(0, PIECE * 512).r("p (a b) -> p a b", b=512),
                                   wd_d[r0:r0 + PIECE * 128, cb * 512:(cb + 1) * 512].rearrange("(a p) c -> p a c", p=128))
                            for ti in range(ST):
                                bank = pb[3 + ti]
                                for q in range(PIECE):
                                    fc = pi * PIECE + q
                                    mm(bank.c(0, 512), ACT_(fc, ti * 128, (ti + 1) * 128), ws.c(q * 512, (q + 1) * 512),
                                       fc == 0, fc == HALF - 1)
                        for ti in range(ST):
                            bank = pb[3 + ti]
                            xv = xt[ti].c(cb * 512, (cb + 1) * 512)
                            if moe:
                                wv = wgt.c(ti * NEXP + e, ti * NEXP + e + 1)
                                P.op('dve', lambda h, xv=xv, bank=bank, wv=wv: h.scalar_tensor_tensor(
                                    out=xv.ap, in0=bank.t[:, 0:512], scalar=wv.ap, in1=xv.ap, op0=ALU.mult, op1=ALU.add),
                                    reads=[bank.c(0, 512), wv, xv], writes=[xv])
                            else:
                                P.op('dve', lambda h, xv=xv, bank=bank: h.tensor_tensor(out=xv.ap, in0=bank.t[:, 0:512], in1=xv.ap, op=ALU.add),
                                     reads=[bank.c(0, 512), xv], writes=[xv])

            if final:
                load('sp', fgb.c(0, D), fing.partition_broadcast(128))
            for ti in range(ST):
                g = st * ST + ti
                xv = xt[ti].c(0, D)
                if final:
                    ssq = stat.c(0, 1)
                    rs = stat.c(1, 2)
                    hsv = hs.c(0, D)
                    P.op('act', lambda h, xv=xv: h.activation(out=hsv.ap, in_=xv.ap, func=AF.Square, accum_out=ssq.ap),
                         reads=[xv], writes=[hsv, ssq])
                    rstd_from_ssq(ssq, D, rs)
                    P.op('dve', lambda h, xv=xv: h.scalar_tensor_tensor(out=xv.ap, in0=xv.ap, scalar=rs.ap, in1=fgb.t[:, :], op0=ALU.mult, op1=ALU.mult),
                         reads=[xv, rs, fgb.c(0, D)], writes=[xv])
                out_sems.append(P.dma('sp', lambda h, xv=xv, g=g: h.dma_start(out=x_out[g * 128:(g + 1) * 128, :], in_=xv.ap), reads=[xv]))

        if moe and phase == 2:
            moe_experts()
            moe_final()
        if phase == 1:
            out_sems.append(P.dma('sp', lambda h: h.dma_start(out=lst_o[:, :], in_=Sf.t[:, :]), reads=[Sf.c(0, 1024)]))
            out_sems.append(P.dma('sp', lambda h: h.dma_start(out=bsum_o[:, :], in_=bsum.t[:, :]), reads=[bsum.c(0, 4)]))
            lastu = Z(ST - 1, 0, 1024)
            out_sems.append(P.dma('sp', lambda h: h.dma_start(out=ulast_o[:, :], in_=lastu.ap), reads=[lastu]))
        fin = {}
        for s, v in out_sems:
            fin[id(s)] = (s, max(v, fin.get(id(s), (s, 0))[1]))
        P.wait_all('sp', list(fin.values()))

        with nc.Block() as block:
            @block.tensor
            def _(h):
                P.emit_engine('pe', h)

            @block.scalar
            def _(h):
                P.emit_engine('act', h)

            @block.vector
            def _(h):
                P.emit_engine('dve', h)

            @block.gpsimd
            def _(h):
                P.emit_engine('pool', h)

            @block.sync
            def _(h):
                P.emit_engine('sp', h)
    return nc


_PROG_CACHE = {}


def _get(kind, phase, final):
    key = (kind, phase, final)
    if key not in _PROG_CACHE:
        _PROG_CACHE[key] = build(kind, phase, final)
    return _PROG_CACHE[key]


def kernel(x, mix_norm, w_in, w_pool, pool_scale, w_gate_up, b_gate, gla_norm, w_out,
           ffn_norm, dense_w_gate, dense_w_up, dense_w_down, w_router,
           exp_w_gate, exp_w_up, exp_w_down, final_norm, _nlayers=2):
    f = lambda a: np.ascontiguousarray(np.asarray(a, dtype=np.float32))
    x = f(x).reshape(SEQ, D)
    consts = [_consts(c) for c in range(NCORES)]
    xs = [np.ascontiguousarray(x[c * TPC:(c + 1) * TPC]) for c in range(NCORES)]
    bf = ml_dtypes.bfloat16
    for l in range(_nlayers):
        kind = 'dense' if l % 2 == 0 else 'moe'
        common = {
            "mixg": _fm(f(mix_norm[l])), "w_in": f(w_in[l]), "wgu": f(w_gate_up[l]),
            "bgate": f(b_gate[l]).reshape(1, 512),
        }
        nc1 = _get(kind, 1, False)
        maps = []
        for c in range(NCORES):
            m = dict(common)
            m["x_in"] = xs[c]
            for k in ("ident_f", "ident_b", "trin"):
                m[k] = consts[c][k]
            maps.append(m)
        r1 = run_bass_kernel_spmd(nc1, maps, core_ids=list(range(NCORES))).results
        lall = np.concatenate([np.asarray(r["lst"]) for r in r1], axis=0)
        ball = np.concatenate([np.asarray(r["bsum"]) for r in r1], axis=0)
        final = l == 1
        nc2 = _get(kind, 2, final)
        maps = []
        for c in range(NCORES):
            m = dict(common)
            m["x_in"] = xs[c]
            m.update(consts[c])
            m["lall"] = lall
            m["ball"] = ball
            m["uhalo"] = np.asarray(r1[c - 1]["ulast"]) if c > 0 else np.zeros((128, 1024), bf)
            m["kT_i"] = np.asarray(r1[c]["kT_o"])
            m["v_i"] = np.asarray(r1[c]["v_o"])
            m["gl_i"] = np.asarray(r1[c]["gl_o"])
            m["w_pool"] = f(w_pool[l]).reshape(4 * 256, 256)
            m["pscale"] = _fm(f(pool_scale[l]).reshape(-1))
            m["gnorm"] = f(gla_norm[l]).reshape(1, 1024)
            m["w_out"] = f(w_out[l])
            m["ffng"] = _fm(f(ffn_norm[l]))
            if kind == 'moe':
                i = l // 2
                m["w_router"] = f(w_router[i])
                m["wg"] = f(exp_w_gate[i]).reshape(NEXP * D, FF_EXP)
                m["wu"] = f(exp_w_up[i]).reshape(NEXP * D, FF_EXP)
                m["wd"] = f(exp_w_down[i]).reshape(NEXP * FF_EXP, D)
            else:
                i = l // 2
                m["wg"] = f(dense_w_gate[i])
                m["wu"] = f(dense_w_up[i])
                m["wd"] = f(dense_w_down[i])
            if final:
                m["fing"] = f(final_norm).reshape(1, D)
            maps.append(m)
        r2 = run_bass_kernel_spmd(nc2, maps, core_ids=list(range(NCORES))).results
        xs = [np.asarray(r["x_out"]) for r in r2]
    out = np.concatenate(xs, axis=0).reshape(1, SEQ, D).astype(np.float32)
    return out
```
